# Optimizing a Trainium2 kernel written in Bass

```python
import math
import jax
import jax.numpy as jnp
from jax import lax
import numpy as np

D_MODEL = 2048
BATCH = 4
SEQ = 2048
DEPTH = 2
DEC_BATCH = 128
DEC_SEQ = 8
PAST_LEN = 16384
PAGE_SIZE = 128

N_META = 16
N_EVEN = (DEPTH + 1) // 2
N_ODD = DEPTH // 2

A_WIDTH = D_MODEL // 2
A_HEADS = 4
A_DV = A_WIDTH // A_HEADS
A_DK = A_DV // 2
A_GATE_RANK = 16
A_GATE_TAU = 16.0
A_CHUNK = 64
A_COLS = 2 * A_HEADS * A_DK + 2 * A_WIDTH + A_GATE_RANK

B_WIDTH = D_MODEL // 2
B_HEAD = 64
B_HEADS = B_WIDTH // B_HEAD
B_DECAY_RANK = 64
B_AAA_RANK = 64
B_GATE_RANK = 128
B_COLS = 3 * B_WIDTH + B_DECAY_RANK + B_AAA_RANK + B_GATE_RANK

C_WIDTH = D_MODEL
C_GROUP = 16
C_GROUPS = C_WIDTH // C_GROUP
C_STATE = 64

N_EXPERTS = 16
N_EXPERT_GROUPS = 4
EXPERTS_PER_GROUP = N_EXPERTS // N_EXPERT_GROUPS
TOP_K = 2
D_EXPERT = D_MODEL // 2

ALPHA = (2.0 * DEPTH) ** 0.25
BETA = (8.0 * DEPTH) ** -0.25
LN_EPS = 1e-5
HEAD_NORM_EPS = 1e-5
RWKV_GN_EPS = 64e-5

kernel_name = 'hybrid_gla_rwkv7_s5_moe_step'


def layer_norm(x, g, b):
    xf = x.astype(jnp.float32)
    mu = jnp.mean(xf, -1, keepdims=True)
    xc = xf - mu
    var = jnp.mean(xc * xc, -1, keepdims=True)
    y = xc * lax.rsqrt(var + LN_EPS) * g.astype(jnp.float32) + b.astype(jnp.float32)
    return y.astype(x.dtype)


def gla_segment(q, k, v, lg, s0, chunk):
    bn, L, H, DK = q.shape
    DV = v.shape[-1]
    n = L // chunk

    def to_chunks(t):
        return t.reshape(bn, n, chunk, H, t.shape[-1]).transpose(1, 0, 2, 3, 4)

    qc, kc, vc, gc = to_chunks(q), to_chunks(k), to_chunks(v), to_chunks(lg)
    mask = jnp.tril(jnp.ones((chunk, chunk), dtype=bool))[None, :, :, None, None]

    def step(S, inp):
        qi, ki, vi, gi = inp
        b = jnp.cumsum(gi, axis=1)
        diff = b[:, :, None] - b[:, None, :]
        dec = jnp.exp(jnp.where(mask, diff, -jnp.inf))
        att = jnp.einsum('bihd,bjhd,bijhd->bhij', qi, ki, dec)
        o = (jnp.einsum('bhij,bjhv->bihv', att, vi)
             + jnp.einsum('bihd,bhdv->bihv', qi * jnp.exp(b), S))
        bl = b[:, -1]
        S = (jnp.exp(bl)[..., None] * S
             + jnp.einsum('bjhd,bjhv->bhdv', ki * jnp.exp(bl[:, None] - b), vi))
        return S, o

    S, o = lax.scan(step, s0, (qc, kc, vc, gc))
    o = o.transpose(1, 0, 2, 3, 4).reshape(bn, L, H, DV)
    return o, S


def gla_mixer(pa, gate_up, gate_b, norm_g, s0, segments):
    bn, L, _ = pa.shape
    qk = A_HEADS * A_DK
    q, k, v, r, gd = jnp.split(pa, [qk, 2 * qk, 2 * qk + A_WIDTH, 2 * qk + 2 * A_WIDTH], axis=-1)
    q = q.reshape(bn, L, A_HEADS, A_DK) * (A_DK ** -0.5)
    k = k.reshape(bn, L, A_HEADS, A_DK)
    v = v.reshape(bn, L, A_HEADS, A_DV)
    lg = (jax.nn.log_sigmoid(gd @ gate_up + gate_b) / A_GATE_TAU).reshape(bn, L, A_HEADS, A_DK)
    s = s0.astype(jnp.float32)
    outs = []
    start = 0
    for seg in segments:
        sl = slice(start, start + seg)
        o, s = gla_segment(q[:, sl], k[:, sl], v[:, sl], lg[:, sl], s, math.gcd(seg, A_CHUNK))
        outs.append(o)
        start += seg
    o = jnp.concatenate(outs, axis=1) if len(outs) > 1 else outs[0]
    o = o * lax.rsqrt(jnp.mean(o * o, -1, keepdims=True) + HEAD_NORM_EPS)
    o = o.reshape(bn, L, A_WIDTH) * norm_g * jax.nn.silu(r)
    return o, s


def rwkv7_scan(r, w_log, k, v, kk, a, s0):
    def step(S, inp):
        rt, wt, kt, vt, kkt, at = inp
        sa = jnp.einsum('bhvk,bhk->bhv', S, kkt)
        S = (S * jnp.exp(wt)[:, :, None, :]
             - sa[..., None] * (kkt * at)[:, :, None, :]
             + vt[..., None] * kt[:, :, None, :])
        o = jnp.einsum('bhvk,bhk->bhv', S, rt)
        return S, o

    xs = tuple(t.transpose(1, 0, 2, 3) for t in (r, w_log, k, v, kk, a))
    S, o = lax.scan(step, s0, xs)
    return o.transpose(1, 0, 2, 3), S


def rwkv7_mixer(pb, mu, w0, w_up, a0, a_up, g_up, k_k, k_a, r_k, ln_g, ln_b, s0, shift0):
    bn, L, _ = pb.shape
    W = B_WIDTH
    prev = jnp.concatenate([shift0.astype(jnp.float32)[:, None], pb[:, :-1]], axis=1)
    xm = pb + (prev - pb) * mu
    r, k, v, wd, ad, gd = jnp.split(
        xm, [W, 2 * W, 3 * W, 3 * W + B_DECAY_RANK, 3 * W + B_DECAY_RANK + B_AAA_RANK], axis=-1)
    w = -jax.nn.softplus(-(w0 + jnp.tanh(wd) @ w_up)) - 0.5
    w_log = -jnp.exp(w)
    a = jax.nn.sigmoid(a0 + ad @ a_up)
    g = jax.nn.sigmoid(gd) @ g_up

    def heads(t):
        return t.reshape(bn, L, B_HEADS, B_HEAD)

    kk = heads(k * k_k)
    kk = kk / jnp.maximum(jnp.linalg.norm(kk, axis=-1, keepdims=True), 1e-12)
    k = k * (1.0 + (a - 1.0) * k_a)
    r_h, k_h, v_h, a_h, w_h = heads(r), heads(k), heads(v), heads(a), heads(w_log)
    o, s_new = rwkv7_scan(r_h, w_h, k_h, v_h, kk, a_h, s0.astype(jnp.float32))
    mu_o = jnp.mean(o, -1, keepdims=True)
    oc = o - mu_o
    o = oc * lax.rsqrt(jnp.mean(oc * oc, -1, keepdims=True) + RWKV_GN_EPS)
    o = o.reshape(bn, L, W) * ln_g + ln_b
    bonus = jnp.sum(r_h * k_h * r_k.reshape(B_HEADS, B_HEAD), -1, keepdims=True) * v_h
    o = (o + bonus.reshape(bn, L, W)) * g
    return o, s_new, pb[:, -1]


def even_mixer(x, w_in, w_out, a_gate_up, a_gate_b, a_norm_g, b_mu, b_w0, b_w_up, b_a0,
               b_a_up, b_g_up, b_k_k, b_k_a, b_r_k, b_ln_g, b_ln_b, s_gla, s_rwkv, s_shift,
               segments):
    p = jnp.einsum('bld,dc->blc', x, w_in).astype(jnp.float32)
    o_a, s_gla_new = gla_mixer(p[..., :A_COLS], a_gate_up, a_gate_b, a_norm_g, s_gla, segments)
    o_b, s_rwkv_new, s_shift_new = rwkv7_mixer(
        p[..., A_COLS:], b_mu, b_w0, b_w_up, b_a0, b_a_up, b_g_up, b_k_k, b_k_a, b_r_k,
        b_ln_g, b_ln_b, s_rwkv, s_shift)
    o = jnp.concatenate([o_a, o_b], axis=-1).astype(x.dtype)
    return jnp.einsum('blc,cd->bld', o, w_out), s_gla_new, s_rwkv_new, s_shift_new


def _s5_combine(e1, e2):
    a1, b1 = e1
    a2, b2 = e2
    return a1 * a2, a2 * b1 + b2


def s5_mixer(x, w_in, a_re, a_im, log_dt, b_re, b_im, c_re, c_im, d, w_glu, b_glu, w_out,
             s_re, s_im):
    f32 = jnp.float32
    bn, L, _ = x.shape
    u = jnp.einsum('bld,dc->blc', x, w_in).astype(f32)
    lam = lax.complex(a_re.astype(f32), a_im.astype(f32))
    dt = jnp.exp(log_dt.astype(f32))[:, None]
    a_bar = jnp.exp(lam * dt)
    b_bar = ((a_bar - 1.0) / lam)[..., None] * lax.complex(b_re.astype(f32), b_im.astype(f32))
    ug = u.reshape(bn, L, C_GROUPS, C_GROUP).transpose(1, 0, 2, 3)
    bu = jnp.einsum('lbgc,gpc->lbgp', ug.astype(jnp.complex64), b_bar)
    x0 = lax.complex(s_re.astype(f32), s_im.astype(f32))
    bu = bu.at[0].add(a_bar * x0)
    a_seq = jnp.broadcast_to(a_bar, (L, 1) + a_bar.shape)
    _, states = lax.associative_scan(_s5_combine, (a_seq, bu), axis=0)
    c = lax.complex(c_re.astype(f32), c_im.astype(f32))
    y = jnp.real(jnp.einsum('lbgp,gcp->lbgc', states, c)) + d.astype(f32).reshape(C_GROUPS, C_GROUP) * ug
    y = y.transpose(1, 0, 2, 3).reshape(bn, L, C_WIDTH)
    z = jax.nn.gelu(y)
    z = z * jax.nn.sigmoid(z @ w_glu + b_glu)
    out = jnp.einsum('blc,cd->bld', z.astype(x.dtype), w_out)
    last = states[-1]
    return out, jnp.real(last), jnp.imag(last)


def moe(x, w_router, w_up, w_down):
    bn, L, D = x.shape
    t = x.reshape(bn * L, D)
    probs = jax.nn.softmax(jnp.einsum('td,de->te', t, w_router).astype(jnp.float32), axis=-1)
    pg = probs.reshape(-1, N_EXPERT_GROUPS, EXPERTS_PER_GROUP)
    group_score = jnp.sum(lax.top_k(pg, TOP_K)[0], axis=-1)
    g_sel = jnp.argmax(group_score, axis=-1)
    in_group = jnp.einsum('tg,tge->te', jax.nn.one_hot(g_sel, N_EXPERT_GROUPS, dtype=jnp.float32), pg)
    vals, idx = lax.top_k(in_group, TOP_K)
    vals = vals / jnp.sum(vals, -1, keepdims=True)
    eidx = g_sel[:, None] * EXPERTS_PER_GROUP + idx
    gate = jnp.sum(jax.nn.one_hot(eidx, N_EXPERTS, dtype=jnp.float32) * vals[..., None], axis=1)
    h = jnp.einsum('td,edf->tef', t, w_up)
    h = jax.nn.silu(h[..., :D_EXPERT]) * h[..., D_EXPERT:] * gate[..., None].astype(h.dtype)
    y = jnp.einsum('tef,efd->td', h, w_down)
    return y.reshape(bn, L, D)


def trunk(x, s_gla, s_rwkv, s_shift, s_re, s_im, segments, p):
    gla_new, rwkv_new, shift_new, re_new, im_new = [], [], [], [], []
    for layer in range(DEPTH):
        i = layer // 2
        if layer % 2 == 0:
            h, sg, sr, ss = even_mixer(
                x, p['ev_w_in'][i], p['ev_w_out'][i], p['a_gate_up'][i], p['a_gate_b'][i],
                p['a_norm_g'][i], p['b_mu'][i], p['b_w0'][i], p['b_w_up'][i], p['b_a0'][i],
                p['b_a_up'][i], p['b_g_up'][i], p['b_k_k'][i], p['b_k_a'][i], p['b_r_k'][i],
                p['b_ln_g'][i], p['b_ln_b'][i], s_gla[i], s_rwkv[i], s_shift[i], segments)
            gla_new.append(sg)
            rwkv_new.append(sr)
            shift_new.append(ss)
        else:
            h, sre, sim = s5_mixer(
                x, p['od_w_in'][i], p['c_a_re'][i], p['c_a_im'][i], p['c_log_dt'][i],
                p['c_b_re'][i], p['c_b_im'][i], p['c_c_re'][i], p['c_c_im'][i], p['c_d'][i],
                p['c_w_glu'][i], p['c_b_glu'][i], p['od_w_out'][i], s_re[i], s_im[i])
            re_new.append(sre)
            im_new.append(sim)
        x = layer_norm(ALPHA * x + h, p['ln_mix_g'][layer], p['ln_mix_b'][layer])
        f = moe(x, p['w_router'], p['moe_w_up'][layer], p['moe_w_down'][layer])
        x = layer_norm(ALPHA * x + f, p['ln_ffn_g'][layer], p['ln_ffn_b'][layer])
    return (x, jnp.stack(gla_new), jnp.stack(rwkv_new), jnp.stack(shift_new),
            jnp.stack(re_new), jnp.stack(im_new))


def setup_inputs(seed: int = 0) -> dict:
    key = jax.random.key(seed)
    ks = iter(jax.random.split(key, 64))

    def nrm(shape, std):
        return std * jax.random.normal(next(ks), shape, jnp.float32)

    def unif(shape, lo, hi):
        return jax.random.uniform(next(ks), shape, jnp.float32, lo, hi)

    ev_cols = A_COLS + B_COLS
    n_idx = jnp.arange(C_STATE, dtype=jnp.float32)
    w0_base = -6.5 + 5.0 * jnp.linspace(0.0, 1.0, B_WIDTH, dtype=jnp.float32) ** 1.5
    return {
        'x_prompt': nrm((BATCH, SEQ, D_MODEL), 1.0),
        'x_sample': nrm((DEC_BATCH, DEC_SEQ, D_MODEL), 1.0),
        'state_gla': nrm((N_EVEN, DEC_BATCH, A_HEADS, A_DK, A_DV), 0.3),
        'state_rwkv': nrm((N_EVEN, DEC_BATCH, B_HEADS, B_HEAD, B_HEAD), 0.3),
        'state_shift': nrm((N_EVEN, DEC_BATCH, B_COLS), 1.0),
        'state_s5_re': nrm((N_ODD, DEC_BATCH, C_GROUPS, C_STATE), 0.3),
        'state_s5_im': nrm((N_ODD, DEC_BATCH, C_GROUPS, C_STATE), 0.3),
        'meta': nrm((N_META, D_MODEL), 1.0),
        'ev_w_in': nrm((N_EVEN, D_MODEL, ev_cols), D_MODEL ** -0.5),
        'ev_w_out': nrm((N_EVEN, A_WIDTH + B_WIDTH, D_MODEL), BETA * (A_WIDTH + B_WIDTH) ** -0.5),
        'a_gate_up': nrm((N_EVEN, A_GATE_RANK, A_HEADS * A_DK), A_GATE_RANK ** -0.5),
        'a_gate_b': nrm((N_EVEN, A_HEADS * A_DK), 0.1),
        'a_norm_g': 1.0 + nrm((N_EVEN, A_WIDTH), 0.02),
        'b_mu': unif((N_EVEN, B_COLS), 0.0, 1.0),
        'b_w0': w0_base + nrm((N_EVEN, B_WIDTH), 0.1),
        'b_w_up': nrm((N_EVEN, B_DECAY_RANK, B_WIDTH), 0.1 * B_DECAY_RANK ** -0.5),
        'b_a0': nrm((N_EVEN, B_WIDTH), 0.1),
        'b_a_up': nrm((N_EVEN, B_AAA_RANK, B_WIDTH), 0.1 * B_AAA_RANK ** -0.5),
        'b_g_up': nrm((N_EVEN, B_GATE_RANK, B_WIDTH), B_GATE_RANK ** -0.5),
        'b_k_k': 0.85 + nrm((N_EVEN, B_WIDTH), 0.02),
        'b_k_a': 1.0 + nrm((N_EVEN, B_WIDTH), 0.02),
        'b_r_k': nrm((N_EVEN, B_WIDTH), 0.1),
        'b_ln_g': 1.0 + nrm((N_EVEN, B_WIDTH), 0.02),
        'b_ln_b': nrm((N_EVEN, B_WIDTH), 0.01),
        'od_w_in': nrm((N_ODD, D_MODEL, C_WIDTH), D_MODEL ** -0.5),
        'c_a_re': -0.5 + nrm((N_ODD, C_GROUPS, C_STATE), 0.01),
        'c_a_im': math.pi * n_idx + nrm((N_ODD, C_GROUPS, C_STATE), 0.01),
        'c_log_dt': unif((N_ODD, C_GROUPS), math.log(1e-3), math.log(1e-1)),
        'c_b_re': nrm((N_ODD, C_GROUPS, C_STATE, C_GROUP), (2.0 * C_GROUP) ** -0.5),
        'c_b_im': nrm((N_ODD, C_GROUPS, C_STATE, C_GROUP), (2.0 * C_GROUP) ** -0.5),
        'c_c_re': nrm((N_ODD, C_GROUPS, C_GROUP, C_STATE), 0.5),
        'c_c_im': nrm((N_ODD, C_GROUPS, C_GROUP, C_STATE), 0.5),
        'c_d': nrm((N_ODD, C_WIDTH), 0.5),
        'c_w_glu': nrm((N_ODD, C_WIDTH, C_WIDTH), C_WIDTH ** -0.5),
        'c_b_glu': nrm((N_ODD, C_WIDTH), 0.01),
        'od_w_out': nrm((N_ODD, C_WIDTH, D_MODEL), BETA * C_WIDTH ** -0.5),
        'w_router': nrm((D_MODEL, N_EXPERTS), D_MODEL ** -0.5),
        'moe_w_up': nrm((DEPTH, N_EXPERTS, D_MODEL, 2 * D_EXPERT), D_MODEL ** -0.5),
        'moe_w_down': nrm((DEPTH, N_EXPERTS, D_EXPERT, D_MODEL), BETA * D_EXPERT ** -0.5),
        'ln_mix_g': 1.0 + nrm((DEPTH, D_MODEL), 0.02),
        'ln_mix_b': nrm((DEPTH, D_MODEL), 0.01),
        'ln_ffn_g': 1.0 + nrm((DEPTH, D_MODEL), 0.02),
        'ln_ffn_b': nrm((DEPTH, D_MODEL), 0.01),
    }


def reference(x_prompt, x_sample, state_gla, state_rwkv, state_shift, state_s5_re, state_s5_im,
              meta, ev_w_in, ev_w_out, a_gate_up, a_gate_b, a_norm_g, b_mu, b_w0, b_w_up, b_a0,
              b_a_up, b_g_up, b_k_k, b_k_a, b_r_k, b_ln_g, b_ln_b, od_w_in, c_a_re, c_a_im,
              c_log_dt, c_b_re, c_b_im, c_c_re, c_c_im, c_d, c_w_glu, c_b_glu, od_w_out,
              w_router, moe_w_up, moe_w_down, ln_mix_g, ln_mix_b, ln_ffn_g, ln_ffn_b):
    p = dict(ev_w_in=ev_w_in, ev_w_out=ev_w_out, a_gate_up=a_gate_up, a_gate_b=a_gate_b,
             a_norm_g=a_norm_g, b_mu=b_mu, b_w0=b_w0, b_w_up=b_w_up, b_a0=b_a0, b_a_up=b_a_up,
             b_g_up=b_g_up, b_k_k=b_k_k, b_k_a=b_k_a, b_r_k=b_r_k, b_ln_g=b_ln_g, b_ln_b=b_ln_b,
             od_w_in=od_w_in, c_a_re=c_a_re, c_a_im=c_a_im, c_log_dt=c_log_dt, c_b_re=c_b_re,
             c_b_im=c_b_im, c_c_re=c_c_re, c_c_im=c_c_im, c_d=c_d, c_w_glu=c_w_glu,
             c_b_glu=c_b_glu, od_w_out=od_w_out, w_router=w_router, moe_w_up=moe_w_up,
             moe_w_down=moe_w_down, ln_mix_g=ln_mix_g, ln_mix_b=ln_mix_b, ln_ffn_g=ln_ffn_g,
             ln_ffn_b=ln_ffn_b)
    f32 = jnp.float32
    bp, sp = x_prompt.shape[0], x_prompt.shape[1]
    xp = jnp.concatenate(
        [jnp.broadcast_to(meta.astype(x_prompt.dtype)[None], (bp, N_META, D_MODEL)), x_prompt], axis=1)
    yp, gla_p, rwkv_p, shift_p, re_p, im_p = trunk(
        xp,
        jnp.zeros((N_EVEN, bp, A_HEADS, A_DK, A_DV), f32),
        jnp.zeros((N_EVEN, bp, B_HEADS, B_HEAD, B_HEAD), f32),
        jnp.zeros((N_EVEN, bp, B_COLS), f32),
        jnp.zeros((N_ODD, bp, C_GROUPS, C_STATE), f32),
        jnp.zeros((N_ODD, bp, C_GROUPS, C_STATE), f32),
        (N_META, sp), p)
    ys, gla_s, rwkv_s, shift_s, re_s, im_s = trunk(
        x_sample, state_gla, state_rwkv, state_shift, state_s5_re, state_s5_im,
        (x_sample.shape[1],), p)
    return (yp[:, N_META:], ys, gla_p, gla_s, rwkv_p, rwkv_s, shift_p, shift_s,
            re_p, re_s, im_p, im_s)
```

```python
import contextlib
import math
import os
import numpy as np
import concourse.bass as bass
import concourse.mybir as mybir
from concourse.bass_utils import run_bass_kernel_spmd

F32 = mybir.dt.float32
BF16 = mybir.dt.bfloat16
AF = mybir.ActivationFunctionType
ALU = mybir.AluOpType
AX = mybir.AxisListType

D = 2048
A_COLS = 3088
B_COLS = 3328
EV_COLS = A_COLS + B_COLS
ALPHA = 4.0 ** 0.25
LN_EPS = 1e-5


class Buf:
    __slots__ = ("name", "w", "r")

    def __init__(self, name=""):
        self.name = name
        self.w = []
        self.r = []


class Op:
    __slots__ = ("eng", "fn", "waits", "signal", "sigval", "is_dma", "dsem", "dval", "epoch")

    def __init__(self, eng, fn, is_dma=False, epoch=0):
        self.eng = eng
        self.fn = fn
        self.waits = []
        self.signal = False
        self.sigval = 0
        self.is_dma = is_dma
        self.dsem = None
        self.dval = 0
        self.epoch = epoch


class Prog:
    ENGS = ("pe", "act", "dve", "pool", "sp")
    ENGOBJ = {"pe": "tensor", "act": "scalar", "dve": "vector", "pool": "gpsimd", "sp": "sync"}

    def __init__(self, nc, stack, dma_slots=None):
        self.nc = nc
        self.ops = {e: [] for e in self.ENGS}
        self.dma_slots = dma_slots or {"sp": 16, "act": 8, "pool": 12}
        self.dma_count = {q: 0 for q in self.dma_slots}
        self.dma_last = {q: [None] * n for q, n in self.dma_slots.items()}
        self.epoch = 0
        self.sigbase = {e: 0 for e in self.ENGS}
        self.known = {e: {} for e in self.ENGS}
        self.esem = {e: stack.enter_context(nc.semaphore(f"s_{e}")) for e in self.ENGS}
        self.dsem = {}
        for q, n in self.dma_slots.items():
            for s in range(n):
                self.dsem[(q, s)] = stack.enter_context(nc.semaphore(f"d_{q}{s}"))

    def _deps(self, rec, reads, writes):
        deps = []
        for b in reads:
            deps.extend(b.w)
        for b in writes:
            for d in b.w:
                if d.is_dma or rec.is_dma or d.eng != rec.eng:
                    deps.append(d)
            for d in b.r:
                if d.is_dma or rec.is_dma or d.eng != rec.eng:
                    deps.append(d)
        seen = set()
        for d in deps:
            if d is rec or id(d) in seen or d.epoch < self.epoch:
                continue
            if (not d.is_dma) and (not rec.is_dma) and d.eng == "pe" and rec.eng == "pe":
                continue
            seen.add(id(d))
            rec.waits.append(d)
            d.signal = True
        for b in reads:
            if not rec.is_dma:
                b.r = [x for x in b.r if x.is_dma or x.eng != rec.eng]
            b.r.append(rec)
        for b in writes:
            b.w = [rec]
            b.r = []

    def op(self, eng, fn, reads=(), writes=()):
        rec = Op(eng, fn, epoch=self.epoch)
        self._deps(rec, reads, writes)
        self.ops[eng].append(rec)
        return rec

    def dma(self, q, out, in_, reads=(), writes=(), **kw):
        rec = Op(q, (out, in_, kw), is_dma=True, epoch=self.epoch)
        i = self.dma_count[q]
        self.dma_count[q] += 1
        K = self.dma_slots[q]
        slot = i % K
        rec.dsem = (q, slot)
        rec.dval = 16 * (i // K + 1)
        prev = self.dma_last[q][slot]
        if prev is not None and prev.epoch == self.epoch:
            rec.waits.append(prev)
        self.dma_last[q][slot] = rec
        self._deps(rec, reads, writes)
        self.ops[q].append(rec)
        return rec

    def flush(self):
        nc = self.nc
        for e in self.ENGS:
            last = None
            for rec in self.ops[e]:
                if not rec.is_dma:
                    last = rec
            if last is not None:
                last.signal = True
            c = self.sigbase[e]
            for rec in self.ops[e]:
                if (not rec.is_dma) and rec.signal:
                    c += 1
                    rec.sigval = c
            self.sigbase[e] = c
        final = {}
        for e in self.ENGS:
            final[e] = self.sigbase[e]
        dfinal = {}
        for q, lst in self.dma_last.items():
            for s, rec in enumerate(lst):
                if rec is not None:
                    dfinal[(q, s)] = rec.dval
        with nc.Block() as block:
            def make(e):
                def body(eng):
                    known = self.known[e]
                    for rec in self.ops[e]:
                        for d in rec.waits:
                            if d.is_dma:
                                key, val, sem = d.dsem, d.dval, self.dsem[d.dsem]
                            else:
                                key, val, sem = d.eng, d.sigval, self.esem[d.eng]
                            if known.get(key, 0) >= val:
                                continue
                            known[key] = val
                            eng.wait_ge(sem, val)
                        if rec.is_dma:
                            out, in_, kw = rec.fn
                            eng.dma_start(out=out, in_=in_, **kw).then_inc(self.dsem[rec.dsem], 16)
                        else:
                            ins = rec.fn(eng)
                            if rec.signal:
                                ins.then_inc(self.esem[e], 1)
                    for k, val in final.items():
                        if val > 0 and known.get(k, 0) < val and k != e:
                            known[k] = val
                            eng.wait_ge(self.esem[k], val)
                    for k, val in dfinal.items():
                        if known.get(k, 0) < val:
                            known[k] = val
                            eng.wait_ge(self.dsem[k], val)
                return body
            for e in self.ENGS:
                getattr(block, self.ENGOBJ[e])(make(e))
        self.ops = {e: [] for e in self.ENGS}
        self.epoch += 1


class V:
    __slots__ = ("ap", "buf")

    def __init__(self, ap, buf):
        self.ap = ap
        self.buf = buf

    def __getitem__(self, idx):
        return V(self.ap[idx], self.buf)

    def re(self, pat, **kw):
        return V(self.ap.rearrange(pat, **kw), self.buf)

    def bc(self, shape):
        return V(self.ap.to_broadcast(list(shape)), self.buf)


class K:
    def __init__(self, nc, stack):
        self.nc = nc
        self.P = Prog(nc, stack)
        self.n = 0

    def sb(self, st, shape, dt=F32, name=None):
        self.n += 1
        h = st.enter_context(self.nc.sbuf_tensor(f"{name or 't'}{self.n}", list(shape), dt))
        return V(h[tuple(slice(None) for _ in shape)], Buf(name or "t"))

    def ps(self, st, shape, dt=F32, name=None):
        self.n += 1
        h = st.enter_context(self.nc.psum_tensor(f"{name or 'p'}{self.n}", list(shape), dt))
        return V(h[tuple(slice(None) for _ in shape)], Buf(name or "p"))

    def dma(self, q, out, in_, **kw):
        outv = out if isinstance(out, V) else V(out, None)
        inv = in_ if isinstance(in_, V) else V(in_, None)
        return self.P.dma(q, outv.ap, inv.ap,
                          reads=[inv.buf] if inv.buf is not None else [],
                          writes=[outv.buf] if outv.buf is not None else [], **kw)

    def mm(self, out, lhsT, rhs, start=True, stop=True):
        self.P.op("pe", lambda e: e.matmul(out.ap, lhsT.ap, rhs.ap, start=start, stop=stop),
                  reads=[lhsT.buf, rhs.buf], writes=[out.buf])

    def tr(self, out, in_, ident):
        self.P.op("pe", lambda e: e.transpose(out.ap, in_.ap, ident.ap),
                  reads=[in_.buf, ident.buf], writes=[out.buf])

    def tt(self, eng, out, a, b, op):
        self.P.op(eng, lambda e: e.tensor_tensor(out.ap, a.ap, b.ap, op),
                  reads=[a.buf, b.buf], writes=[out.buf])

    def ts(self, eng, out, a, s1, s2=None, op0=ALU.mult, op1=None):
        rd = [a.buf]
        s1a = s1.ap if isinstance(s1, V) else s1
        s2a = s2.ap if isinstance(s2, V) else s2
        if isinstance(s1, V):
            rd.append(s1.buf)
        if isinstance(s2, V):
            rd.append(s2.buf)
        if op1 is None:
            self.P.op(eng, lambda e: e.tensor_scalar(out.ap, a.ap, s1a, None, op0), reads=rd, writes=[out.buf])
        else:
            self.P.op(eng, lambda e: e.tensor_scalar(out.ap, a.ap, s1a, s2a, op0, op1), reads=rd, writes=[out.buf])

    def stt(self, eng, out, a, s, b, op0, op1):
        rd = [a.buf, b.buf]
        sa = s.ap if isinstance(s, V) else s
        if isinstance(s, V):
            rd.append(s.buf)
        self.P.op(eng, lambda e: e.scalar_tensor_tensor(out.ap, a.ap, sa, b.ap, op0, op1), reads=rd, writes=[out.buf])

    def act(self, out, a, func, bias=None, scale=None):
        rd = [a.buf]
        kw = {}
        if bias is not None:
            kw["bias"] = bias.ap if isinstance(bias, V) else bias
            if isinstance(bias, V):
                rd.append(bias.buf)
        if scale is not None:
            kw["scale"] = scale.ap if isinstance(scale, V) else scale
            if isinstance(scale, V):
                rd.append(scale.buf)
        self.P.op("act", lambda e: e.activation(out.ap, a.ap, func, **kw), reads=rd, writes=[out.buf])

    def cp(self, eng, out, a):
        if eng == "act":
            self.P.op("act", lambda e: e.copy(out.ap, a.ap), reads=[a.buf], writes=[out.buf])
        else:
            self.P.op(eng, lambda e: e.tensor_copy(out.ap, a.ap), reads=[a.buf], writes=[out.buf])

    def red(self, eng, out, a, op=ALU.add, axis=AX.X):
        self.P.op(eng, lambda e: e.tensor_reduce(out.ap, a.ap, axis, op), reads=[a.buf], writes=[out.buf])

    def recip(self, out, a):
        self.P.op("dve", lambda e: e.reciprocal(out.ap, a.ap), reads=[a.buf], writes=[out.buf])

    def rsqrt(self, out, a, scale, bias):
        self.act(out, a, AF.Sqrt, bias=bias, scale=scale)
        self.recip(out, out)

    def memset(self, eng, out, val):
        self.P.op(eng, lambda e: e.memset(out.ap, val), reads=[], writes=[out.buf])


def row_bcast(ap1d, nparts):
    n = ap1d.shape[-1]
    return ap1d.rearrange("(o n) -> o n", o=1).to_broadcast([nparts, n])


def make_consts():
    c = {}
    c["ident"] = np.eye(128, dtype=np.float32)
    i = np.arange(128)
    c["tri_incl"] = (i[:, None] <= i[None, :]).astype(np.float32)
    c["tri_strict"] = (i[:, None] < i[None, :]).astype(np.float32)
    c["tri_gt"] = (i[:, None] > i[None, :]).astype(np.float32)
    c["ones"] = np.ones((128, 128), dtype=np.float32)
    sel = np.zeros((128, 16, 128), dtype=np.float32)
    for e in range(16):
        sel[e, e, :] = 1.0
    c["sel"] = sel.reshape(128, 16 * 128)
    pm = np.zeros((128, 8), dtype=np.float32)
    for q in range(4):
        pm[32 * q:32 * q + 32, 4 + q] = 1.0
    pm[:64, 0] = 1.0
    pm[64:, 1] = 1.0
    pm[:, 2] = ((i // 16) % 2 == 0)
    pm[:, 3] = ((i // 16) % 2 == 1)
    return np.concatenate([c["ident"], c["tri_incl"], c["tri_strict"], c["tri_gt"], c["ones"], c["sel"], pm], axis=1)


C_IDENT, C_TRII, C_TRIS, C_TRIG, C_ONES, C_SEL = 0, 128, 256, 384, 512, 640
C_PM = 640 + 16 * 128
C_TOTAL = C_PM + 8


class Cfg:
    def __init__(self, n_prompt=2048, n_samp=16, debug=False, phases=None):
        self.n_meta = 16
        self.n_prompt = n_prompt
        self.NP = 16 + n_prompt
        self.n_samp = n_samp
        self.NT = self.NP + 8 * n_samp
        self.debug = debug
        self.phases = phases
        ch = [(0, 16, True, -1)]
        r = 16
        while r < self.NP:
            ch.append((r, 64, False, -1))
            r += 64
        for s in range(n_samp):
            ch.append((self.NP + 8 * s, 8, True, s))
        self.chunks = ch
        self.subtiles = []
        r = 0
        while r < self.NT:
            n = min(128, self.NT - r)
            self.subtiles.append((r, n))
            r += n


def load_consts(kb, st, cst):
    t = kb.sb(st, [128, C_TOTAL], F32, "consts")
    kb.dma("sp", t, cst)
    tb = kb.sb(st, [128, 128], BF16, "identb")
    kb.cp("dve", tb, t[:, C_IDENT:C_IDENT + 128])
    return t, tb


def dense_phase(kb, cfg, cst, src, W, ncols, epi, post=None, pre=None, srcT=None):
    with contextlib.ExitStack() as st:
        consts, identb = load_consts(kb, st, cst)
        KC = 16
        Wb = kb.sb(st, [128, KC, ncols], BF16, "Wb")
        Wv = W.rearrange("(kc p) n -> p kc n", p=128)
        for g in range(4):
            kb.dma("pool", Wb[:, 4 * g:4 * g + 4, :], Wv[:, 4 * g:4 * g + 4, :])
        xs = [kb.sb(st, [128, D], F32, "xs") for _ in range(2)]
        xb = [kb.sb(st, [128, D], BF16, "xb") for _ in range(2)]
        xT = [kb.sb(st, [128, KC, 128], BF16, "xT") for _ in range(2)]
        ptb = [kb.ps(st, [128, KC, 128], BF16, "ptb") for _ in range(1)]
        pacc = [kb.ps(st, [128, 512], F32, "pacc") for _ in range(4)]
        ctx = {"st": st, "consts": consts, "identb": identb}
        if pre is not None:
            pre(ctx)
        ia = 0
        for ts, (r0, n) in enumerate(cfg.subtiles):
            a = ts % 2
            if srcT is not None:
                kb.dma("sp", xT[a][:, :, :n], srcT[:, :, r0:r0 + n].rearrange("kc p t -> p kc t"))
            else:
                kb.dma("sp", xs[a][:n, :], src[r0:r0 + n, :])
                kb.cp("act", xb[a][:n, :], xs[a][:n, :])
                pt = ptb[0]
                for kc in range(KC):
                    kb.tr(pt[:, kc, :n], xb[a][:n, kc * 128:(kc + 1) * 128], identb[:n, :n])
                kb.cp("dve", xT[a][:, 0:8, :n], pt[:, 0:8, :n])
                kb.cp("dve", xT[a][:, 8:16, :n], pt[:, 8:16, :n])
            ncb = (ncols + 511) // 512
            for cb in range(ncb):
                cw = min(512, ncols - cb * 512)
                pa = pacc[ia % 4]
                ia += 1
                for kc in range(KC):
                    kb.mm(pa[:n, :cw], xT[a][:, kc, :n], Wb[:, kc, cb * 512:cb * 512 + cw],
                          start=(kc == 0), stop=(kc == KC - 1))
                epi(ctx, ts, r0, n, cb, cw, pa)
            if post is not None:
                post(ctx, ts, r0, n)
        kb.P.flush()


def phase_inproj(kb, cfg, cst, xtok, w_in, P0):
    for c0 in range(0, EV_COLS, 2048):
        ncols = min(2048, EV_COLS - c0)
        state = {"i": 0}

        def pre(ctx):
            ctx["ot"] = [kb.sb(ctx["st"], [128, 512], F32, "ot") for _ in range(4)]

        def epi(ctx, ts, r0, n, cb, cw, pa, c0=c0, state=state):
            i = state["i"]
            state["i"] += 1
            ot = ctx["ot"][i % 4]
            kb.cp("act" if i % 2 == 0 else "dve", ot[:n, :cw], pa[:n, :cw])
            kb.dma("sp", P0[r0:r0 + n, c0 + cb * 512:c0 + cb * 512 + cw], ot[:n, :cw])

        dense_phase(kb, cfg, cst, xtok, w_in[:, c0:c0 + ncols], ncols, epi, pre=pre)


WEIGHT_SHAPES = {
    "ev_w_in": [D, EV_COLS], "ev_w_out": [D, D], "a_gate_up": [16, 512], "a_gate_b": [512],
    "a_norm_g": [1024], "b_mu": [B_COLS], "b_w0": [1024], "b_w_up": [64, 1024], "b_a0": [1024],
    "b_a_up": [64, 1024], "b_g_up": [128, 1024], "b_k_k": [1024], "b_k_a": [1024], "b_r_k": [1024],
    "b_ln_g": [1024], "b_ln_b": [1024], "od_w_in": [D, D], "c_a_re": [128, 64], "c_a_im": [128, 64],
    "c_log_dt": [128], "c_b_re": [128, 64, 16], "c_b_im": [128, 64, 16], "c_c_re": [128, 16, 64],
    "c_c_im": [128, 16, 64], "c_d": [D], "c_w_glu": [D, D], "c_b_glu": [D], "od_w_out": [D, D],
    "w_router": [D, 16], "moe_w_up": [2, 16, D, D], "moe_w_down": [2, 16, 1024, D],
    "ln_mix_g": [2, D], "ln_mix_b": [2, D], "ln_ffn_g": [2, D], "ln_ffn_b": [2, D],
}


def build(cfg):
    nc = bass.Bass("TRN2", target_bir_lowering=False)
    NT, NS = cfg.NT, cfg.n_samp
    ins = {}

    def inp(name, shape):
        ins[name] = nc.dram_tensor(name, list(shape), F32, kind="ExternalInput").ap()
        return ins[name]

    inp("xtok", [NT, D])
    inp("consts", [128, C_TOTAL])
    inp("state_gla", [NS, 4, 128, 256])
    inp("state_rwkv", [NS, 16, 64, 64])
    inp("state_shift", [NS, B_COLS])
    inp("state_s5_re", [NS, 128, 64])
    inp("state_s5_im", [NS, 128, 64])
    for k, shp in WEIGHT_SHAPES.items():
        if cfg.phases is not None and k.startswith("moe_") and not any(p.startswith("moe") for p in cfg.phases):
            continue
        inp(k, shp)
    outs = {}

    def outp(name, shape):
        outs[name] = nc.dram_tensor(name, list(shape), F32, kind="ExternalOutput").ap()
        return outs[name]

    def scratch(name, shape, dt=F32):
        kind = "ExternalOutput" if cfg.debug else "Internal"
        t = nc.dram_tensor(name, list(shape), dt, kind=kind).ap()
        if cfg.debug:
            outs[name] = t
        return t

    outp("y", [NT, D])
    outp("gla_p", [4, 128, 256]); outp("gla_s", [NS, 4, 128, 256])
    outp("rwkv_p", [16, 64, 64]); outp("rwkv_s", [NS, 16, 64, 64])
    outp("shift_p", [B_COLS]); outp("shift_s", [NS, B_COLS])
    outp("s5re_p", [128, 64]); outp("s5re_s", [NS, 128, 64])
    outp("s5im_p", [128, 64]); outp("s5im_s", [NS, 128, 64])
    S = {}
    S["P0"] = scratch("P0", [NT, EV_COLS])
    S["OM"] = scratch("OM", [NT, D])
    S["X1"] = scratch("X1", [NT, D])
    S["X1T"] = scratch("X1T", [16, 128, NT], BF16)
    S["GT"] = scratch("GT", [16, NT])
    S["X2"] = scratch("X2", [NT, D])
    S["UT"] = scratch("UT", [16, 128, NT])
    S["ZT"] = scratch("ZT", [16, 128, NT], BF16)
    S["X2T"] = scratch("X2T", [16, 128, NT], BF16)
    S["ZZT"] = scratch("ZZT", [16, 128, NT], BF16)
    S["X3"] = scratch("X3", [NT, D])
    S["X3T"] = scratch("X3T", [16, 128, NT], BF16)
    S["GT3"] = scratch("GT3", [16, NT])

    ph = cfg.phases
    with contextlib.ExitStack() as stack:
        kb = K(nc, stack)
        if ph is None or "inproj" in ph:
            phase_inproj(kb, cfg, ins["consts"], ins["xtok"], ins["ev_w_in"], S["P0"])
        if ph is None or "mix0" in ph:
            phase_mix0(kb, cfg, ins, outs, S)
        if ph is None or "out0" in ph:
            phase_outln(kb, cfg, ins, S["OM"], ins["ev_w_out"], ins["xtok"], 0, S["X1"], S["X1T"], S["GT"])
        if ph is None or "moe0" in ph:
            phase_moe(kb, cfg, ins, 0, S["X1"], S["X1T"], S["GT"], S["X2"], S["X2T"])
        if ph is None or "l1" in ph:
            phase_l1(kb, cfg, ins, outs, S)
        if ph is None or "moe1" in ph:
            phase_moe(kb, cfg, ins, 1, S["X3"], S["X3T"], S["GT3"], outs["y"])
    return nc, ins, outs


def phase_l1(*a, **k):
    raise NotImplementedError


class Rot:
    def __init__(self, tiles):
        self.t = tiles
        self.i = 0

    def __call__(self):
        t = self.t[self.i % len(self.t)]
        self.i += 1
        return t


def phase_mix0(kb, cfg, ins, outs, S):
    RSTOP = int(os.environ.get('RSTOP', '99'))
    P0, OM = S["P0"], S["OM"]
    C0 = math.exp(-0.5)
    with contextlib.ExitStack() as st:
        cs = kb.sb(st, [128, 640], F32, "consts")
        kb.dma("sp", cs, ins["consts"][:, 0:640])
        ident = cs[:, C_IDENT:C_IDENT + 128]
        tri_i = cs[:, C_TRII:C_TRII + 128]
        tri_s = cs[:, C_TRIS:C_TRIS + 128]
        tri_g = cs[:, C_TRIG:C_TRIG + 128]
        ones = cs[:, C_ONES:C_ONES + 128]

        def bcast(name, n, parts=64):
            t = kb.sb(st, [parts, n], F32, name)
            kb.dma("sp", t, row_bcast(ins[name], parts))
            return t

        mu_bc = bcast("b_mu", B_COLS)
        kk_bc = bcast("b_k_k", 1024)
        ka_bc = bcast("b_k_a", 1024)
        rk_bc = bcast("b_r_k", 1024)
        lng_bc = bcast("b_ln_g", 1024)
        lnb_bc = bcast("b_ln_b", 1024)
        ng_bc = bcast("a_norm_g", 1024)
        w0_row = bcast("b_w0", 1024, 1)
        a0_row = bcast("b_a0", 1024, 1)
        gb_row = bcast("a_gate_b", 512, 1)
        w_up = kb.sb(st, [64, 1024], F32, "w_up"); kb.dma("sp", w_up, ins["b_w_up"])
        a_up = kb.sb(st, [64, 1024], F32, "a_up"); kb.dma("sp", a_up, ins["b_a_up"])
        g_up = kb.sb(st, [128, 1024], F32, "g_up"); kb.dma("sp", g_up, ins["b_g_up"])
        gate_up = kb.sb(st, [16, 512], F32, "gate_up"); kb.dma("sp", gate_up, ins["a_gate_up"])

        pa = kb.sb(st, [64, A_COLS], F32, "pa")
        pb = kb.sb(st, [64, B_COLS], F32, "pb")
        xm = kb.sb(st, [64, B_COLS], F32, "xm")
        omix = kb.sb(st, [64, D], F32, "omix")
        Sg = kb.sb(st, [128, 4, 256], F32, "Sg")
        M = kb.sb(st, [64, 16, 64], F32, "M")
        Mio = kb.sb(st, [64, 16, 64], F32, "Mio")
        W = [kb.sb(st, [64, 512], F32, f"w{i}") for i in range(16)]
        XT = [kb.sb(st, [64, 8, 64], F32, f"xt{i}") for i in range(4)]
        AM = [kb.sb(st, [64, 8, 64], F32, f"am{i}") for i in range(7)]
        small = Rot([kb.sb(st, [128, 256], F32, f"sm{i}") for i in range(10)])
        col = Rot([kb.sb(st, [128, 16], F32, f"col{i}") for i in range(12)])
        psum = Rot([kb.ps(st, [128, 512], F32, f"ps{i}") for i in range(8)])

        def h3(v, C):
            return v.re("p (h k) -> p h k", h=8)

        def mask3(m, C):
            return m[:C, :C].re("p (o c) -> p o c", o=1).bc([C, 8, C])

        for (r0, C, seq_start, sidx) in cfg.chunks:
            last = (r0 + C == cfg.NP) if sidx < 0 else True
            kb.dma("sp", pa[:C, :], P0[r0:r0 + C, 0:A_COLS])
            kb.dma("sp", pb[:C, :], P0[r0:r0 + C, A_COLS:EV_COLS])
            if seq_start:
                if sidx < 0:
                    kb.memset("dve", xm[0:1, :], 0.0)
                    kb.memset("dve", Sg, 0.0)
                    kb.memset("dve", M, 0.0)
                else:
                    kb.dma("sp", xm[0:1, :], ins["state_shift"][sidx:sidx + 1, :])
                    kb.dma("sp", Sg, ins["state_gla"][sidx].rearrange("h k v -> k h v"))
                    kb.dma("sp", Mio, ins["state_rwkv"][sidx].rearrange("h v k -> v h k"))
                    for hh in range(2):
                        pt = psum()
                        for h in range(8):
                            kb.tr(pt[:64, h * 64:(h + 1) * 64], Mio[:, hh * 8 + h, :], ident[:64, :64])
                        kb.cp("dve", M[:, hh * 8:hh * 8 + 8, :], pt[:64, :].re("p (h v) -> p h v", h=8))
                if C > 1:
                    kb.dma("sp", xm[1:C, :], P0[r0:r0 + C - 1, A_COLS:EV_COLS])
            else:
                kb.dma("sp", xm[:C, :], P0[r0 - 1:r0 + C - 1, A_COLS:EV_COLS])

            pt = psum()
            kb.tr(pt[:16, :C], pa[:C, 3072:3088], ident[:C, :C])
            gdT = small()
            kb.cp("dve", gdT[:16, :C], pt[:16, :C])
            pg = psum()
            kb.mm(pg[:C, :512], gdT[:16, :C], gate_up[:16, :], start=True, stop=False)
            kb.mm(pg[:C, :512], ones[0:1, :C], gb_row[0:1, :], start=False, stop=True)
            lgp = W[0]
            kb.act(lgp[:C, :], pg[:C, :512], AF.Exp, scale=-1.0)
            kb.act(lgp[:C, :], lgp[:C, :], AF.Ln, bias=1.0)
            for h in range(4 if os.environ.get('NOGLA') is None else 0):
                lgh = lgp[:C, h * 128:(h + 1) * 128]
                q_tok = pa[:C, h * 128:(h + 1) * 128]
                k_tok = pa[:C, 512 + h * 128:512 + (h + 1) * 128]
                v_tok = pa[:C, 1024 + h * 256:1024 + (h + 1) * 256]
                r_tok = pa[:C, 2048 + h * 256:2048 + (h + 1) * 256]
                pbT = psum()
                kb.mm(pbT[:, :C], lgh, tri_i[:C, :C])
                eqT = small(); ekT = small()
                kb.act(eqT[:, :C], pbT[:, :C], AF.Exp, scale=-1.0 / 16)
                kb.act(ekT[:, :C], pbT[:, :C], AF.Exp, scale=1.0 / 16)
                pq = psum()
                kb.tr(pq[:, :C], q_tok, ident[:C, :C])
                kb.tr(pq[:, 128:128 + C], k_tok, ident[:C, :C])
                qtT = small(); ktT = small()
                kb.stt("dve", qtT[:, :C], pq[:, :C], 128.0 ** -0.5, eqT[:, :C], ALU.mult, ALU.mult)
                kb.tt("dve", ktT[:, :C], pq[:, 128:128 + C], ekT[:, :C], ALU.mult)
                patt = psum()
                kb.mm(patt[:C, :C], ktT[:, :C], qtT[:, :C])
                attm = small()
                kb.tt("dve", attm[:C, :C], patt[:C, :C], tri_i[:C, :C], ALU.mult)
                pdl = psum()
                kb.mm(pdl[:C, :128], tri_g[:C, :C], lgh)
                khat = small()
                kb.act(khat[:C, :128], pdl[:C, :128], AF.Exp, scale=-1.0 / 16)
                kb.tt("dve", khat[:C, :128], khat[:C, :128], k_tok, ALU.mult)
                po = psum()
                kb.mm(po[:C, :256], attm[:C, :C], v_tok, start=True, stop=False)
                kb.mm(po[:C, :256], qtT[:, :C], Sg[:, h, :], start=False, stop=True)
                pS = psum()
                kb.mm(pS[:, :256], khat[:C, :128], v_tok)
                kb.stt("dve", Sg[:, h, :], Sg[:, h, :], eqT[:, C - 1:C], pS[:, :256], ALU.mult, ALU.add)
                sq = small(); ssq = col()
                kb.act(sq[:C, :], po[:C, :256], AF.Square)
                kb.red("dve", ssq[:C, 0:1], sq[:C, :])
                kb.rsqrt(ssq[:C, 0:1], ssq[:C, 0:1], 1.0 / 256, 1e-5)
                og = small(); sr = small()
                kb.stt("dve", og[:C, :], po[:C, :256], ssq[:C, 0:1], ng_bc[:C, h * 256:(h + 1) * 256], ALU.mult, ALU.mult)
                kb.act(sr[:C, :], r_tok, AF.Silu)
                kb.tt("dve", omix[:C, h * 256:(h + 1) * 256], og[:C, :], sr[:C, :], ALU.mult)
            if last:
                dst = outs["gla_p"] if sidx < 0 else outs["gla_s"][sidx]
                kb.dma("sp", dst.rearrange("h k v -> k h v"), Sg)

            kb.tt("dve", xm[:C, :], xm[:C, :], pb[:C, :], ALU.subtract)
            kb.tt("dve", xm[:C, :], xm[:C, :], mu_bc[:C, :], ALU.mult)
            kb.tt("dve", xm[:C, :], xm[:C, :], pb[:C, :], ALU.add)
            twd = small(); sgd = small()
            kb.act(twd[:C, :64], xm[:C, 3072:3136], AF.Tanh)
            kb.act(sgd[:C, :128], xm[:C, 3200:3328], AF.Sigmoid)
            pt = psum()
            kb.tr(pt[:64, 0:C], twd[:C, :64], ident[:C, :C])
            kb.tr(pt[:64, 64:64 + C], xm[:C, 3136:3200], ident[:C, :C])
            kb.tr(pt[:128, 128:128 + C], sgd[:C, :128], ident[:C, :C])
            tT = small()
            kb.cp("dve", tT[:64, 0:128], pt[:64, 0:128])
            kb.cp("dve", tT[:128, 128:128 + C], pt[:128, 128:128 + C])
            twdT = tT[:64, 0:C]; adT = tT[:64, 64:64 + C]; sgdT = tT[:128, 128:128 + C]
            for hh in range(2 if os.environ.get('NORWKV') is None else 0):
                hs = hh * 512
                r = xm[:C, hs:hs + 512]
                k = xm[:C, 1024 + hs:1024 + hs + 512]
                v = xm[:C, 2048 + hs:2048 + hs + 512]
                wl, a, g, kk, kmod, b, Gs, rt, eGn, nbt, kt, kap, oc, tmp1, tmp2, tmp3 = [w[:C, :] for w in W]
                pz = psum()
                kb.mm(pz[:C, :], twdT, w_up[:64, hs:hs + 512], start=True, stop=False)
                kb.mm(pz[:C, :], ones[0:1, :C], w0_row[0:1, hs:hs + 512], start=False, stop=True)
                kb.act(wl, pz[:C, :], AF.Sigmoid)
                pz = psum()
                kb.mm(pz[:C, :], adT, a_up[:64, hs:hs + 512], start=True, stop=False)
                kb.mm(pz[:C, :], ones[0:1, :C], a0_row[0:1, hs:hs + 512], start=False, stop=True)
                kb.act(a, pz[:C, :], AF.Sigmoid)
                pz = psum()
                kb.mm(pz[:C, :], sgdT, g_up[:128, hs:hs + 512])
                kb.cp("act", g, pz[:C, :])
                if RSTOP <= 1:
                    continue
                kb.tt("dve", kk, k, kk_bc[:C, hs:hs + 512], ALU.mult)
                kb.tt("dve", tmp1, kk, kk, ALU.mult)
                ss = col()
                kb.red("dve", ss[:C, 0:8], h3(tmp1, C))
                kb.act(ss[:C, 0:8], ss[:C, 0:8], AF.Sqrt)
                kb.ts("dve", ss[:C, 0:8], ss[:C, 0:8], 1e-12, None, ALU.max)
                kb.recip(ss[:C, 0:8], ss[:C, 0:8])
                kb.tt("dve", h3(kk, C), h3(kk, C), ss[:C, 0:8].re("p (h o) -> p h o", o=1).bc([C, 8, 64]), ALU.mult)
                kb.stt("dve", tmp1, a, -1.0, ka_bc[:C, hs:hs + 512], ALU.add, ALU.mult)
                kb.stt("dve", kmod, tmp1, 1.0, k, ALU.add, ALU.mult)
                kb.tt("dve", b, kk, a, ALU.mult)
                if RSTOP <= 2:
                    continue
                pG = psum()
                kb.mm(pG[:C, :], tri_i[:C, :C], wl)
                kb.cp("act", Gs, pG[:C, :])
                kb.act(rt, Gs, AF.Exp, scale=-C0)
                kb.act(eGn, Gs, AF.Exp, scale=C0)
                kb.tt("dve", tmp1, Gs, wl, ALU.subtract)
                kb.act(kap, tmp1, AF.Exp, scale=-C0)
                kb.tt("dve", kap, kap, kk, ALU.mult)
                kb.tt("dve", rt, rt, r, ALU.mult)
                kb.tt("dve", b, b, eGn, ALU.mult)
                kb.ts("dve", nbt, b, -1.0, None, ALU.mult)
                kb.tt("dve", kt, kmod, eGn, ALU.mult)
                if RSTOP <= 3:
                    continue
                for i, src in enumerate((kap, b, kt, rt)):
                    pt = psum()
                    for h in range(8):
                        kb.tr(pt[:64, h * 64:h * 64 + C], src[:, h * 64:(h + 1) * 64], ident[:C, :C])
                    kb.cp("act" if i % 2 == 0 else "dve", XT[i][:, :, :C],
                          pt[:64, :].re("p (h c) -> p h c", h=8)[:, :, :C])
                if RSTOP <= 4:
                    continue
                kapT, btT, ktT, rtT = XT
                AT, A, A2T, A3T, A4T, Q0, Q1 = AM

                def amat(dst, lT, rT, mask, neg=False):
                    pp = psum()
                    for h in range(8):
                        kb.mm(pp[:C, h * 64:h * 64 + C], lT[:, h, :C], rT[:, h, :C])
                    src = pp[:C, :].re("p (h c) -> p h c", h=8)[:, :, :C]
                    if neg:
                        kb.stt("dve", dst[:C, :, :C], src, -1.0, mask3(mask, C), ALU.mult, ALU.mult)
                    else:
                        kb.tt("dve", dst[:C, :, :C], src, mask3(mask, C), ALU.mult)

                amat(AT, btT, kapT, tri_s)
                amat(A, kapT, btT, tri_g)
                amat(A2T, ktT, kapT, tri_s)
                amat(A3T, ktT, rtT, tri_i)
                amat(A4T, btT, rtT, tri_i, neg=True)
                if RSTOP <= 5:
                    continue
                pW = psum()
                for h in range(8):
                    kb.mm(pW[:C, h * 64:(h + 1) * 64], kapT[:, h, :C], M[:, hh * 8 + h, :], start=True, stop=False)
                    kb.mm(pW[:C, h * 64:(h + 1) * 64], A2T[:C, h, :C], v[:, h * 64:(h + 1) * 64], start=False, stop=True)
                U = tmp2
                kb.cp("act", U, pW[:C, :])
                if RSTOP <= 6:
                    continue
                curP, curPT = A, AT
                nxt = [(Q0, Q1), (A2T, A), (Q0, Q1), (A2T, A), (Q0, Q1)]
                n = 0
                while True:
                    pU = psum()
                    for h in range(8):
                        kb.mm(pU[:C, h * 64:(h + 1) * 64], curPT[:C, h, :C], U[:, h * 64:(h + 1) * 64])
                    kb.tt("dve", U, U, pU[:C, :], ALU.subtract if n == 0 else ALU.add)
                    if (2 << n) >= C:
                        break
                    nP, nPT = nxt[n]
                    if n == 1:
                        nP, nPT = A2T, AT
                    p1 = psum(); p2 = psum()
                    for h in range(8):
                        kb.mm(p1[:C, h * 64:h * 64 + C], curPT[:C, h, :C], curP[:C, h, :C])
                        kb.mm(p2[:C, h * 64:h * 64 + C], curP[:C, h, :C], curPT[:C, h, :C])
                    tgt = [t for t in (Q0, Q1, A2T, A, AT) if t is not curP and t is not curPT][:2]
                    nP, nPT = tgt
                    kb.cp("act", nP[:C, :, :C], p1[:C, :].re("p (h c) -> p h c", h=8)[:, :, :C])
                    kb.cp("dve", nPT[:C, :, :C], p2[:C, :].re("p (h c) -> p h c", h=8)[:, :, :C])
                    curP, curPT = nP, nPT
                    n += 1
                if RSTOP <= 7:
                    continue
                pO = psum()
                for h in range(8):
                    sl = slice(h * 64, (h + 1) * 64)
                    kb.mm(pO[:C, sl], rtT[:, h, :C], M[:, hh * 8 + h, :], start=True, stop=False)
                    kb.mm(pO[:C, sl], A3T[:C, h, :C], v[:, sl], start=False, stop=False)
                    kb.mm(pO[:C, sl], A4T[:C, h, :C], U[:, sl], start=False, stop=True)
                if RSTOP <= 8:
                    continue
                pM = psum()
                for h in range(8):
                    sl = slice(h * 64, (h + 1) * 64)
                    kb.mm(pM[:64, sl], kt[:, sl], v[:, sl], start=True, stop=False)
                    kb.mm(pM[:64, sl], nbt[:, sl], U[:, sl], start=False, stop=True)
                pGm = psum()
                for h in range(8):
                    kb.mm(pGm[:64, 64 * h:64 * h + 64], wl[:, h * 64:(h + 1) * 64], ones[:C, 0:64])
                gam = col()
                kb.act(gam[:64, 0:8], pGm[:64, 0:512].re('p (h t) -> p h t', t=64)[:, :, 0], AF.Exp, scale=-C0)
                Mh = M[:, hh * 8:hh * 8 + 8, :]
                kb.tt("dve", Mh, Mh, pM[:64, :].re("p (h v) -> p h v", h=8), ALU.add)
                kb.tt("dve", Mh, Mh, gam[:64, 0:8].re("p (h o) -> p h o", o=1).bc([64, 8, 64]), ALU.mult)
                if RSTOP <= 9:
                    continue
                kb.cp("act", oc, pO[:C, :])
                mean = col()
                kb.red("dve", mean[:C, 0:8], h3(oc, C))
                kb.ts("dve", mean[:C, 0:8], mean[:C, 0:8], -1.0 / 64, None, ALU.mult)
                kb.tt("dve", h3(oc, C), h3(oc, C), mean[:C, 0:8].re("p (h o) -> p h o", o=1).bc([C, 8, 64]), ALU.add)
                if RSTOP <= 10:
                    continue
                kb.tt("dve", tmp1, oc, oc, ALU.mult)
                var = col()
                kb.red("dve", var[:C, 0:8], h3(tmp1, C))
                kb.rsqrt(var[:C, 0:8], var[:C, 0:8], 1.0 / 64, 64e-5)
                kb.tt("dve", h3(oc, C), h3(oc, C), var[:C, 0:8].re("p (h o) -> p h o", o=1).bc([C, 8, 64]), ALU.mult)
                if RSTOP <= 11:
                    continue
                kb.tt("dve", oc, oc, lng_bc[:C, hs:hs + 512], ALU.mult)
                kb.tt("dve", oc, oc, lnb_bc[:C, hs:hs + 512], ALU.add)
                if RSTOP <= 12:
                    continue
                kb.tt("dve", tmp1, r, kmod, ALU.mult)
                kb.tt("dve", tmp1, tmp1, rk_bc[:C, hs:hs + 512], ALU.mult)
                if RSTOP <= 13:
                    continue
                bon = col()
                kb.red("dve", bon[:C, 0:8], h3(tmp1, C))
                kb.tt("dve", h3(tmp1, C), h3(v, C), bon[:C, 0:8].re("p (h o) -> p h o", o=1).bc([C, 8, 64]), ALU.mult)
                if RSTOP <= 14:
                    continue
                kb.tt("dve", oc, oc, tmp1, ALU.add)
                if RSTOP <= 15:
                    continue
                kb.tt("dve", omix[:C, 1024 + hs:1024 + hs + 512], oc, g, ALU.mult)
            kb.dma("sp", OM[r0:r0 + C, :], omix[:C, :])
            if last:
                for hh in range(2):
                    pt = psum()
                    for h in range(8):
                        kb.tr(pt[:64, h * 64:(h + 1) * 64], M[:, hh * 8 + h, :], ident[:64, :64])
                    kb.cp("dve", Mio[:, hh * 8:hh * 8 + 8, :], pt[:64, :].re("p (h v) -> p h v", h=8))
                dst = outs["rwkv_p"] if sidx < 0 else outs["rwkv_s"][sidx]
                kb.dma("sp", dst.rearrange("h v k -> v h k"), Mio)
                lastrow = r0 + C - 1
                dsts = outs["shift_p"] if sidx < 0 else outs["shift_s"][sidx]
                kb.dma("sp", dsts.rearrange("(o n) -> o n", o=1), P0[lastrow:lastrow + 1, A_COLS:EV_COLS])
        kb.P.flush()


def ln_and_route(kb, ctx, cfg, ins, n, r0, v, layer, which, X1, X1T, GT, psum):
    col, c, ident, g_bc, b_bc = ctx["col"], ctx["c"], ctx["ident"], ctx["g_bc"], ctx["b_bc"]
    s1 = col()
    kb.red("dve", s1[:n, 0:1], v[:n, :])
    kb.ts("dve", s1[:n, 0:1], s1[:n, 0:1], -1.0 / D, None, ALU.mult)
    kb.ts("dve", v[:n, :], v[:n, :], s1[:n, 0:1], None, ALU.add)
    kb.act(c[:n, :], v[:n, :], AF.Square)
    s2 = col()
    kb.red("dve", s2[:n, 0:1], c[:n, :])
    kb.rsqrt(s2[:n, 0:1], s2[:n, 0:1], 1.0 / D, LN_EPS)
    kb.stt("dve", v[:n, :], v[:n, :], s2[:n, 0:1], g_bc[:n, :], ALU.mult, ALU.mult)
    kb.tt("dve", v[:n, :], v[:n, :], b_bc[:n, :], ALU.add)
    kb.dma("sp", X1[r0:r0 + n, :], v[:n, :])
    if X1T is None or os.environ.get('NOROUTE'):
        return
    x1T, x1Tb, wr = ctx["x1T"], ctx["x1Tb"], ctx.get("wr")
    for grp in range(4):
        pt = psum()
        for j in range(4):
            kc = grp * 4 + j
            kb.tr(pt[:, j * 128:j * 128 + n], v[:n, kc * 128:(kc + 1) * 128], ident[:n, :n])
        src = pt.re("p (j t) -> p j t", j=4)[:, :, :n]
        kb.cp("act", x1T[:, grp * 4:grp * 4 + 4, :n], src)
        kb.cp("dve", x1Tb[:, grp * 4:grp * 4 + 4, :n], x1T[:, grp * 4:grp * 4 + 4, :n])
    if not os.environ.get('NOX1T'):
        kb.dma("sp", X1T[:, :, r0:r0 + n].rearrange("kc p t -> p kc t"), x1Tb[:, :, :n])
    if GT is None:
        return
    pl = psum()
    for kc in range(16):
        kb.mm(pl[:n, 0:128], x1T[:, kc, :n], wr[:, kc, :], start=(kc == 0), stop=(kc == 15))
    if os.environ.get('NOGATE'):
        return
    sm = ctx["sm"]
    l = sm(); e = sm(); t3 = sm(); t4 = sm(); gate = sm()
    BIG = 1e30
    kb.cp("dve", l[:n, 0:16], pl[:n, 0:16])
    mx = col()
    kb.red("dve", mx[:n, 0:1], l[:n, 0:16], op=ALU.max)
    kb.ts("dve", mx[:n, 0:1], mx[:n, 0:1], -1.0, None, ALU.mult)
    kb.act(e[:n, 0:16], l[:n, 0:16], AF.Exp, bias=mx[:n, 0:1], scale=1.0)
    e3 = e[:n, 0:16].re("p (g j) -> p g j", g=4)
    m1 = col(); m2 = col(); sc = col(); oh = col()
    kb.red("dve", m1[:n, 0:4], e3, op=ALU.max)
    t33 = t3[:n, 0:16].re("p (g j) -> p g j", g=4)
    kb.tt("dve", t33, e3, m1[:n, 0:4].re("p (g o) -> p g o", o=1).bc([n, 4, 4]), ALU.is_equal)
    kb.stt("dve", t33, t33, -BIG, e3, ALU.mult, ALU.add)
    kb.red("dve", m2[:n, 0:4], t33, op=ALU.max)
    kb.tt("dve", sc[:n, 0:4], m1[:n, 0:4], m2[:n, 0:4], ALU.add)
    gm = col()
    kb.red("dve", gm[:n, 0:1], sc[:n, 0:4], op=ALU.max)
    kb.ts("dve", oh[:n, 0:4], sc[:n, 0:4], gm[:n, 0:1], None, ALU.is_equal)
    kb.tt("dve", t33, e3, oh[:n, 0:4].re("p (g o) -> p g o", o=1).bc([n, 4, 4]), ALU.mult)
    ing = col()
    kb.red("dve", ing[:n, 0:4], t3[:n, 0:16].re("p (g j) -> p j g", g=4))
    v1 = col(); v2 = col(); q1 = col(); q2 = col(); i2 = col()
    kb.red("dve", v1[:n, 0:1], ing[:n, 0:4], op=ALU.max)
    kb.ts("dve", q1[:n, 0:4], ing[:n, 0:4], v1[:n, 0:1], None, ALU.is_equal)
    kb.stt("dve", i2[:n, 0:4], q1[:n, 0:4], -BIG, ing[:n, 0:4], ALU.mult, ALU.add)
    kb.red("dve", v2[:n, 0:1], i2[:n, 0:4], op=ALU.max)
    kb.ts("dve", q2[:n, 0:4], i2[:n, 0:4], v2[:n, 0:1], None, ALU.is_equal)
    kb.tt("dve", q1[:n, 0:4], q1[:n, 0:4], q2[:n, 0:4], ALU.add)
    kb.tt("dve", v1[:n, 0:1], v1[:n, 0:1], v2[:n, 0:1], ALU.add)
    kb.recip(v1[:n, 0:1], v1[:n, 0:1])
    kb.tt("dve", q1[:n, 0:4], q1[:n, 0:4], ing[:n, 0:4], ALU.mult)
    kb.ts("dve", q1[:n, 0:4], q1[:n, 0:4], v1[:n, 0:1], None, ALU.mult)
    kb.tt("dve", gate[:n, 0:16].re("p (g j) -> p g j", g=4),
          oh[:n, 0:4].re("p (g o) -> p g o", o=1).bc([n, 4, 4]),
          q1[:n, 0:4].re("p (o j) -> p o j", o=1).bc([n, 4, 4]), ALU.mult)
    pg = psum()
    kb.tr(pg[:16, :n], gate[:n, 0:16], ident[:n, :n])
    gT = sm()
    kb.cp("dve", gT[:16, :n], pg[:16, :n])
    kb.dma("sp", GT[:, r0:r0 + n], gT[:16, :n])


def ln_ctx(kb, ctx, ins, gname, bname, layer, route=True, trans=False):
    st = ctx["st"]
    ctx["ident"] = ctx["consts"][:, C_IDENT:C_IDENT + 128]
    ctx["g_bc"] = kb.sb(st, [128, D], F32, "g_bc")
    kb.dma("sp", ctx["g_bc"], row_bcast(ins[gname][layer], 128))
    ctx["b_bc"] = kb.sb(st, [128, D], F32, "b_bc")
    kb.dma("sp", ctx["b_bc"], row_bcast(ins[bname][layer], 128))
    ctx["c"] = kb.sb(st, [128, D], F32, "c")
    ctx["col"] = Rot([kb.sb(st, [128, 4], F32, f"col{i}") for i in range(24)])
    if route or trans:
        ctx["x1T"] = kb.sb(st, [128, 16, 128], F32, "x1T")
        ctx["x1Tb"] = kb.sb(st, [128, 16, 128], BF16, "x1Tb")
    if route:
        ctx["wr"] = kb.sb(st, [128, 16, 128], F32, "wr")
        kb.memset("dve", ctx["wr"], 0.0)
        kb.dma("sp", ctx["wr"][:, :, 0:16], ins["w_router"].rearrange("(kc p) e -> p kc e", p=128))
        ctx["sm"] = Rot([kb.sb(st, [128, 128], F32, f"sm{i}") for i in range(8)])


def phase_outln(kb, cfg, ins, src, W, xres, layer, X1, X1T, GT, srcT=None):
    def pre(ctx):
        ln_ctx(kb, ctx, ins, "ln_mix_g", "ln_mix_b", layer)
        ctx["xr"] = [kb.sb(ctx["st"], [128, D], F32, "xr") for _ in range(2)]
        ctx["v"] = kb.sb(ctx["st"], [128, D], F32, "v")
        ctx["ps2"] = Rot([kb.ps(ctx["st"], [128, 512], F32, f"ps2{i}") for i in range(2)])

    def epi(ctx, ts, r0, n, cb, cw, pa):
        xr = ctx["xr"][ts % 2]
        if cb == 0:
            kb.dma("sp", xr[:n, :], xres[r0:r0 + n, :])
        kb.stt("dve", ctx["v"][:n, cb * 512:cb * 512 + cw], xr[:n, cb * 512:cb * 512 + cw], ALPHA, pa[:n, :cw], ALU.mult, ALU.add)

    def post(ctx, ts, r0, n):
        ln_and_route(kb, ctx, cfg, ins, n, r0, ctx["v"], layer, "mix", X1, X1T, GT, ctx["ps2"])

    dense_phase(kb, cfg, ins["consts"], src, W, D, epi, post=post, pre=pre, srcT=srcT)


def inherit(dsts, srcs):
    w = [op for b in srcs for op in b.w]
    r = [op for b in srcs for op in b.r]
    for d in dsts:
        d.w = list(w)
        d.r = list(r)


def phase_moe(kb, cfg, ins, layer, X1, X1T, GT, OUT, OUTT=None):
    NT = cfg.NT
    w_up, w_dn = ins["moe_w_up"][layer], ins["moe_w_down"][layer]
    ntile = (NT + 1095) // 1096
    base = ((NT + ntile - 1) // ntile + 7) // 8 * 8
    tiles = []
    t0 = 0
    while t0 < NT:
        tm = min(base, NT - t0)
        tiles.append((t0, tm))
        t0 += tm
    TMAX = max(tm for _, tm in tiles)
    NSUB = (TMAX + 127) // 128
    with contextlib.ExitStack() as st:
        xT = kb.sb(st, [128, 16, TMAX], BF16, "xT")
        yacc = kb.sb(st, [128, NSUB, D], F32, "yacc")
        gb = kb.sb(st, [128, TMAX], F32, "gbc")
        hhT = kb.sb(st, [128, 8, TMAX], BF16, "hhT")
        arWd = kb.sb(st, [128, 8192], F32, "arWd")
        arW = kb.sb(st, [128, 8192], F32, "arW")
        sA = Rot([kb.sb(st, [128, 512], F32, f"sA{i}") for i in range(2)])
        sB = Rot([kb.sb(st, [128, 512], F32, f"sB{i}") for i in range(2)])
        ident = kb.sb(st, [128, 128], F32, "ident")
        kb.dma("sp", ident, ins["consts"][:, 0:128])
        col = Rot([kb.sb(st, [128, 4], F32, f"col{i}") for i in range(24)])
        psum = Rot([kb.ps(st, [128, 512], F32, f"ps{i}") for i in range(8)])
        Wd = V(arWd.ap.bitcast(BF16).rearrange("p (f n) -> p f n", f=8), Buf("Wd"))
        Wv = [V(arW.ap[:, i * 2048:(i + 1) * 2048].bitcast(BF16).rearrange("p (k n) -> p k n", k=16), Buf(f"W{i}")) for i in range(4)]
        W1 = [Wv[0], Wv[2]]
        W2 = [Wv[1], Wv[3]]
        g_bc = V(arWd.ap[:, 0:2048], Buf("g_bc"))
        b_bc = V(arWd.ap[:, 2048:4096], Buf("b_bc"))
        cc = V(arWd.ap[:, 4096:6144], Buf("c"))
        xr = V(arWd.ap[:, 6144:8192], Buf("xr"))
        x1T = V(arW.ap[:, 0:2048].rearrange("p (k n) -> p k n", k=16), Buf("x1T"))
        x1Tb = V(arW.ap[:, 2048:3072].bitcast(BF16).rearrange("p (k n) -> p k n", k=16), Buf("x1Tb"))
        ctx = {"ident": ident, "g_bc": g_bc, "b_bc": b_bc, "c": cc, "col": col, "x1T": x1T, "x1Tb": x1Tb}
        for (t0, tm) in tiles:
            nsub = (tm + 127) // 128
            cblocks = [(c0, min(512, tm - c0)) for c0 in range(0, tm, 512)]
            inherit([Wd.buf], [g_bc.buf, b_bc.buf, cc.buf, xr.buf])
            inherit([w.buf for w in Wv], [x1T.buf, x1Tb.buf])
            kb.dma("sp", xT[:, :, :tm], X1T[:, :, t0:t0 + tm].rearrange("kc p t -> p kc t"))
            iw = 0
            for e in range(16):
                kb.dma("sp", gb[:, :tm], GT[e:e + 1, t0:t0 + tm].to_broadcast([128, tm]))
                wup = w_up[e].rearrange("(kc p) n -> p kc n", p=128)
                kb.dma("pool", Wd, w_dn[e].rearrange("(fc p) n -> p fc n", p=128))
                for fb in range(4):
                    w1, w2 = W1[iw % 2], W2[iw % 2]
                    iw += 1
                    kb.dma("pool", w1, wup[:, :, fb * 256:(fb + 1) * 256])
                    kb.dma("pool", w2, wup[:, :, 1024 + fb * 256:1024 + (fb + 1) * 256])
                    for f2 in range(2):
                        fc = fb * 2 + f2
                        for (c0, cw) in cblocks:
                            pA = psum(); pB = psum()
                            for kc in range(16):
                                kb.mm(pA[:, :cw], w1[:, kc, f2 * 128:(f2 + 1) * 128], xT[:, kc, c0:c0 + cw], start=(kc == 0), stop=(kc == 15))
                            for kc in range(16):
                                kb.mm(pB[:, :cw], w2[:, kc, f2 * 128:(f2 + 1) * 128], xT[:, kc, c0:c0 + cw], start=(kc == 0), stop=(kc == 15))
                            a = sA(); b = sB()
                            kb.act(a[:, :cw], pA[:, :cw], AF.Silu)
                            kb.tt("dve", b[:, :cw], pB[:, :cw], gb[:, c0:c0 + cw], ALU.mult)
                            kb.tt("dve", hhT[:, fc, c0:c0 + cw], a[:, :cw], b[:, :cw], ALU.mult)
                for su in range(nsub):
                    n = min(128, tm - su * 128)
                    for db in range(4):
                        py = psum()
                        for fc in range(8):
                            kb.mm(py[:n, :], hhT[:, fc, su * 128:su * 128 + n], Wd[:, fc, db * 512:(db + 1) * 512],
                                  start=(fc == 0), stop=(fc == 7))
                        ya = yacc[:n, su, db * 512:(db + 1) * 512]
                        if e == 0:
                            kb.cp("act", ya, py[:n, :])
                        else:
                            kb.tt("dve", ya, ya, py[:n, :], ALU.add)
            inherit([g_bc.buf, b_bc.buf, cc.buf, xr.buf], [Wd.buf])
            inherit([x1T.buf, x1Tb.buf], [w.buf for w in Wv])
            kb.dma("sp", g_bc, row_bcast(ins["ln_ffn_g"][layer], 128))
            kb.dma("sp", b_bc, row_bcast(ins["ln_ffn_b"][layer], 128))
            for su in range(nsub):
                n = min(128, tm - su * 128)
                r0 = t0 + su * 128
                kb.dma("sp", xr[:n, :], X1[r0:r0 + n, :])
                kb.stt("dve", yacc[:n, su, :], xr[:n, :], ALPHA, yacc[:n, su, :], ALU.mult, ALU.add)
                ln_and_route(kb, ctx, cfg, ins, n, r0, yacc[:, su, :], layer, "ffn", OUT, OUTT, None, psum)
        kb.P.flush()


_CACHE = {}


def kernel(**inputs):
    cfg = Cfg()
    if "nc" not in _CACHE:
        _CACHE["nc"] = build(cfg)
    nc, ins, outs = _CACHE["nc"]
    f = lambda k: np.ascontiguousarray(np.asarray(inputs[k], dtype=np.float32))
    consts = make_consts()
    shared = {"consts": consts}
    for k in WEIGHT_SHAPES:
        a = f(k)
        if k.startswith("moe_") or k.startswith("ln_") or k == "w_router":
            shared[k] = a
        else:
            shared[k] = np.ascontiguousarray(a[0])
    xp, xs, meta = f("x_prompt"), f("x_sample"), f("meta")
    in_maps = []
    for c in range(8):
        b = c % 4
        s0 = 16 * c
        m = dict(shared)
        m["xtok"] = np.ascontiguousarray(np.concatenate([meta, xp[b], xs[s0:s0 + 16].reshape(128, D)], 0))
        m["state_gla"] = np.ascontiguousarray(f("state_gla")[0, s0:s0 + 16])
        m["state_rwkv"] = np.ascontiguousarray(f("state_rwkv")[0, s0:s0 + 16])
        m["state_shift"] = np.ascontiguousarray(f("state_shift")[0, s0:s0 + 16])
        m["state_s5_re"] = np.ascontiguousarray(f("state_s5_re")[0, s0:s0 + 16])
        m["state_s5_im"] = np.ascontiguousarray(f("state_s5_im")[0, s0:s0 + 16])
        in_maps.append({k: m[k] for k in ins})
    res = run_bass_kernel_spmd(nc, in_maps, core_ids=list(range(8)))
    R = res.results
    NP = cfg.NP
    y_p = np.stack([R[b]["y"][16:NP] for b in range(4)], 0)
    y_s = np.concatenate([R[c]["y"][NP:].reshape(16, 8, D) for c in range(8)], 0)

    def pr(name):
        return np.stack([R[b][name] for b in range(4)], 0)[None]

    def sm(name):
        return np.concatenate([R[c][name] for c in range(8)], 0)[None]

    return (y_p.astype(np.float32), y_s.astype(np.float32),
            pr("gla_p"), sm("gla_s"), pr("rwkv_p"), sm("rwkv_s"), pr("shift_p"), sm("shift_s"),
            pr("s5re_p"), sm("s5re_s"), pr("s5im_p"), sm("s5im_s"))


def dense_T(kb, cfg, ins, srcT, W, epi, pre=None, TB=512):
    NT = cfg.NT
    with contextlib.ExitStack() as st:
        Wb = kb.sb(st, [128, 16, D], BF16, "Wb")
        Wv = W.rearrange("(kc p) n -> p kc n", p=128)
        for g in range(4):
            kb.dma("pool", Wb[:, 4 * g:4 * g + 4, :], Wv[:, 4 * g:4 * g + 4, :])
        xT = [kb.sb(st, [128, 16, TB], BF16, "xT") for _ in range(2)]
        psum = Rot([kb.ps(st, [128, 512], F32, f"ps{i}") for i in range(8)])
        ctx = {"st": st}
        if pre is not None:
            pre(ctx)
        t0 = 0
        i = 0
        while t0 < NT:
            tb = min(TB, NT - t0)
            x = xT[i % 2]
            kb.dma("sp", x[:, :, :tb], srcT[:, :, t0:t0 + tb].rearrange("kc p t -> p kc t"))
            for cc in range(16):
                pa = psum()
                for kc in range(16):
                    kb.mm(pa[:, :tb], Wb[:, kc, cc * 128:(cc + 1) * 128], x[:, kc, :tb], start=(kc == 0), stop=(kc == 15))
                epi(ctx, cc, t0, tb, pa, x)
            t0 += tb
            i += 1
        kb.P.flush()


def phase_s5(kb, cfg, ins, outs, UT, ZT):
    NT, NP = cfg.NT, cfg.NP
    LMAX = 96
    with contextlib.ExitStack() as st:
        cs = kb.sb(st, [128, C_TOTAL], F32, "consts")
        kb.dma("sp", cs, ins["consts"])
        ident = cs[:, C_IDENT:C_IDENT + 128]
        pm = cs[:, C_PM:C_PM + 8]
        psum = Rot([kb.ps(st, [128, 512], F32, f"ps{i}") for i in range(8)])

        def pj(name):
            t = kb.sb(st, [128, 64], F32, name)
            kb.dma("sp", t, ins[name].rearrange("(j gl) p -> (gl p) j", gl=2), allow_slow_non_contiguous=True)
            return t

        are, aim = pj("c_a_re"), pj("c_a_im")
        dt = kb.sb(st, [128, 64], F32, "dt")
        ld = ins["c_log_dt"].rearrange("(j gl) -> gl j", gl=2)
        for gl in range(2):
            kb.dma("sp", dt[gl * 64:(gl + 1) * 64, :], ld[gl:gl + 1, :].to_broadcast([64, 64]), allow_slow_non_contiguous=True)
        kb.act(dt, dt, AF.Exp)
        T = [kb.sb(st, [128, 64], F32, f"pt{i}") for i in range(10)]
        er, sn, cn, Are, Aim, x_, den, cre, cim, tmp = T
        kb.tt("dve", tmp, are, dt, ALU.mult)
        kb.act(er, tmp, AF.Exp)
        kb.tt("dve", tmp, aim, dt, ALU.mult)
        kb.act(sn, tmp, AF.Sin, scale=1.0 / 16)
        kb.ts("dve", tmp, tmp, 1.0 / 16, math.pi / 2, ALU.mult, ALU.add)
        kb.act(cn, tmp, AF.Sin)
        for _ in range(4):
            kb.tt("dve", tmp, sn, cn, ALU.mult)
            kb.tt("dve", cn, cn, cn, ALU.mult)
            kb.tt("dve", sn, sn, sn, ALU.mult)
            kb.tt("dve", cn, cn, sn, ALU.subtract)
            kb.ts("dve", sn, tmp, 2.0, None, ALU.mult)
        kb.tt("dve", Are, er, cn, ALU.mult)
        kb.tt("dve", Aim, er, sn, ALU.mult)
        kb.ts("dve", x_, Are, -1.0, None, ALU.add)
        kb.tt("dve", den, are, are, ALU.mult)
        kb.tt("dve", tmp, aim, aim, ALU.mult)
        kb.tt("dve", den, den, tmp, ALU.add)
        kb.recip(den, den)
        kb.tt("dve", cre, x_, are, ALU.mult)
        kb.tt("dve", tmp, Aim, aim, ALU.mult)
        kb.tt("dve", cre, cre, tmp, ALU.add)
        kb.tt("dve", cre, cre, den, ALU.mult)
        kb.tt("dve", cim, Aim, are, ALU.mult)
        kb.tt("dve", tmp, x_, aim, ALU.mult)
        kb.tt("dve", cim, cim, tmp, ALU.subtract)
        kb.tt("dve", cim, cim, den, ALU.mult)
        LB = [kb.sb(st, [128, 16, 128], F32, f"LB{i}") for i in range(2)]
        LC = [kb.sb(st, [128, 16, 128], F32, f"LC{i}") for i in range(2)]
        LB3 = [kb.sb(st, [128, 16, 128], F32, f"LB3{i}") for i in range(2)]
        with contextlib.ExitStack() as st2:
            Bre = kb.sb(st2, [128, 64, 16], F32, "Bre")
            Bim = kb.sb(st2, [128, 64, 16], F32, "Bim")
            kb.dma("sp", Bre, ins["c_b_re"].rearrange("(j gl) p c -> (gl p) j c", gl=2))
            kb.dma("sp", Bim, ins["c_b_im"].rearrange("(j gl) p c -> (gl p) j c", gl=2))
            Xr = kb.sb(st2, [128, 64, 16], F32, "Xr")
            Xi = kb.sb(st2, [128, 64, 16], F32, "Xi")
            t1 = kb.sb(st2, [128, 64, 16], F32, "t1")
            X4 = kb.sb(st2, [128, 64, 2, 16], F32, "X4")
            bc = lambda v: v.re("p (j o) -> p j o", o=1).bc([128, 64, 16])
            kb.tt("dve", Xr, Bre, bc(cre), ALU.mult)
            kb.tt("dve", t1, Bim, bc(cim), ALU.mult)
            kb.tt("dve", Xr, Xr, t1, ALU.subtract)
            kb.tt("dve", Xi, Bim, bc(cre), ALU.mult)
            kb.tt("dve", t1, Bre, bc(cim), ALU.mult)
            kb.tt("dve", Xi, Xi, t1, ALU.add)
            for i, X in enumerate((Xr, Xi)):
                for gl in range(2):
                    kb.ts("dve", X4[:, :, gl, :], X, pm[:, gl:gl + 1], None, ALU.mult)
                for m in range(16):
                    pt = psum()
                    kb.tr(pt[:, 0:128], X4[:, 4 * m:4 * m + 4, :, :].re("p a b c -> p (a b c)"), ident)
                    kb.cp("act" if m % 2 == 0 else "dve", LB[i][:, m, :], pt[:, 0:128])
            for i in range(2):
                kb.ts("dve", LB3[i][64:128, :, :], LB[i][64:128, :, :], pm[64:128, 7:8], None, ALU.mult)
            Rr = kb.sb(st2, [128, 16, 64], F32, "Rr")
            R4 = kb.sb(st2, [128, 16, 2, 64], F32, "R4")
            for i, name in enumerate(("c_c_re", "c_c_im")):
                kb.dma("sp", Rr, ins[name].rearrange("(m qg) c p -> (qg c) m p", m=16))
                for gl in range(2):
                    kb.ts("dve", R4[:, :, gl, :], Rr, pm[:, 2 + gl:3 + gl], (1.0 if i == 0 else -1.0), ALU.mult, ALU.mult)
                for m in range(16):
                    pt = psum()
                    kb.tr(pt[:, 0:128], R4[:, m, :, :].re("p a b -> p (a b)"), ident)
                    kb.cp("act" if m % 2 == 0 else "dve", LC[i][:, m, :], pt[:, 0:128])
            kb.P.flush()
        S5STOP = int(os.environ.get("L1STOP", "99"))
        if S5STOP <= 2:
            return
        Dcol = kb.sb(st, [128, 16], F32, "Dcol")
        kb.dma("sp", Dcol, ins["c_d"].rearrange("(m p) -> p m", p=128), allow_slow_non_contiguous=True)
        HS = [kb.sb(st, [128, 64, LMAX], F32, f"HS{i}") for i in range(2)]
        BU = [kb.sb(st, [128, 64, LMAX], F32, f"BU{i}") for i in range(2)]
        carry = [kb.sb(st, [128, 64], F32, f"carry{i}") for i in range(2)]
        init = [kb.sb(st, [128, 64], F32, f"init{i}") for i in range(2)]
        sio = [kb.sb(st, [64, 128], F32, f"sio{i}") for i in range(2)]
        sT = [kb.sb(st, [128, 64], F32, f"st{i}") for i in range(4)]
        uT = [kb.sb(st, [128, 16, LMAX], F32, f"uT{i}") for i in range(2)]
        yT = kb.sb(st, [128, LMAX], F32, "yT")
        yq = kb.sb(st, [128, LMAX], F32, "yq")
        zb = Rot([kb.sb(st, [128, LMAX], BF16, f"zb{i}") for i in range(2)])
        starts = {0: -1}
        ends = {NP - 1: -1}
        for sidx in range(cfg.n_samp):
            starts[NP + 8 * sidx] = sidx
            ends[NP + 8 * sidx + 7] = sidx
        t0 = 0
        bi = 0
        while t0 < NT:
            rem = NT - t0
            L = 64 if (rem == 64 or rem - 64 >= 32) else rem
            assert L <= LMAX
            u = uT[bi % 2]
            kb.dma("sp", u[:, :, :L], UT[:, :, t0:t0 + L].rearrange("kc p t -> p kc t"))
            ppb = min(8, 512 // L)
            ev = 0
            for i in range(2):
                BUv = BU[i].re("p (m q) t -> p q m t", q=4)
                for q in range(4):
                    m0 = 0
                    while m0 < 16:
                        nm = min(ppb, 16 - m0)
                        pp = psum()
                        for m in range(m0, m0 + nm):
                            osl = pp[:, (m - m0) * L:(m - m0 + 1) * L]
                            if q < 3:
                                kb.mm(osl, LB[i][32 * q:32 * q + 32, m, :], u[32 * q:32 * q + 32, m, :L])
                            else:
                                kb.mm(osl, LB3[i][64:128, m, :], u[64:128, m, :L])
                        kb.cp("act" if ev % 2 == 0 else "dve", BUv[:, q, m0:m0 + nm, :L],
                              pp[:, 0:nm * L].re("p (a t) -> p a t", t=L))
                        ev += 1
                        m0 += nm
            if S5STOP <= 3:
                t0 += L
                bi += 1
                continue
            for tl in range(L):
                t = t0 + tl
                if t in starts:
                    sidx = starts[t]
                    if sidx < 0:
                        kb.memset("dve", init[0], 0.0)
                        kb.memset("dve", init[1], 0.0)
                    else:
                        for i, nm in enumerate(("state_s5_re", "state_s5_im")):
                            kb.dma("sp", sio[i], ins[nm][sidx].rearrange("(j gl) p -> j (gl p)", gl=2))
                            pt = psum()
                            kb.tr(pt[:, 0:64], sio[i], ident[:64, :64])
                            kb.cp("dve", init[i], pt[:, 0:64])
                    pr, pi = init[0], init[1]
                elif tl == 0:
                    pr, pi = carry[0], carry[1]
                else:
                    pr, pi = HS[0][:, :, tl - 1], HS[1][:, :, tl - 1]
                kb.tt("dve", sT[0], Are, pr, ALU.mult)
                kb.tt("dve", sT[1], Aim, pi, ALU.mult)
                kb.tt("dve", sT[2], Are, pi, ALU.mult)
                kb.tt("dve", sT[3], Aim, pr, ALU.mult)
                kb.tt("dve", sT[0], sT[0], sT[1], ALU.subtract)
                kb.tt("dve", sT[2], sT[2], sT[3], ALU.add)
                kb.tt("dve", HS[0][:, :, tl], sT[0], BU[0][:, :, tl], ALU.add)
                kb.tt("dve", HS[1][:, :, tl], sT[2], BU[1][:, :, tl], ALU.add)
                if t in ends:
                    sidx = ends[t]
                    for i, (pn, sn_) in enumerate((("s5re_p", "s5re_s"), ("s5im_p", "s5im_s"))):
                        pt = psum()
                        kb.tr(pt[:64, 0:128], HS[i][:, :, tl], ident)
                        so = sio[i]
                        kb.cp("dve", so, pt[:64, 0:128])
                        dst = outs[pn] if sidx < 0 else outs[sn_][sidx]
                        kb.dma("sp", dst.rearrange("(j gl) p -> j (gl p)", gl=2), so)
            kb.cp("dve", carry[0], HS[0][:, :, L - 1])
            kb.cp("dve", carry[1], HS[1][:, :, L - 1])
            if S5STOP <= 4:
                t0 += L
                bi += 1
                continue
            for m in range(16):
                pps = [psum(), psum()] if 4 * L > 512 else [psum()]
                for q in range(4):
                    pp = pps[q // 2] if len(pps) == 2 else pps[0]
                    off = (q % 2 if len(pps) == 2 else q) * L
                    kb.mm(pp[:, off:off + L], LC[0][:, m, :], HS[0][:, 4 * m + q, :L], start=True, stop=False)
                    kb.mm(pp[:, off:off + L], LC[1][:, m, :], HS[1][:, 4 * m + q, :L], start=False, stop=True)
                    if q == 0:
                        kb.ts("dve", yq[:, :L], pp[:, off:off + L], pm[:, 4:5], None, ALU.mult)
                    else:
                        kb.stt("dve", yq[:, :L], pp[:, off:off + L], pm[:, 4 + q:5 + q], yq[:, :L], ALU.mult, ALU.add)
                kb.stt("dve", yT[:, :L], u[:, m, :L], Dcol[:, m:m + 1], yq[:, :L], ALU.mult, ALU.add)
                z = zb()
                kb.act(z[:, :L], yT[:, :L], AF.Gelu_apprx_tanh)
                kb.dma("sp", ZT[m, :, t0:t0 + L], z[:, :L])
            t0 += L
            bi += 1
        kb.P.flush()


def phase_l1(kb, cfg, ins, outs, S):
    def pre_a(ctx):
        ctx["ot"] = Rot([kb.sb(ctx["st"], [128, 512], F32, "ot") for _ in range(4)])
        ctx["i"] = 0

    def epi_a(ctx, cc, t0, tb, pa, x):
        ot = ctx["ot"]()
        ctx["i"] += 1
        kb.cp("act" if ctx["i"] % 2 == 0 else "dve", ot[:, :tb], pa[:, :tb])
        kb.dma("sp", S["UT"][cc, :, t0:t0 + tb], ot[:, :tb])

    L1STOP = int(os.environ.get("L1STOP", "99"))
    dense_T(kb, cfg, ins, S["X2T"], ins["od_w_in"], epi_a, pre=pre_a)
    if L1STOP <= 1:
        return
    phase_s5(kb, cfg, ins, outs, S["UT"], S["ZT"])
    if L1STOP <= 5:
        return

    def pre_c(ctx):
        ctx["sg"] = Rot([kb.sb(ctx["st"], [128, 512], F32, "sg") for _ in range(2)])
        ctx["zz"] = Rot([kb.sb(ctx["st"], [128, 512], BF16, "zz") for _ in range(4)])
        ctx["bcol"] = kb.sb(ctx["st"], [128, 16], F32, "bcol")
        kb.dma("sp", ctx["bcol"], ins["c_b_glu"].rearrange("(m p) -> p m", p=128), allow_slow_non_contiguous=True)

    def epi_c(ctx, cc, t0, tb, pa, x):
        sg = ctx["sg"]()
        zz = ctx["zz"]()
        kb.act(sg[:, :tb], pa[:, :tb], AF.Sigmoid, bias=ctx["bcol"][:, cc:cc + 1], scale=1.0)
        kb.tt("dve", zz[:, :tb], x[:, cc, :tb], sg[:, :tb], ALU.mult)
        kb.dma("sp", S["ZZT"][cc, :, t0:t0 + tb], zz[:, :tb])

    dense_T(kb, cfg, ins, S["ZT"], ins["c_w_glu"], epi_c, pre=pre_c)
    phase_outln(kb, cfg, ins, None, ins["od_w_out"], S["X2"], 1, S["X3"], S["X3T"], S["GT3"], srcT=S["ZZT"])
```

```python
import contextlib
import math
import os
import numpy as np
import concourse.bass as bass
import concourse.mybir as mybir
from concourse.bass_utils import run_bass_kernel_spmd

F32 = mybir.dt.float32
BF16 = mybir.dt.bfloat16
AF = mybir.ActivationFunctionType
ALU = mybir.AluOpType
AX = mybir.AxisListType

D = 2048
A_COLS = 3088
B_COLS = 3328
EV_COLS = A_COLS + B_COLS
ALPHA = 4.0 ** 0.25
LN_EPS = 1e-5


class Buf:
    __slots__ = ("name", "w", "r")

    def __init__(self, name=""):
        self.name = name
        self.w = []
        self.r = []


class Op:
    __slots__ = ("eng", "fn", "waits", "signal", "sigval", "is_dma", "dsem", "dval", "epoch")

    def __init__(self, eng, fn, is_dma=False, epoch=0):
        self.eng = eng
        self.fn = fn
        self.waits = []
        self.signal = False
        self.sigval = 0
        self.is_dma = is_dma
        self.dsem = None
        self.dval = 0
        self.epoch = epoch


class Prog:
    ENGS = ("pe", "act", "dve", "pool", "sp")
    ENGOBJ = {"pe": "tensor", "act": "scalar", "dve": "vector", "pool": "gpsimd", "sp": "sync"}

    def __init__(self, nc, stack, dma_slots=None):
        self.nc = nc
        self.ops = {e: [] for e in self.ENGS}
        self.dma_slots = dma_slots or {"sp": 16, "act": 8, "pool": 12}
        self.dma_count = {q: 0 for q in self.dma_slots}
        self.dma_last = {q: [None] * n for q, n in self.dma_slots.items()}
        self.epoch = 0
        self.sigbase = {e: 0 for e in self.ENGS}
        self.known = {e: {} for e in self.ENGS}
        self.esem = {e: stack.enter_context(nc.semaphore(f"s_{e}")) for e in self.ENGS}
        self.dsem = {}
        for q, n in self.dma_slots.items():
            for s in range(n):
                self.dsem[(q, s)] = stack.enter_context(nc.semaphore(f"d_{q}{s}"))

    def _deps(self, rec, reads, writes):
        deps = []
        for b in reads:
            deps.extend(b.w)
        for b in writes:
            for d in b.w:
                if d.is_dma or rec.is_dma or d.eng != rec.eng:
                    deps.append(d)
            for d in b.r:
                if d.is_dma or rec.is_dma or d.eng != rec.eng:
                    deps.append(d)
        seen = set()
        for d in deps:
            if d is rec or id(d) in seen or d.epoch < self.epoch:
                continue
            if (not d.is_dma) and (not rec.is_dma) and d.eng == "pe" and rec.eng == "pe":
                continue
            seen.add(id(d))
            rec.waits.append(d)
            d.signal = True
        for b in reads:
            if not rec.is_dma:
                b.r = [x for x in b.r if x.is_dma or x.eng != rec.eng]
            b.r.append(rec)
        for b in writes:
            b.w = [rec]
            b.r = []

    def op(self, eng, fn, reads=(), writes=()):
        rec = Op(eng, fn, epoch=self.epoch)
        self._deps(rec, reads, writes)
        self.ops[eng].append(rec)
        return rec

    def dma(self, q, out, in_, reads=(), writes=(), **kw):
        rec = Op(q, (out, in_, kw), is_dma=True, epoch=self.epoch)
        i = self.dma_count[q]
        self.dma_count[q] += 1
        K = self.dma_slots[q]
        slot = i % K
        rec.dsem = (q, slot)
        rec.dval = 16 * (i // K + 1)
        prev = self.dma_last[q][slot]
        if prev is not None and prev.epoch == self.epoch:
            rec.waits.append(prev)
        self.dma_last[q][slot] = rec
        self._deps(rec, reads, writes)
        self.ops[q].append(rec)
        return rec

    def flush(self):
        nc = self.nc
        for e in self.ENGS:
            last = None
            for rec in self.ops[e]:
                if not rec.is_dma:
                    last = rec
            if last is not None:
                last.signal = True
            c = self.sigbase[e]
            for rec in self.ops[e]:
                if (not rec.is_dma) and rec.signal:
                    c += 1
                    rec.sigval = c
            self.sigbase[e] = c
        final = {}
        for e in self.ENGS:
            final[e] = self.sigbase[e]
        dfinal = {}
        for q, lst in self.dma_last.items():
            for s, rec in enumerate(lst):
                if rec is not None:
                    dfinal[(q, s)] = rec.dval
        with nc.Block() as block:
            def make(e):
                def body(eng):
                    known = self.known[e]
                    for rec in self.ops[e]:
                        for d in rec.waits:
                            if d.is_dma:
                                key, val, sem = d.dsem, d.dval, self.dsem[d.dsem]
                            else:
                                key, val, sem = d.eng, d.sigval, self.esem[d.eng]
                            if known.get(key, 0) >= val:
                                continue
                            known[key] = val
                            eng.wait_ge(sem, val)
                        if rec.is_dma:
                            out, in_, kw = rec.fn
                            eng.dma_start(out=out, in_=in_, **kw).then_inc(self.dsem[rec.dsem], 16)
                        else:
                            ins = rec.fn(eng)
                            if rec.signal:
                                ins.then_inc(self.esem[e], 1)
                    for k, val in final.items():
                        if val > 0 and known.get(k, 0) < val and k != e:
                            known[k] = val
                            eng.wait_ge(self.esem[k], val)
                    for k, val in dfinal.items():
                        if known.get(k, 0) < val:
                            known[k] = val
                            eng.wait_ge(self.dsem[k], val)
                return body
            for e in self.ENGS:
                getattr(block, self.ENGOBJ[e])(make(e))
        self.ops = {e: [] for e in self.ENGS}
        self.epoch += 1


class V:
    __slots__ = ("ap", "buf")

    def __init__(self, ap, buf):
        self.ap = ap
        self.buf = buf

    def __getitem__(self, idx):
        return V(self.ap[idx], self.buf)

    def re(self, pat, **kw):
        return V(self.ap.rearrange(pat, **kw), self.buf)

    def bc(self, shape):
        return V(self.ap.to_broadcast(list(shape)), self.buf)


class K:
    def __init__(self, nc, stack):
        self.nc = nc
        self.P = Prog(nc, stack)
        self.n = 0

    def sb(self, st, shape, dt=F32, name=None):
        self.n += 1
        h = st.enter_context(self.nc.sbuf_tensor(f"{name or 't'}{self.n}", list(shape), dt))
        return V(h[tuple(slice(None) for _ in shape)], Buf(name or "t"))

    def ps(self, st, shape, dt=F32, name=None):
        self.n += 1
        h = st.enter_context(self.nc.psum_tensor(f"{name or 'p'}{self.n}", list(shape), dt))
        return V(h[tuple(slice(None) for _ in shape)], Buf(name or "p"))

    def dma(self, q, out, in_, **kw):
        outv = out if isinstance(out, V) else V(out, None)
        inv = in_ if isinstance(in_, V) else V(in_, None)
        return self.P.dma(q, outv.ap, inv.ap,
                          reads=[inv.buf] if inv.buf is not None else [],
                          writes=[outv.buf] if outv.buf is not None else [], **kw)

    def mm(self, out, lhsT, rhs, start=True, stop=True):
        self.P.op("pe", lambda e: e.matmul(out.ap, lhsT.ap, rhs.ap, start=start, stop=stop),
                  reads=[lhsT.buf, rhs.buf], writes=[out.buf])

    def tr(self, out, in_, ident):
        self.P.op("pe", lambda e: e.transpose(out.ap, in_.ap, ident.ap),
                  reads=[in_.buf, ident.buf], writes=[out.buf])

    def tt(self, eng, out, a, b, op):
        self.P.op(eng, lambda e: e.tensor_tensor(out.ap, a.ap, b.ap, op),
                  reads=[a.buf, b.buf], writes=[out.buf])

    def ts(self, eng, out, a, s1, s2=None, op0=ALU.mult, op1=None):
        rd = [a.buf]
        s1a = s1.ap if isinstance(s1, V) else s1
        s2a = s2.ap if isinstance(s2, V) else s2
        if isinstance(s1, V):
            rd.append(s1.buf)
        if isinstance(s2, V):
            rd.append(s2.buf)
        if op1 is None:
            self.P.op(eng, lambda e: e.tensor_scalar(out.ap, a.ap, s1a, None, op0), reads=rd, writes=[out.buf])
        else:
            self.P.op(eng, lambda e: e.tensor_scalar(out.ap, a.ap, s1a, s2a, op0, op1), reads=rd, writes=[out.buf])

    def stt(self, eng, out, a, s, b, op0, op1):
        rd = [a.buf, b.buf]
        sa = s.ap if isinstance(s, V) else s
        if isinstance(s, V):
            rd.append(s.buf)
        self.P.op(eng, lambda e: e.scalar_tensor_tensor(out.ap, a.ap, sa, b.ap, op0, op1), reads=rd, writes=[out.buf])

    def act(self, out, a, func, bias=None, scale=None):
        rd = [a.buf]
        kw = {}
        if bias is not None:
            kw["bias"] = bias.ap if isinstance(bias, V) else bias
            if isinstance(bias, V):
                rd.append(bias.buf)
        if scale is not None:
            kw["scale"] = scale.ap if isinstance(scale, V) else scale
            if isinstance(scale, V):
                rd.append(scale.buf)
        self.P.op("act", lambda e: e.activation(out.ap, a.ap, func, **kw), reads=rd, writes=[out.buf])

    def cp(self, eng, out, a):
        if eng == "act":
            self.P.op("act", lambda e: e.copy(out.ap, a.ap), reads=[a.buf], writes=[out.buf])
        else:
            self.P.op(eng, lambda e: e.tensor_copy(out.ap, a.ap), reads=[a.buf], writes=[out.buf])

    def red(self, eng, out, a, op=ALU.add, axis=AX.X):
        self.P.op(eng, lambda e: e.tensor_reduce(out.ap, a.ap, axis, op), reads=[a.buf], writes=[out.buf])

    def recip(self, out, a):
        self.P.op("dve", lambda e: e.reciprocal(out.ap, a.ap), reads=[a.buf], writes=[out.buf])

    def rsqrt(self, out, a, scale, bias):
        self.act(out, a, AF.Sqrt, bias=bias, scale=scale)
        self.recip(out, out)

    def memset(self, eng, out, val):
        self.P.op(eng, lambda e: e.memset(out.ap, val), reads=[], writes=[out.buf])


def row_bcast(ap1d, nparts):
    n = ap1d.shape[-1]
    return ap1d.rearrange("(o n) -> o n", o=1).to_broadcast([nparts, n])


def make_consts():
    c = {}
    c["ident"] = np.eye(128, dtype=np.float32)
    i = np.arange(128)
    c["tri_incl"] = (i[:, None] <= i[None, :]).astype(np.float32)
    c["tri_strict"] = (i[:, None] < i[None, :]).astype(np.float32)
    c["tri_gt"] = (i[:, None] > i[None, :]).astype(np.float32)
    c["ones"] = np.ones((128, 128), dtype=np.float32)
    sel = np.zeros((128, 16, 128), dtype=np.float32)
    for e in range(16):
        sel[e, e, :] = 1.0
    c["sel"] = sel.reshape(128, 16 * 128)
    pm = np.zeros((128, 8), dtype=np.float32)
    for q in range(4):
        pm[32 * q:32 * q + 32, 4 + q] = 1.0
    pm[:64, 0] = 1.0
    pm[64:, 1] = 1.0
    pm[:, 2] = ((i // 16) % 2 == 0)
    pm[:, 3] = ((i // 16) % 2 == 1)
    return np.concatenate([c["ident"], c["tri_incl"], c["tri_strict"], c["tri_gt"], c["ones"], c["sel"], pm], axis=1)


C_IDENT, C_TRII, C_TRIS, C_TRIG, C_ONES, C_SEL = 0, 128, 256, 384, 512, 640
C_PM = 640 + 16 * 128
C_TOTAL = C_PM + 8


class Cfg:
    def __init__(self, n_prompt=2048, n_samp=16, debug=False, phases=None):
        self.n_meta = 16
        self.n_prompt = n_prompt
        self.NP = 16 + n_prompt
        self.n_samp = n_samp
        self.NT = self.NP + 8 * n_samp
        self.debug = debug
        self.phases = phases
        ch = [(0, 16, True, -1)]
        r = 16
        while r < self.NP:
            ch.append((r, 64, False, -1))
            r += 64
        for s in range(n_samp):
            ch.append((self.NP + 8 * s, 8, True, s))
        self.chunks = ch
        self.subtiles = []
        r = 0
        while r < self.NT:
            n = min(128, self.NT - r)
            self.subtiles.append((r, n))
            r += n


def load_consts(kb, st, cst):
    t = kb.sb(st, [128, C_TOTAL], F32, "consts")
    kb.dma("sp", t, cst)
    tb = kb.sb(st, [128, 128], BF16, "identb")
    kb.cp("dve", tb, t[:, C_IDENT:C_IDENT + 128])
    return t, tb


def dense_phase(kb, cfg, cst, src, W, ncols, epi, post=None, pre=None, srcT=None):
    with contextlib.ExitStack() as st:
        consts, identb = load_consts(kb, st, cst)
        KC = 16
        Wb = kb.sb(st, [128, KC, ncols], BF16, "Wb")
        Wv = W.rearrange("(kc p) n -> p kc n", p=128)
        for g in range(4):
            kb.dma("pool", Wb[:, 4 * g:4 * g + 4, :], Wv[:, 4 * g:4 * g + 4, :])
        xs = [kb.sb(st, [128, D], F32, "xs") for _ in range(2)]
        xb = [kb.sb(st, [128, D], BF16, "xb") for _ in range(2)]
        xT = [kb.sb(st, [128, KC, 128], BF16, "xT") for _ in range(2)]
        ptb = [kb.ps(st, [128, KC, 128], BF16, "ptb") for _ in range(1)]
        pacc = [kb.ps(st, [128, 512], F32, "pacc") for _ in range(4)]
        ctx = {"st": st, "consts": consts, "identb": identb}
        if pre is not None:
            pre(ctx)
        ia = 0
        for ts, (r0, n) in enumerate(cfg.subtiles):
            a = ts % 2
            if srcT is not None:
                kb.dma("sp", xT[a][:, :, :n], srcT[:, :, r0:r0 + n].rearrange("kc p t -> p kc t"))
            else:
                kb.dma("sp", xs[a][:n, :], src[r0:r0 + n, :])
                kb.cp("act", xb[a][:n, :], xs[a][:n, :])
                pt = ptb[0]
                for kc in range(KC):
                    kb.tr(pt[:, kc, :n], xb[a][:n, kc * 128:(kc + 1) * 128], identb[:n, :n])
                kb.cp("dve", xT[a][:, 0:8, :n], pt[:, 0:8, :n])
                kb.cp("dve", xT[a][:, 8:16, :n], pt[:, 8:16, :n])
            ncb = (ncols + 511) // 512
            for cb in range(ncb):
                cw = min(512, ncols - cb * 512)
                pa = pacc[ia % 4]
                ia += 1
                for kc in range(KC):
                    kb.mm(pa[:n, :cw], xT[a][:, kc, :n], Wb[:, kc, cb * 512:cb * 512 + cw],
                          start=(kc == 0), stop=(kc == KC - 1))
                epi(ctx, ts, r0, n, cb, cw, pa)
            if post is not None:
                post(ctx, ts, r0, n)
        kb.P.flush()


def phase_inproj(kb, cfg, cst, xtok, w_in, P0):
    for c0 in range(0, EV_COLS, 2048):
        ncols = min(2048, EV_COLS - c0)
        state = {"i": 0}

        def pre(ctx):
            ctx["ot"] = [kb.sb(ctx["st"], [128, 512], F32, "ot") for _ in range(4)]

        def epi(ctx, ts, r0, n, cb, cw, pa, c0=c0, state=state):
            i = state["i"]
            state["i"] += 1
            ot = ctx["ot"][i % 4]
            kb.cp("act" if i % 2 == 0 else "dve", ot[:n, :cw], pa[:n, :cw])
            kb.dma("sp", P0[r0:r0 + n, c0 + cb * 512:c0 + cb * 512 + cw], ot[:n, :cw])

        dense_phase(kb, cfg, cst, xtok, w_in[:, c0:c0 + ncols], ncols, epi, pre=pre)


WEIGHT_SHAPES = {
    "ev_w_in": [D, EV_COLS], "ev_w_out": [D, D], "a_gate_up": [16, 512], "a_gate_b": [512],
    "a_norm_g": [1024], "b_mu": [B_COLS], "b_w0": [1024], "b_w_up": [64, 1024], "b_a0": [1024],
    "b_a_up": [64, 1024], "b_g_up": [128, 1024], "b_k_k": [1024], "b_k_a": [1024], "b_r_k": [1024],
    "b_ln_g": [1024], "b_ln_b": [1024], "od_w_in": [D, D], "c_a_re": [128, 64], "c_a_im": [128, 64],
    "c_log_dt": [128], "c_b_re": [128, 64, 16], "c_b_im": [128, 64, 16], "c_c_re": [128, 16, 64],
    "c_c_im": [128, 16, 64], "c_d": [D], "c_w_glu": [D, D], "c_b_glu": [D], "od_w_out": [D, D],
    "w_router": [D, 16], "moe_w_up": [2, 16, D, D], "moe_w_down": [2, 16, 1024, D],
    "ln_mix_g": [2, D], "ln_mix_b": [2, D], "ln_ffn_g": [2, D], "ln_ffn_b": [2, D],
}


def build(cfg):
    nc = bass.Bass("TRN2", target_bir_lowering=False)
    NT, NS = cfg.NT, cfg.n_samp
    ins = {}

    def inp(name, shape):
        ins[name] = nc.dram_tensor(name, list(shape), F32, kind="ExternalInput").ap()
        return ins[name]

    inp("xtok", [NT, D])
    inp("consts", [128, C_TOTAL])
    inp("state_gla", [NS, 4, 128, 256])
    inp("state_rwkv", [NS, 16, 64, 64])
    inp("state_shift", [NS, B_COLS])
    inp("state_s5_re", [NS, 128, 64])
    inp("state_s5_im", [NS, 128, 64])
    for k, shp in WEIGHT_SHAPES.items():
        if cfg.phases is not None and k.startswith("moe_") and not any(p.startswith("moe") for p in cfg.phases):
            continue
        inp(k, shp)
    outs = {}

    def outp(name, shape):
        outs[name] = nc.dram_tensor(name, list(shape), F32, kind="ExternalOutput").ap()
        return outs[name]

    def scratch(name, shape, dt=F32):
        kind = "ExternalOutput" if cfg.debug else "Internal"
        t = nc.dram_tensor(name, list(shape), dt, kind=kind).ap()
        if cfg.debug:
            outs[name] = t
        return t

    outp("y", [NT, D])
    outp("gla_p", [4, 128, 256]); outp("gla_s", [NS, 4, 128, 256])
    outp("rwkv_p", [16, 64, 64]); outp("rwkv_s", [NS, 16, 64, 64])
    outp("shift_p", [B_COLS]); outp("shift_s", [NS, B_COLS])
    outp("s5re_p", [128, 64]); outp("s5re_s", [NS, 128, 64])
    outp("s5im_p", [128, 64]); outp("s5im_s", [NS, 128, 64])
    S = {}
    S["P0"] = scratch("P0", [NT, EV_COLS])
    S["OM"] = scratch("OM", [NT, D])
    S["X1"] = scratch("X1", [NT, D])
    S["X1T"] = scratch("X1T", [16, 128, NT], BF16)
    S["GT"] = scratch("GT", [16, NT])
    S["X2"] = scratch("X2", [NT, D])
    S["UT"] = scratch("UT", [16, 128, NT])
    S["ZT"] = scratch("ZT", [16, 128, NT], BF16)
    S["X2T"] = scratch("X2T", [16, 128, NT], BF16)
    S["ZZT"] = scratch("ZZT", [16, 128, NT], BF16)
    S["X3"] = scratch("X3", [NT, D])
    S["X3T"] = scratch("X3T", [16, 128, NT], BF16)
    S["GT3"] = scratch("GT3", [16, NT])

    ph = cfg.phases
    with contextlib.ExitStack() as stack:
        kb = K(nc, stack)
        if ph is None or "inproj" in ph:
            phase_inproj(kb, cfg, ins["consts"], ins["xtok"], ins["ev_w_in"], S["P0"])
        if ph is None or "mix0" in ph:
            phase_mix0(kb, cfg, ins, outs, S)
        if ph is None or "out0" in ph:
            phase_outln(kb, cfg, ins, S["OM"], ins["ev_w_out"], ins["xtok"], 0, S["X1"], S["X1T"], S["GT"])
        if ph is None or "moe0" in ph:
            phase_moe(kb, cfg, ins, 0, S["X1"], S["X1T"], S["GT"], S["X2"], S["X2T"])
        if ph is None or "l1" in ph:
            phase_l1(kb, cfg, ins, outs, S)
        if ph is None or "moe1" in ph:
            phase_moe(kb, cfg, ins, 1, S["X3"], S["X3T"], S["GT3"], outs["y"])
    return nc, ins, outs


def phase_l1(*a, **k):
    raise NotImplementedError


class Rot:
    def __init__(self, tiles):
        self.t = tiles
        self.i = 0

    def __call__(self):
        t = self.t[self.i % len(self.t)]
        self.i += 1
        return t


def phase_mix0(kb, cfg, ins, outs, S):
    RSTOP = int(os.environ.get('RSTOP', '99'))
    P0, OM = S["P0"], S["OM"]
    C0 = math.exp(-0.5)
    with contextlib.ExitStack() as st:
        cs = kb.sb(st, [128, 640], F32, "consts")
        kb.dma("sp", cs, ins["consts"][:, 0:640])
        ident = cs[:, C_IDENT:C_IDENT + 128]
        tri_i = cs[:, C_TRII:C_TRII + 128]
        tri_s = cs[:, C_TRIS:C_TRIS + 128]
        tri_g = cs[:, C_TRIG:C_TRIG + 128]
        ones = cs[:, C_ONES:C_ONES + 128]

        def bcast(name, n, parts=64):
            t = kb.sb(st, [parts, n], F32, name)
            kb.dma("sp", t, row_bcast(ins[name], parts))
            return t

        mu_bc = bcast("b_mu", B_COLS)
        kk_bc = bcast("b_k_k", 1024)
        ka_bc = bcast("b_k_a", 1024)
        rk_bc = bcast("b_r_k", 1024)
        lng_bc = bcast("b_ln_g", 1024)
        lnb_bc = bcast("b_ln_b", 1024)
        ng_bc = bcast("a_norm_g", 1024)
        w0_row = bcast("b_w0", 1024, 1)
        a0_row = bcast("b_a0", 1024, 1)
        gb_row = bcast("a_gate_b", 512, 1)
        w_up = kb.sb(st, [64, 1024], F32, "w_up"); kb.dma("sp", w_up, ins["b_w_up"])
        a_up = kb.sb(st, [64, 1024], F32, "a_up"); kb.dma("sp", a_up, ins["b_a_up"])
        g_up = kb.sb(st, [128, 1024], F32, "g_up"); kb.dma("sp", g_up, ins["b_g_up"])
        gate_up = kb.sb(st, [16, 512], F32, "gate_up"); kb.dma("sp", gate_up, ins["a_gate_up"])

        pa = kb.sb(st, [64, A_COLS], F32, "pa")
        pb = kb.sb(st, [64, B_COLS], F32, "pb")
        xm = kb.sb(st, [64, B_COLS], F32, "xm")
        omix = kb.sb(st, [64, D], F32, "omix")
        Sg = kb.sb(st, [128, 4, 256], F32, "Sg")
        M = kb.sb(st, [64, 16, 64], F32, "M")
        Mio = kb.sb(st, [64, 16, 64], F32, "Mio")
        W = [kb.sb(st, [64, 512], F32, f"w{i}") for i in range(16)]
        XT = [kb.sb(st, [64, 8, 64], F32, f"xt{i}") for i in range(4)]
        AM = [kb.sb(st, [64, 8, 64], F32, f"am{i}") for i in range(7)]
        small = Rot([kb.sb(st, [128, 256], F32, f"sm{i}") for i in range(10)])
        col = Rot([kb.sb(st, [128, 16], F32, f"col{i}") for i in range(12)])
        psum = Rot([kb.ps(st, [128, 512], F32, f"ps{i}") for i in range(8)])

        def h3(v, C):
            return v.re("p (h k) -> p h k", h=8)

        def mask3(m, C):
            return m[:C, :C].re("p (o c) -> p o c", o=1).bc([C, 8, C])

        for (r0, C, seq_start, sidx) in cfg.chunks:
            last = (r0 + C == cfg.NP) if sidx < 0 else True
            kb.dma("sp", pa[:C, :], P0[r0:r0 + C, 0:A_COLS])
            kb.dma("sp", pb[:C, :], P0[r0:r0 + C, A_COLS:EV_COLS])
            if seq_start:
                if sidx < 0:
                    kb.memset("dve", xm[0:1, :], 0.0)
                    kb.memset("dve", Sg, 0.0)
                    kb.memset("dve", M, 0.0)
                else:
                    kb.dma("sp", xm[0:1, :], ins["state_shift"][sidx:sidx + 1, :])
                    kb.dma("sp", Sg, ins["state_gla"][sidx].rearrange("h k v -> k h v"))
                    kb.dma("sp", Mio, ins["state_rwkv"][sidx].rearrange("h v k -> v h k"))
                    for hh in range(2):
                        pt = psum()
                        for h in range(8):
                            kb.tr(pt[:64, h * 64:(h + 1) * 64], Mio[:, hh * 8 + h, :], ident[:64, :64])
                        kb.cp("dve", M[:, hh * 8:hh * 8 + 8, :], pt[:64, :].re("p (h v) -> p h v", h=8))
                if C > 1:
                    kb.dma("sp", xm[1:C, :], P0[r0:r0 + C - 1, A_COLS:EV_COLS])
            else:
                kb.dma("sp", xm[:C, :], P0[r0 - 1:r0 + C - 1, A_COLS:EV_COLS])

            pt = psum()
            kb.tr(pt[:16, :C], pa[:C, 3072:3088], ident[:C, :C])
            gdT = small()
            kb.cp("dve", gdT[:16, :C], pt[:16, :C])
            pg = psum()
            kb.mm(pg[:C, :512], gdT[:16, :C], gate_up[:16, :], start=True, stop=False)
            kb.mm(pg[:C, :512], ones[0:1, :C], gb_row[0:1, :], start=False, stop=True)
            lgp = W[0]
            kb.act(lgp[:C, :], pg[:C, :512], AF.Exp, scale=-1.0)
            kb.act(lgp[:C, :], lgp[:C, :], AF.Ln, bias=1.0)
            for h in range(4 if os.environ.get('NOGLA') is None else 0):
                lgh = lgp[:C, h * 128:(h + 1) * 128]
                q_tok = pa[:C, h * 128:(h + 1) * 128]
                k_tok = pa[:C, 512 + h * 128:512 + (h + 1) * 128]
                v_tok = pa[:C, 1024 + h * 256:1024 + (h + 1) * 256]
                r_tok = pa[:C, 2048 + h * 256:2048 + (h + 1) * 256]
                pbT = psum()
                kb.mm(pbT[:, :C], lgh, tri_i[:C, :C])
                eqT = small(); ekT = small()
                kb.act(eqT[:, :C], pbT[:, :C], AF.Exp, scale=-1.0 / 16)
                kb.act(ekT[:, :C], pbT[:, :C], AF.Exp, scale=1.0 / 16)
                pq = psum()
                kb.tr(pq[:, :C], q_tok, ident[:C, :C])
                kb.tr(pq[:, 128:128 + C], k_tok, ident[:C, :C])
                qtT = small(); ktT = small()
                kb.stt("dve", qtT[:, :C], pq[:, :C], 128.0 ** -0.5, eqT[:, :C], ALU.mult, ALU.mult)
                kb.tt("dve", ktT[:, :C], pq[:, 128:128 + C], ekT[:, :C], ALU.mult)
                patt = psum()
                kb.mm(patt[:C, :C], ktT[:, :C], qtT[:, :C])
                attm = small()
                kb.tt("dve", attm[:C, :C], patt[:C, :C], tri_i[:C, :C], ALU.mult)
                pdl = psum()
                kb.mm(pdl[:C, :128], tri_g[:C, :C], lgh)
                khat = small()
                kb.act(khat[:C, :128], pdl[:C, :128], AF.Exp, scale=-1.0 / 16)
                kb.tt("dve", khat[:C, :128], khat[:C, :128], k_tok, ALU.mult)
                po = psum()
                kb.mm(po[:C, :256], attm[:C, :C], v_tok, start=True, stop=False)
                kb.mm(po[:C, :256], qtT[:, :C], Sg[:, h, :], start=False, stop=True)
                pS = psum()
                kb.mm(pS[:, :256], khat[:C, :128], v_tok)
                kb.stt("dve", Sg[:, h, :], Sg[:, h, :], eqT[:, C - 1:C], pS[:, :256], ALU.mult, ALU.add)
                sq = small(); ssq = col()
                kb.act(sq[:C, :], po[:C, :256], AF.Square)
                kb.red("dve", ssq[:C, 0:1], sq[:C, :])
                kb.rsqrt(ssq[:C, 0:1], ssq[:C, 0:1], 1.0 / 256, 1e-5)
                og = small(); sr = small()
                kb.stt("dve", og[:C, :], po[:C, :256], ssq[:C, 0:1], ng_bc[:C, h * 256:(h + 1) * 256], ALU.mult, ALU.mult)
                kb.act(sr[:C, :], r_tok, AF.Silu)
                kb.tt("dve", omix[:C, h * 256:(h + 1) * 256], og[:C, :], sr[:C, :], ALU.mult)
            if last:
                dst = outs["gla_p"] if sidx < 0 else outs["gla_s"][sidx]
                kb.dma("sp", dst.rearrange("h k v -> k h v"), Sg)

            kb.tt("dve", xm[:C, :], xm[:C, :], pb[:C, :], ALU.subtract)
            kb.tt("dve", xm[:C, :], xm[:C, :], mu_bc[:C, :], ALU.mult)
            kb.tt("dve", xm[:C, :], xm[:C, :], pb[:C, :], ALU.add)
            twd = small(); sgd = small()
            kb.act(twd[:C, :64], xm[:C, 3072:3136], AF.Tanh)
            kb.act(sgd[:C, :128], xm[:C, 3200:3328], AF.Sigmoid)
            pt = psum()
            kb.tr(pt[:64, 0:C], twd[:C, :64], ident[:C, :C])
            kb.tr(pt[:64, 64:64 + C], xm[:C, 3136:3200], ident[:C, :C])
            kb.tr(pt[:128, 128:128 + C], sgd[:C, :128], ident[:C, :C])
            tT = small()
            kb.cp("dve", tT[:64, 0:128], pt[:64, 0:128])
            kb.cp("dve", tT[:128, 128:128 + C], pt[:128, 128:128 + C])
            twdT = tT[:64, 0:C]; adT = tT[:64, 64:64 + C]; sgdT = tT[:128, 128:128 + C]
            for hh in range(2 if os.environ.get('NORWKV') is None else 0):
                hs = hh * 512
                r = xm[:C, hs:hs + 512]
                k = xm[:C, 1024 + hs:1024 + hs + 512]
                v = xm[:C, 2048 + hs:2048 + hs + 512]
                wl, a, g, kk, kmod, b, Gs, rt, eGn, nbt, kt, kap, oc, tmp1, tmp2, tmp3 = [w[:C, :] for w in W]
                pz = psum()
                kb.mm(pz[:C, :], twdT, w_up[:64, hs:hs + 512], start=True, stop=False)
                kb.mm(pz[:C, :], ones[0:1, :C], w0_row[0:1, hs:hs + 512], start=False, stop=True)
                kb.act(wl, pz[:C, :], AF.Sigmoid)
                pz = psum()
                kb.mm(pz[:C, :], adT, a_up[:64, hs:hs + 512], start=True, stop=False)
                kb.mm(pz[:C, :], ones[0:1, :C], a0_row[0:1, hs:hs + 512], start=False, stop=True)
                kb.act(a, pz[:C, :], AF.Sigmoid)
                pz = psum()
                kb.mm(pz[:C, :], sgdT, g_up[:128, hs:hs + 512])
                kb.cp("act", g, pz[:C, :])
                if RSTOP <= 1:
                    continue
                kb.tt("dve", kk, k, kk_bc[:C, hs:hs + 512], ALU.mult)
                kb.tt("dve", tmp1, kk, kk, ALU.mult)
                ss = col()
                kb.red("dve", ss[:C, 0:8], h3(tmp1, C))
                kb.act(ss[:C, 0:8], ss[:C, 0:8], AF.Sqrt)
                kb.ts("dve", ss[:C, 0:8], ss[:C, 0:8], 1e-12, None, ALU.max)
                kb.recip(ss[:C, 0:8], ss[:C, 0:8])
                kb.tt("dve", h3(kk, C), h3(kk, C), ss[:C, 0:8].re("p (h o) -> p h o", o=1).bc([C, 8, 64]), ALU.mult)
                kb.stt("dve", tmp1, a, -1.0, ka_bc[:C, hs:hs + 512], ALU.add, ALU.mult)
                kb.stt("dve", kmod, tmp1, 1.0, k, ALU.add, ALU.mult)
                kb.tt("dve", b, kk, a, ALU.mult)
                if RSTOP <= 2:
                    continue
                pG = psum()
                kb.mm(pG[:C, :], tri_i[:C, :C], wl)
                kb.cp("act", Gs, pG[:C, :])
                kb.act(rt, Gs, AF.Exp, scale=-C0)
                kb.act(eGn, Gs, AF.Exp, scale=C0)
                kb.tt("dve", tmp1, Gs, wl, ALU.subtract)
                kb.act(kap, tmp1, AF.Exp, scale=-C0)
                kb.tt("dve", kap, kap, kk, ALU.mult)
                kb.tt("dve", rt, rt, r, ALU.mult)
                kb.tt("dve", b, b, eGn, ALU.mult)
                kb.ts("dve", nbt, b, -1.0, None, ALU.mult)
                kb.tt("dve", kt, kmod, eGn, ALU.mult)
                if RSTOP <= 3:
                    continue
                for i, src in enumerate((kap, b, kt, rt)):
                    pt = psum()
                    for h in range(8):
                        kb.tr(pt[:64, h * 64:h * 64 + C], src[:, h * 64:(h + 1) * 64], ident[:C, :C])
                    kb.cp("act" if i % 2 == 0 else "dve", XT[i][:, :, :C],
                          pt[:64, :].re("p (h c) -> p h c", h=8)[:, :, :C])
                if RSTOP <= 4:
                    continue
                kapT, btT, ktT, rtT = XT
                AT, A, A2T, A3T, A4T, Q0, Q1 = AM

                def amat(dst, lT, rT, mask, neg=False):
                    pp = psum()
                    for h in range(8):
                        kb.mm(pp[:C, h * 64:h * 64 + C], lT[:, h, :C], rT[:, h, :C])
                    src = pp[:C, :].re("p (h c) -> p h c", h=8)[:, :, :C]
                    if neg:
                        kb.stt("dve", dst[:C, :, :C], src, -1.0, mask3(mask, C), ALU.mult, ALU.mult)
                    else:
                        kb.tt("dve", dst[:C, :, :C], src, mask3(mask, C), ALU.mult)

                amat(AT, btT, kapT, tri_s)
                amat(A, kapT, btT, tri_g)
                amat(A2T, ktT, kapT, tri_s)
                amat(A3T, ktT, rtT, tri_i)
                amat(A4T, btT, rtT, tri_i, neg=True)
                if RSTOP <= 5:
                    continue
                pW = psum()
                for h in range(8):
                    kb.mm(pW[:C, h * 64:(h + 1) * 64], kapT[:, h, :C], M[:, hh * 8 + h, :], start=True, stop=False)
                    kb.mm(pW[:C, h * 64:(h + 1) * 64], A2T[:C, h, :C], v[:, h * 64:(h + 1) * 64], start=False, stop=True)
                U = tmp2
                kb.cp("act", U, pW[:C, :])
                if RSTOP <= 6:
                    continue
                curP, curPT = A, AT
                nxt = [(Q0, Q1), (A2T, A), (Q0, Q1), (A2T, A), (Q0, Q1)]
                n = 0
                while True:
                    pU = psum()
                    for h in range(8):
                        kb.mm(pU[:C, h * 64:(h + 1) * 64], curPT[:C, h, :C], U[:, h * 64:(h + 1) * 64])
                    kb.tt("dve", U, U, pU[:C, :], ALU.subtract if n == 0 else ALU.add)
                    if (2 << n) >= C:
                        break
                    nP, nPT = nxt[n]
                    if n == 1:
                        nP, nPT = A2T, AT
                    p1 = psum(); p2 = psum()
                    for h in range(8):
                        kb.mm(p1[:C, h * 64:h * 64 + C], curPT[:C, h, :C], curP[:C, h, :C])
                        kb.mm(p2[:C, h * 64:h * 64 + C], curP[:C, h, :C], curPT[:C, h, :C])
                    tgt = [t for t in (Q0, Q1, A2T, A, AT) if t is not curP and t is not curPT][:2]
                    nP, nPT = tgt
                    kb.cp("act", nP[:C, :, :C], p1[:C, :].re("p (h c) -> p h c", h=8)[:, :, :C])
                    kb.cp("dve", nPT[:C, :, :C], p2[:C, :].re("p (h c) -> p h c", h=8)[:, :, :C])
                    curP, curPT = nP, nPT
                    n += 1
                if RSTOP <= 7:
                    continue
                pO = psum()
                for h in range(8):
                    sl = slice(h * 64, (h + 1) * 64)
                    kb.mm(pO[:C, sl], rtT[:, h, :C], M[:, hh * 8 + h, :], start=True, stop=False)
                    kb.mm(pO[:C, sl], A3T[:C, h, :C], v[:, sl], start=False, stop=False)
                    kb.mm(pO[:C, sl], A4T[:C, h, :C], U[:, sl], start=False, stop=True)
                if RSTOP <= 8:
                    continue
                pM = psum()
                for h in range(8):
                    sl = slice(h * 64, (h + 1) * 64)
                    kb.mm(pM[:64, sl], kt[:, sl], v[:, sl], start=True, stop=False)
                    kb.mm(pM[:64, sl], nbt[:, sl], U[:, sl], start=False, stop=True)
                pGm = psum()
                for h in range(8):
                    kb.mm(pGm[:64, 64 * h:64 * h + 64], wl[:, h * 64:(h + 1) * 64], ones[:C, 0:64])
                gam = col()
                kb.act(gam[:64, 0:8], pGm[:64, 0:512].re('p (h t) -> p h t', t=64)[:, :, 0], AF.Exp, scale=-C0)
                Mh = M[:, hh * 8:hh * 8 + 8, :]
                kb.tt("dve", Mh, Mh, pM[:64, :].re("p (h v) -> p h v", h=8), ALU.add)
                kb.tt("dve", Mh, Mh, gam[:64, 0:8].re("p (h o) -> p h o", o=1).bc([64, 8, 64]), ALU.mult)
                if RSTOP <= 9:
                    continue
                kb.cp("act", oc, pO[:C, :])
                mean = col()
                kb.red("dve", mean[:C, 0:8], h3(oc, C))
                kb.ts("dve", mean[:C, 0:8], mean[:C, 0:8], -1.0 / 64, None, ALU.mult)
                kb.tt("dve", h3(oc, C), h3(oc, C), mean[:C, 0:8].re("p (h o) -> p h o", o=1).bc([C, 8, 64]), ALU.add)
                if RSTOP <= 10:
                    continue
                kb.tt("dve", tmp1, oc, oc, ALU.mult)
                var = col()
                kb.red("dve", var[:C, 0:8], h3(tmp1, C))
                kb.rsqrt(var[:C, 0:8], var[:C, 0:8], 1.0 / 64, 64e-5)
                kb.tt("dve", h3(oc, C), h3(oc, C), var[:C, 0:8].re("p (h o) -> p h o", o=1).bc([C, 8, 64]), ALU.mult)
                if RSTOP <= 11:
                    continue
                kb.tt("dve", oc, oc, lng_bc[:C, hs:hs + 512], ALU.mult)
                kb.tt("dve", oc, oc, lnb_bc[:C, hs:hs + 512], ALU.add)
                if RSTOP <= 12:
                    continue
                kb.tt("dve", tmp1, r, kmod, ALU.mult)
                kb.tt("dve", tmp1, tmp1, rk_bc[:C, hs:hs + 512], ALU.mult)
                if RSTOP <= 13:
                    continue
                bon = col()
                kb.red("dve", bon[:C, 0:8], h3(tmp1, C))
                kb.tt("dve", h3(tmp1, C), h3(v, C), bon[:C, 0:8].re("p (h o) -> p h o", o=1).bc([C, 8, 64]), ALU.mult)
                if RSTOP <= 14:
                    continue
                kb.tt("dve", oc, oc, tmp1, ALU.add)
                if RSTOP <= 15:
                    continue
                kb.tt("dve", omix[:C, 1024 + hs:1024 + hs + 512], oc, g, ALU.mult)
            kb.dma("sp", OM[r0:r0 + C, :], omix[:C, :])
            if last:
                for hh in range(2):
                    pt = psum()
                    for h in range(8):
                        kb.tr(pt[:64, h * 64:(h + 1) * 64], M[:, hh * 8 + h, :], ident[:64, :64])
                    kb.cp("dve", Mio[:, hh * 8:hh * 8 + 8, :], pt[:64, :].re("p (h v) -> p h v", h=8))
                dst = outs["rwkv_p"] if sidx < 0 else outs["rwkv_s"][sidx]
                kb.dma("sp", dst.rearrange("h v k -> v h k"), Mio)
                lastrow = r0 + C - 1
                dsts = outs["shift_p"] if sidx < 0 else outs["shift_s"][sidx]
                kb.dma("sp", dsts.rearrange("(o n) -> o n", o=1), P0[lastrow:lastrow + 1, A_COLS:EV_COLS])
        kb.P.flush()


def ln_and_route(kb, ctx, cfg, ins, n, r0, v, layer, which, X1, X1T, GT, psum):
    col, c, ident, g_bc, b_bc = ctx["col"], ctx["c"], ctx["ident"], ctx["g_bc"], ctx["b_bc"]
    s1 = col()
    kb.red("dve", s1[:n, 0:1], v[:n, :])
    kb.ts("dve", s1[:n, 0:1], s1[:n, 0:1], -1.0 / D, None, ALU.mult)
    kb.ts("dve", v[:n, :], v[:n, :], s1[:n, 0:1], None, ALU.add)
    kb.act(c[:n, :], v[:n, :], AF.Square)
    s2 = col()
    kb.red("dve", s2[:n, 0:1], c[:n, :])
    kb.rsqrt(s2[:n, 0:1], s2[:n, 0:1], 1.0 / D, LN_EPS)
    kb.stt("dve", v[:n, :], v[:n, :], s2[:n, 0:1], g_bc[:n, :], ALU.mult, ALU.mult)
    kb.tt("dve", v[:n, :], v[:n, :], b_bc[:n, :], ALU.add)
    kb.dma("sp", X1[r0:r0 + n, :], v[:n, :])
    if X1T is None or os.environ.get('NOROUTE'):
        return
    x1T, x1Tb, wr = ctx["x1T"], ctx["x1Tb"], ctx.get("wr")
    for grp in range(4):
        pt = psum()
        for j in range(4):
            kc = grp * 4 + j
            kb.tr(pt[:, j * 128:j * 128 + n], v[:n, kc * 128:(kc + 1) * 128], ident[:n, :n])
        src = pt.re("p (j t) -> p j t", j=4)[:, :, :n]
        kb.cp("act", x1T[:, grp * 4:grp * 4 + 4, :n], src)
        kb.cp("dve", x1Tb[:, grp * 4:grp * 4 + 4, :n], x1T[:, grp * 4:grp * 4 + 4, :n])
    if not os.environ.get('NOX1T'):
        kb.dma("sp", X1T[:, :, r0:r0 + n].rearrange("kc p t -> p kc t"), x1Tb[:, :, :n])
    if GT is None:
        return
    pl = psum()
    for kc in range(16):
        kb.mm(pl[:n, 0:128], x1T[:, kc, :n], wr[:, kc, :], start=(kc == 0), stop=(kc == 15))
    if os.environ.get('NOGATE'):
        return
    sm = ctx["sm"]
    l = sm(); e = sm(); t3 = sm(); t4 = sm(); gate = sm()
    BIG = 1e30
    kb.cp("dve", l[:n, 0:16], pl[:n, 0:16])
    mx = col()
    kb.red("dve", mx[:n, 0:1], l[:n, 0:16], op=ALU.max)
    kb.ts("dve", mx[:n, 0:1], mx[:n, 0:1], -1.0, None, ALU.mult)
    kb.act(e[:n, 0:16], l[:n, 0:16], AF.Exp, bias=mx[:n, 0:1], scale=1.0)
    e3 = e[:n, 0:16].re("p (g j) -> p g j", g=4)
    m1 = col(); m2 = col(); sc = col(); oh = col()
    kb.red("dve", m1[:n, 0:4], e3, op=ALU.max)
    t33 = t3[:n, 0:16].re("p (g j) -> p g j", g=4)
    kb.tt("dve", t33, e3, m1[:n, 0:4].re("p (g o) -> p g o", o=1).bc([n, 4, 4]), ALU.is_equal)
    kb.stt("dve", t33, t33, -BIG, e3, ALU.mult, ALU.add)
    kb.red("dve", m2[:n, 0:4], t33, op=ALU.max)
    kb.tt("dve", sc[:n, 0:4], m1[:n, 0:4], m2[:n, 0:4], ALU.add)
    gm = col()
    kb.red("dve", gm[:n, 0:1], sc[:n, 0:4], op=ALU.max)
    kb.ts("dve", oh[:n, 0:4], sc[:n, 0:4], gm[:n, 0:1], None, ALU.is_equal)
    kb.tt("dve", t33, e3, oh[:n, 0:4].re("p (g o) -> p g o", o=1).bc([n, 4, 4]), ALU.mult)
    ing = col()
    kb.red("dve", ing[:n, 0:4], t3[:n, 0:16].re("p (g j) -> p j g", g=4))
    v1 = col(); v2 = col(); q1 = col(); q2 = col(); i2 = col()
    kb.red("dve", v1[:n, 0:1], ing[:n, 0:4], op=ALU.max)
    kb.ts("dve", q1[:n, 0:4], ing[:n, 0:4], v1[:n, 0:1], None, ALU.is_equal)
    kb.stt("dve", i2[:n, 0:4], q1[:n, 0:4], -BIG, ing[:n, 0:4], ALU.mult, ALU.add)
    kb.red("dve", v2[:n, 0:1], i2[:n, 0:4], op=ALU.max)
    kb.ts("dve", q2[:n, 0:4], i2[:n, 0:4], v2[:n, 0:1], None, ALU.is_equal)
    kb.tt("dve", q1[:n, 0:4], q1[:n, 0:4], q2[:n, 0:4], ALU.add)
    kb.tt("dve", v1[:n, 0:1], v1[:n, 0:1], v2[:n, 0:1], ALU.add)
    kb.recip(v1[:n, 0:1], v1[:n, 0:1])
    kb.tt("dve", q1[:n, 0:4], q1[:n, 0:4], ing[:n, 0:4], ALU.mult)
    kb.ts("dve", q1[:n, 0:4], q1[:n, 0:4], v1[:n, 0:1], None, ALU.mult)
    kb.tt("dve", gate[:n, 0:16].re("p (g j) -> p g j", g=4),
          oh[:n, 0:4].re("p (g o) -> p g o", o=1).bc([n, 4, 4]),
          q1[:n, 0:4].re("p (o j) -> p o j", o=1).bc([n, 4, 4]), ALU.mult)
    pg = psum()
    kb.tr(pg[:16, :n], gate[:n, 0:16], ident[:n, :n])
    gT = sm()
    kb.cp("dve", gT[:16, :n], pg[:16, :n])
    kb.dma("sp", GT[:, r0:r0 + n], gT[:16, :n])


def ln_ctx(kb, ctx, ins, gname, bname, layer, route=True, trans=False):
    st = ctx["st"]
    ctx["ident"] = ctx["consts"][:, C_IDENT:C_IDENT + 128]
    ctx["g_bc"] = kb.sb(st, [128, D], F32, "g_bc")
    kb.dma("sp", ctx["g_bc"], row_bcast(ins[gname][layer], 128))
    ctx["b_bc"] = kb.sb(st, [128, D], F32, "b_bc")
    kb.dma("sp", ctx["b_bc"], row_bcast(ins[bname][layer], 128))
    ctx["c"] = kb.sb(st, [128, D], F32, "c")
    ctx["col"] = Rot([kb.sb(st, [128, 4], F32, f"col{i}") for i in range(24)])
    if route or trans:
        ctx["x1T"] = kb.sb(st, [128, 16, 128], F32, "x1T")
        ctx["x1Tb"] = kb.sb(st, [128, 16, 128], BF16, "x1Tb")
    if route:
        ctx["wr"] = kb.sb(st, [128, 16, 128], F32, "wr")
        kb.memset("dve", ctx["wr"], 0.0)
        kb.dma("sp", ctx["wr"][:, :, 0:16], ins["w_router"].rearrange("(kc p) e -> p kc e", p=128))
        ctx["sm"] = Rot([kb.sb(st, [128, 128], F32, f"sm{i}") for i in range(8)])


def phase_outln(kb, cfg, ins, src, W, xres, layer, X1, X1T, GT, srcT=None):
    def pre(ctx):
        ln_ctx(kb, ctx, ins, "ln_mix_g", "ln_mix_b", layer)
        ctx["xr"] = [kb.sb(ctx["st"], [128, D], F32, "xr") for _ in range(2)]
        ctx["v"] = kb.sb(ctx["st"], [128, D], F32, "v")
        ctx["ps2"] = Rot([kb.ps(ctx["st"], [128, 512], F32, f"ps2{i}") for i in range(2)])

    def epi(ctx, ts, r0, n, cb, cw, pa):
        xr = ctx["xr"][ts % 2]
        if cb == 0:
            kb.dma("sp", xr[:n, :], xres[r0:r0 + n, :])
        kb.stt("dve", ctx["v"][:n, cb * 512:cb * 512 + cw], xr[:n, cb * 512:cb * 512 + cw], ALPHA, pa[:n, :cw], ALU.mult, ALU.add)

    def post(ctx, ts, r0, n):
        ln_and_route(kb, ctx, cfg, ins, n, r0, ctx["v"], layer, "mix", X1, X1T, GT, ctx["ps2"])

    dense_phase(kb, cfg, ins["consts"], src, W, D, epi, post=post, pre=pre, srcT=srcT)


def inherit(dsts, srcs):
    w = [op for b in srcs for op in b.w]
    r = [op for b in srcs for op in b.r]
    for d in dsts:
        d.w = list(w)
        d.r = list(r)


def phase_moe(kb, cfg, ins, layer, X1, X1T, GT, OUT, OUTT=None):
    NT = cfg.NT
    w_up, w_dn = ins["moe_w_up"][layer], ins["moe_w_down"][layer]
    ntile = (NT + 1095) // 1096
    base = ((NT + ntile - 1) // ntile + 7) // 8 * 8
    tiles = []
    t0 = 0
    while t0 < NT:
        tm = min(base, NT - t0)
        tiles.append((t0, tm))
        t0 += tm
    TMAX = max(tm for _, tm in tiles)
    NSUB = (TMAX + 127) // 128
    with contextlib.ExitStack() as st:
        xT = kb.sb(st, [128, 16, TMAX], BF16, "xT")
        yacc = kb.sb(st, [128, NSUB, D], F32, "yacc")
        gb = kb.sb(st, [128, TMAX], F32, "gbc")
        hhT = kb.sb(st, [128, 8, TMAX], BF16, "hhT")
        arWd = kb.sb(st, [128, 8192], F32, "arWd")
        arW = kb.sb(st, [128, 8192], F32, "arW")
        sA = Rot([kb.sb(st, [128, 512], F32, f"sA{i}") for i in range(2)])
        sB = Rot([kb.sb(st, [128, 512], F32, f"sB{i}") for i in range(2)])
        ident = kb.sb(st, [128, 128], F32, "ident")
        kb.dma("sp", ident, ins["consts"][:, 0:128])
        col = Rot([kb.sb(st, [128, 4], F32, f"col{i}") for i in range(24)])
        psum = Rot([kb.ps(st, [128, 512], F32, f"ps{i}") for i in range(8)])
        Wd = V(arWd.ap.bitcast(BF16).rearrange("p (f n) -> p f n", f=8), Buf("Wd"))
        Wv = [V(arW.ap[:, i * 2048:(i + 1) * 2048].bitcast(BF16).rearrange("p (k n) -> p k n", k=16), Buf(f"W{i}")) for i in range(4)]
        W1 = [Wv[0], Wv[2]]
        W2 = [Wv[1], Wv[3]]
        g_bc = V(arWd.ap[:, 0:2048], Buf("g_bc"))
        b_bc = V(arWd.ap[:, 2048:4096], Buf("b_bc"))
        cc = V(arWd.ap[:, 4096:6144], Buf("c"))
        xr = V(arWd.ap[:, 6144:8192], Buf("xr"))
        x1T = V(arW.ap[:, 0:2048].rearrange("p (k n) -> p k n", k=16), Buf("x1T"))
        x1Tb = V(arW.ap[:, 2048:3072].bitcast(BF16).rearrange("p (k n) -> p k n", k=16), Buf("x1Tb"))
        ctx = {"ident": ident, "g_bc": g_bc, "b_bc": b_bc, "c": cc, "col": col, "x1T": x1T, "x1Tb": x1Tb}
        for (t0, tm) in tiles:
            nsub = (tm + 127) // 128
            cblocks = [(c0, min(512, tm - c0)) for c0 in range(0, tm, 512)]
            inherit([Wd.buf], [g_bc.buf, b_bc.buf, cc.buf, xr.buf])
            inherit([w.buf for w in Wv], [x1T.buf, x1Tb.buf])
            kb.dma("sp", xT[:, :, :tm], X1T[:, :, t0:t0 + tm].rearrange("kc p t -> p kc t"))
            iw = 0
            for e in range(16):
                kb.dma("sp", gb[:, :tm], GT[e:e + 1, t0:t0 + tm].to_broadcast([128, tm]))
                wup = w_up[e].rearrange("(kc p) n -> p kc n", p=128)
                kb.dma("pool", Wd, w_dn[e].rearrange("(fc p) n -> p fc n", p=128))
                for fb in range(4):
                    w1, w2 = W1[iw % 2], W2[iw % 2]
                    iw += 1
                    kb.dma("pool", w1, wup[:, :, fb * 256:(fb + 1) * 256])
                    kb.dma("pool", w2, wup[:, :, 1024 + fb * 256:1024 + (fb + 1) * 256])
                    for f2 in range(2):
                        fc = fb * 2 + f2
                        for (c0, cw) in cblocks:
                            pA = psum(); pB = psum()
                            for kc in range(16):
                                kb.mm(pA[:, :cw], w1[:, kc, f2 * 128:(f2 + 1) * 128], xT[:, kc, c0:c0 + cw], start=(kc == 0), stop=(kc == 15))
                            for kc in range(16):
                                kb.mm(pB[:, :cw], w2[:, kc, f2 * 128:(f2 + 1) * 128], xT[:, kc, c0:c0 + cw], start=(kc == 0), stop=(kc == 15))
                            a = sA(); b = sB()
                            kb.act(a[:, :cw], pA[:, :cw], AF.Silu)
                            kb.tt("dve", b[:, :cw], pB[:, :cw], gb[:, c0:c0 + cw], ALU.mult)
                            kb.tt("dve", hhT[:, fc, c0:c0 + cw], a[:, :cw], b[:, :cw], ALU.mult)
                for su in range(nsub):
                    n = min(128, tm - su * 128)
                    for db in range(4):
                        py = psum()
                        for fc in range(8):
                            kb.mm(py[:n, :], hhT[:, fc, su * 128:su * 128 + n], Wd[:, fc, db * 512:(db + 1) * 512],
                                  start=(fc == 0), stop=(fc == 7))
                        ya = yacc[:n, su, db * 512:(db + 1) * 512]
                        if e == 0:
                            kb.cp("act", ya, py[:n, :])
                        else:
                            kb.tt("dve", ya, ya, py[:n, :], ALU.add)
            inherit([g_bc.buf, b_bc.buf, cc.buf, xr.buf], [Wd.buf])
            inherit([x1T.buf, x1Tb.buf], [w.buf for w in Wv])
            kb.dma("sp", g_bc, row_bcast(ins["ln_ffn_g"][layer], 128))
            kb.dma("sp", b_bc, row_bcast(ins["ln_ffn_b"][layer], 128))
            for su in range(nsub):
                n = min(128, tm - su * 128)
                r0 = t0 + su * 128
                kb.dma("sp", xr[:n, :], X1[r0:r0 + n, :])
                kb.stt("dve", yacc[:n, su, :], xr[:n, :], ALPHA, yacc[:n, su, :], ALU.mult, ALU.add)
                ln_and_route(kb, ctx, cfg, ins, n, r0, yacc[:, su, :], layer, "ffn", OUT, OUTT, None, psum)
        kb.P.flush()


_CACHE = {}


def kernel(**inputs):
    cfg = Cfg()
    if "nc" not in _CACHE:
        _CACHE["nc"] = build(cfg)
    nc, ins, outs = _CACHE["nc"]
    f = lambda k: np.ascontiguousarray(np.asarray(inputs[k], dtype=np.float32))
    consts = make_consts()
    shared = {"consts": consts}
    for k in WEIGHT_SHAPES:
        a = f(k)
        if k.startswith("moe_") or k.startswith("ln_") or k == "w_router":
            shared[k] = a
        else:
            shared[k] = np.ascontiguousarray(a[0])
    xp, xs, meta = f("x_prompt"), f("x_sample"), f("meta")
    in_maps = []
    for c in range(8):
        b = c % 4
        s0 = 16 * c
        m = dict(shared)
        m["xtok"] = np.ascontiguousarray(np.concatenate([meta, xp[b], xs[s0:s0 + 16].reshape(128, D)], 0))
        m["state_gla"] = np.ascontiguousarray(f("state_gla")[0, s0:s0 + 16])
        m["state_rwkv"] = np.ascontiguousarray(f("state_rwkv")[0, s0:s0 + 16])
        m["state_shift"] = np.ascontiguousarray(f("state_shift")[0, s0:s0 + 16])
        m["state_s5_re"] = np.ascontiguousarray(f("state_s5_re")[0, s0:s0 + 16])
        m["state_s5_im"] = np.ascontiguousarray(f("state_s5_im")[0, s0:s0 + 16])
        in_maps.append({k: m[k] for k in ins})
    res = run_bass_kernel_spmd(nc, in_maps, core_ids=list(range(8)))
    R = res.results
    NP = cfg.NP
    y_p = np.stack([R[b]["y"][16:NP] for b in range(4)], 0)
    y_s = np.concatenate([R[c]["y"][NP:].reshape(16, 8, D) for c in range(8)], 0)

    def pr(name):
        return np.stack([R[b][name] for b in range(4)], 0)[None]

    def sm(name):
        return np.concatenate([R[c][name] for c in range(8)], 0)[None]

    return (y_p.astype(np.float32), y_s.astype(np.float32),
            pr("gla_p"), sm("gla_s"), pr("rwkv_p"), sm("rwkv_s"), pr("shift_p"), sm("shift_s"),
            pr("s5re_p"), sm("s5re_s"), pr("s5im_p"), sm("s5im_s"))


def dense_T(kb, cfg, ins, srcT, W, epi, pre=None, TB=512):
    NT = cfg.NT
    with contextlib.ExitStack() as st:
        Wb = kb.sb(st, [128, 16, D], BF16, "Wb")
        Wv = W.rearrange("(kc p) n -> p kc n", p=128)
        for g in range(4):
            kb.dma("pool", Wb[:, 4 * g:4 * g + 4, :], Wv[:, 4 * g:4 * g + 4, :])
        xT = [kb.sb(st, [128, 16, TB], BF16, "xT") for _ in range(2)]
        psum = Rot([kb.ps(st, [128, 512], F32, f"ps{i}") for i in range(8)])
        ctx = {"st": st}
        if pre is not None:
            pre(ctx)
        t0 = 0
        i = 0
        while t0 < NT:
            tb = min(TB, NT - t0)
            x = xT[i % 2]
            kb.dma("sp", x[:, :, :tb], srcT[:, :, t0:t0 + tb].rearrange("kc p t -> p kc t"))
            for cc in range(16):
                pa = psum()
                for kc in range(16):
                    kb.mm(pa[:, :tb], Wb[:, kc, cc * 128:(cc + 1) * 128], x[:, kc, :tb], start=(kc == 0), stop=(kc == 15))
                epi(ctx, cc, t0, tb, pa, x)
            t0 += tb
            i += 1
        kb.P.flush()


def phase_s5(kb, cfg, ins, outs, UT, ZT):
    NT, NP = cfg.NT, cfg.NP
    LMAX = 80 if cfg.NT % 64 == 16 else 64
    with contextlib.ExitStack() as st:
        cs = kb.sb(st, [128, C_TOTAL], F32, "consts")
        kb.dma("sp", cs, ins["consts"])
        ident = cs[:, C_IDENT:C_IDENT + 128]
        pm = cs[:, C_PM:C_PM + 8]
        psum = Rot([kb.ps(st, [128, 512], F32, f"ps{i}") for i in range(8)])

        def pj(name):
            t = kb.sb(st, [128, 64], F32, name)
            kb.dma("sp", t, ins[name].rearrange("(j gl) p -> (gl p) j", gl=2), allow_slow_non_contiguous=True)
            return t

        are, aim = pj("c_a_re"), pj("c_a_im")
        dt = kb.sb(st, [128, 64], F32, "dt")
        ld = ins["c_log_dt"].rearrange("(j gl) -> gl j", gl=2)
        for gl in range(2):
            kb.dma("sp", dt[gl * 64:(gl + 1) * 64, :], ld[gl:gl + 1, :].to_broadcast([64, 64]), allow_slow_non_contiguous=True)
        kb.act(dt, dt, AF.Exp)
        T = [kb.sb(st, [128, 64], F32, f"pt{i}") for i in range(10)]
        er, sn, cn, Are, Aim, x_, den, cre, cim, tmp = T
        kb.tt("dve", tmp, are, dt, ALU.mult)
        kb.act(er, tmp, AF.Exp)
        kb.tt("dve", tmp, aim, dt, ALU.mult)
        kb.act(sn, tmp, AF.Sin, scale=1.0 / 16)
        kb.ts("dve", tmp, tmp, 1.0 / 16, math.pi / 2, ALU.mult, ALU.add)
        kb.act(cn, tmp, AF.Sin)
        for _ in range(4):
            kb.tt("dve", tmp, sn, cn, ALU.mult)
            kb.tt("dve", cn, cn, cn, ALU.mult)
            kb.tt("dve", sn, sn, sn, ALU.mult)
            kb.tt("dve", cn, cn, sn, ALU.subtract)
            kb.ts("dve", sn, tmp, 2.0, None, ALU.mult)
        kb.tt("dve", Are, er, cn, ALU.mult)
        kb.tt("dve", Aim, er, sn, ALU.mult)
        kb.ts("dve", x_, Are, -1.0, None, ALU.add)
        kb.tt("dve", den, are, are, ALU.mult)
        kb.tt("dve", tmp, aim, aim, ALU.mult)
        kb.tt("dve", den, den, tmp, ALU.add)
        kb.recip(den, den)
        kb.tt("dve", cre, x_, are, ALU.mult)
        kb.tt("dve", tmp, Aim, aim, ALU.mult)
        kb.tt("dve", cre, cre, tmp, ALU.add)
        kb.tt("dve", cre, cre, den, ALU.mult)
        kb.tt("dve", cim, Aim, are, ALU.mult)
        kb.tt("dve", tmp, x_, aim, ALU.mult)
        kb.tt("dve", cim, cim, tmp, ALU.subtract)
        kb.tt("dve", cim, cim, den, ALU.mult)
        LB = [kb.sb(st, [128, 16, 128], F32, f"LB{i}") for i in range(2)]
        LC = [kb.sb(st, [128, 16, 128], F32, f"LC{i}") for i in range(2)]
        LB3 = [kb.sb(st, [128, 16, 128], F32, f"LB3{i}") for i in range(2)]
        with contextlib.ExitStack() as st2:
            Bre = kb.sb(st2, [128, 64, 16], F32, "Bre")
            Bim = kb.sb(st2, [128, 64, 16], F32, "Bim")
            kb.dma("sp", Bre, ins["c_b_re"].rearrange("(j gl) p c -> (gl p) j c", gl=2))
            kb.dma("sp", Bim, ins["c_b_im"].rearrange("(j gl) p c -> (gl p) j c", gl=2))
            Xr = kb.sb(st2, [128, 64, 16], F32, "Xr")
            Xi = kb.sb(st2, [128, 64, 16], F32, "Xi")
            t1 = kb.sb(st2, [128, 64, 16], F32, "t1")
            X4 = kb.sb(st2, [128, 64, 2, 16], F32, "X4")
            bc = lambda v: v.re("p (j o) -> p j o", o=1).bc([128, 64, 16])
            kb.tt("dve", Xr, Bre, bc(cre), ALU.mult)
            kb.tt("dve", t1, Bim, bc(cim), ALU.mult)
            kb.tt("dve", Xr, Xr, t1, ALU.subtract)
            kb.tt("dve", Xi, Bim, bc(cre), ALU.mult)
            kb.tt("dve", t1, Bre, bc(cim), ALU.mult)
            kb.tt("dve", Xi, Xi, t1, ALU.add)
            for i, X in enumerate((Xr, Xi)):
                for gl in range(2):
                    kb.ts("dve", X4[:, :, gl, :], X, pm[:, gl:gl + 1], None, ALU.mult)
                for m in range(16):
                    pt = psum()
                    kb.tr(pt[:, 0:128], X4[:, 4 * m:4 * m + 4, :, :].re("p a b c -> p (a b c)"), ident)
                    kb.cp("act" if m % 2 == 0 else "dve", LB[i][:, m, :], pt[:, 0:128])
            for i in range(2):
                kb.ts("dve", LB3[i][64:128, :, :], LB[i][64:128, :, :], pm[64:128, 7:8], None, ALU.mult)
            Rr = kb.sb(st2, [128, 16, 64], F32, "Rr")
            R4 = kb.sb(st2, [128, 16, 2, 64], F32, "R4")
            for i, name in enumerate(("c_c_re", "c_c_im")):
                kb.dma("sp", Rr, ins[name].rearrange("(m qg) c p -> (qg c) m p", m=16))
                for gl in range(2):
                    kb.ts("dve", R4[:, :, gl, :], Rr, pm[:, 2 + gl:3 + gl], (1.0 if i == 0 else -1.0), ALU.mult, ALU.mult)
                for m in range(16):
                    pt = psum()
                    kb.tr(pt[:, 0:128], R4[:, m, :, :].re("p a b -> p (a b)"), ident)
                    kb.cp("act" if m % 2 == 0 else "dve", LC[i][:, m, :], pt[:, 0:128])
            kb.P.flush()
        S5STOP = int(os.environ.get("L1STOP", "99"))
        if S5STOP <= 2:
            return
        Dcol = kb.sb(st, [128, 16], F32, "Dcol")
        kb.dma("sp", Dcol, ins["c_d"].rearrange("(m p) -> p m", p=128), allow_slow_non_contiguous=True)
        HS = [kb.sb(st, [128, 64, LMAX], F32, f"HS{i}") for i in range(2)]
        BUs = [[kb.sb(st, [128, 64, LMAX], F32, f"BU{b}{i}") for i in range(2)] for b in range(2)]
        carry = [kb.sb(st, [128, 64], F32, f"carry{i}") for i in range(2)]
        init = [kb.sb(st, [128, 64], F32, f"init{i}") for i in range(2)]
        sio = [kb.sb(st, [64, 128], F32, f"sio{i}") for i in range(2)]
        sT = [kb.sb(st, [128, 64], F32, f"st{i}") for i in range(4)]
        uT = [kb.sb(st, [128, 16, LMAX], F32, f"uT{i}") for i in range(2)]
        yT = kb.sb(st, [128, LMAX], F32, "yT")
        yq = kb.sb(st, [128, LMAX], F32, "yq")
        zb = Rot([kb.sb(st, [128, LMAX], BF16, f"zb{i}") for i in range(2)])
        starts = {0: -1}
        ends = {NP - 1: -1}
        for sidx in range(cfg.n_samp):
            starts[NP + 8 * sidx] = sidx
            ends[NP + 8 * sidx + 7] = sidx
        blocks = []
        t0 = 0
        while t0 < NT:
            rem = NT - t0
            L = 64 if (rem == 64 or rem - 64 >= 32) else rem
            assert L <= LMAX
            blocks.append((t0, L))
            t0 += L

        def bproj(bi):
            t0, L = blocks[bi]
            u = uT[bi % 2]
            kb.dma("sp", u[:, :, :L], UT[:, :, t0:t0 + L].rearrange("kc p t -> p kc t"))
            ppb = min(8, 512 // L)
            for i in range(2):
                BUv = BUs[bi % 2][i].re("p (m q) t -> p q m t", q=4)
                for q in range(4):
                    m0 = 0
                    while m0 < 16:
                        nm = min(ppb, 16 - m0)
                        pp = psum()
                        for m in range(m0, m0 + nm):
                            osl = pp[:, (m - m0) * L:(m - m0 + 1) * L]
                            if q < 3:
                                kb.mm(osl, LB[i][32 * q:32 * q + 32, m, :], u[32 * q:32 * q + 32, m, :L])
                            else:
                                kb.mm(osl, LB3[i][64:128, m, :], u[64:128, m, :L])
                        kb.cp("act", BUv[:, q, m0:m0 + nm, :L], pp[:, 0:nm * L].re("p (a t) -> p a t", t=L))
                        m0 += nm

        bproj(0)
        for bi, (t0, L) in enumerate(blocks):
            u = uT[bi % 2]
            BU = BUs[bi % 2]
            if bi + 1 < len(blocks):
                bproj(bi + 1)
            for tl in range(L):
                t = t0 + tl
                if t in starts:
                    sidx = starts[t]
                    if sidx < 0:
                        kb.memset("dve", init[0], 0.0)
                        kb.memset("dve", init[1], 0.0)
                    else:
                        for i, nm in enumerate(("state_s5_re", "state_s5_im")):
                            kb.dma("sp", sio[i], ins[nm][sidx].rearrange("(j gl) p -> j (gl p)", gl=2))
                            pt = psum()
                            kb.tr(pt[:, 0:64], sio[i], ident[:64, :64])
                            kb.cp("dve", init[i], pt[:, 0:64])
                    pr, pi = init[0], init[1]
                elif tl == 0:
                    pr, pi = carry[0], carry[1]
                else:
                    pr, pi = HS[0][:, :, tl - 1], HS[1][:, :, tl - 1]
                kb.tt("dve", sT[0], Are, pr, ALU.mult)
                kb.tt("pool", sT[2], Are, pi, ALU.mult)
                kb.tt("dve", sT[1], Aim, pi, ALU.mult)
                kb.tt("pool", sT[3], Aim, pr, ALU.mult)
                kb.tt("dve", sT[0], sT[0], sT[1], ALU.subtract)
                kb.tt("pool", sT[2], sT[2], sT[3], ALU.add)
                kb.tt("dve", HS[0][:, :, tl], sT[0], BU[0][:, :, tl], ALU.add)
                kb.tt("pool", HS[1][:, :, tl], sT[2], BU[1][:, :, tl], ALU.add)
                if t in ends:
                    sidx = ends[t]
                    for i, (pn, sn_) in enumerate((("s5re_p", "s5re_s"), ("s5im_p", "s5im_s"))):
                        pt = psum()
                        kb.tr(pt[:64, 0:128], HS[i][:, :, tl], ident)
                        so = sio[i]
                        kb.cp("dve", so, pt[:64, 0:128])
                        dst = outs[pn] if sidx < 0 else outs[sn_][sidx]
                        kb.dma("sp", dst.rearrange("(j gl) p -> j (gl p)", gl=2), so)
            kb.cp("dve", carry[0], HS[0][:, :, L - 1])
            kb.cp("dve", carry[1], HS[1][:, :, L - 1])
            for m in range(16):
                pps = [psum(), psum()] if 4 * L > 512 else [psum()]
                for q in range(4):
                    pp = pps[q // 2] if len(pps) == 2 else pps[0]
                    off = (q % 2 if len(pps) == 2 else q) * L
                    kb.mm(pp[:, off:off + L], LC[0][:, m, :], HS[0][:, 4 * m + q, :L], start=True, stop=False)
                    kb.mm(pp[:, off:off + L], LC[1][:, m, :], HS[1][:, 4 * m + q, :L], start=False, stop=True)
                    if q == 0:
                        kb.ts("dve", yq[:, :L], pp[:, off:off + L], pm[:, 4:5], None, ALU.mult)
                    else:
                        kb.stt("dve", yq[:, :L], pp[:, off:off + L], pm[:, 4 + q:5 + q], yq[:, :L], ALU.mult, ALU.add)
                kb.stt("dve", yT[:, :L], u[:, m, :L], Dcol[:, m:m + 1], yq[:, :L], ALU.mult, ALU.add)
                z = zb()
                kb.act(z[:, :L], yT[:, :L], AF.Gelu_apprx_tanh)
                kb.dma("sp", ZT[m, :, t0:t0 + L], z[:, :L])
        kb.P.flush()


def phase_l1(kb, cfg, ins, outs, S):
    def pre_a(ctx):
        ctx["ot"] = Rot([kb.sb(ctx["st"], [128, 512], F32, "ot") for _ in range(4)])
        ctx["i"] = 0

    def epi_a(ctx, cc, t0, tb, pa, x):
        ot = ctx["ot"]()
        ctx["i"] += 1
        kb.cp("act" if ctx["i"] % 2 == 0 else "dve", ot[:, :tb], pa[:, :tb])
        kb.dma("sp", S["UT"][cc, :, t0:t0 + tb], ot[:, :tb])

    L1STOP = int(os.environ.get("L1STOP", "99"))
    dense_T(kb, cfg, ins, S["X2T"], ins["od_w_in"], epi_a, pre=pre_a)
    if L1STOP <= 1:
        return
    phase_s5(kb, cfg, ins, outs, S["UT"], S["ZT"])
    if L1STOP <= 5:
        return

    def pre_c(ctx):
        ctx["sg"] = Rot([kb.sb(ctx["st"], [128, 512], F32, "sg") for _ in range(2)])
        ctx["zz"] = Rot([kb.sb(ctx["st"], [128, 512], BF16, "zz") for _ in range(4)])
        ctx["bcol"] = kb.sb(ctx["st"], [128, 16], F32, "bcol")
        kb.dma("sp", ctx["bcol"], ins["c_b_glu"].rearrange("(m p) -> p m", p=128), allow_slow_non_contiguous=True)

    def epi_c(ctx, cc, t0, tb, pa, x):
        sg = ctx["sg"]()
        zz = ctx["zz"]()
        kb.act(sg[:, :tb], pa[:, :tb], AF.Sigmoid, bias=ctx["bcol"][:, cc:cc + 1], scale=1.0)
        kb.tt("dve", zz[:, :tb], x[:, cc, :tb], sg[:, :tb], ALU.mult)
        kb.dma("sp", S["ZZT"][cc, :, t0:t0 + tb], zz[:, :tb])

    dense_T(kb, cfg, ins, S["ZT"], ins["c_w_glu"], epi_c, pre=pre_c)
    phase_outln(kb, cfg, ins, None, ins["od_w_out"], S["X2"], 1, S["X3"], S["X3T"], S["GT3"], srcT=S["ZZT"])
```

```python
import contextlib
import math
import os
import numpy as np
import concourse.bass as bass
import concourse.mybir as mybir
from concourse.bass_utils import run_bass_kernel_spmd

F32 = mybir.dt.float32
BF16 = mybir.dt.bfloat16
AF = mybir.ActivationFunctionType
ALU = mybir.AluOpType
AX = mybir.AxisListType

D = 2048
A_COLS = 3088
B_COLS = 3328
EV_COLS = A_COLS + B_COLS
ALPHA = 4.0 ** 0.25
LN_EPS = 1e-5


class Buf:
    __slots__ = ("name", "w", "r")

    def __init__(self, name=""):
        self.name = name
        self.w = []
        self.r = []


class Op:
    __slots__ = ("eng", "fn", "waits", "signal", "sigval", "is_dma", "dsem", "dval", "epoch")

    def __init__(self, eng, fn, is_dma=False, epoch=0):
        self.eng = eng
        self.fn = fn
        self.waits = []
        self.signal = False
        self.sigval = 0
        self.is_dma = is_dma
        self.dsem = None
        self.dval = 0
        self.epoch = epoch


class Prog:
    ENGS = ("pe", "act", "dve", "pool", "sp")
    ENGOBJ = {"pe": "tensor", "act": "scalar", "dve": "vector", "pool": "gpsimd", "sp": "sync"}

    def __init__(self, nc, stack, dma_slots=None):
        self.nc = nc
        self.ops = {e: [] for e in self.ENGS}
        self.dma_slots = dma_slots or {"sp": 16, "act": 8, "pool": 12}
        self.dma_count = {q: 0 for q in self.dma_slots}
        self.dma_last = {q: [None] * n for q, n in self.dma_slots.items()}
        self.epoch = 0
        self.sigbase = {e: 0 for e in self.ENGS}
        self.known = {e: {} for e in self.ENGS}
        self.esem = {e: stack.enter_context(nc.semaphore(f"s_{e}")) for e in self.ENGS}
        self.dsem = {}
        for q, n in self.dma_slots.items():
            for s in range(n):
                self.dsem[(q, s)] = stack.enter_context(nc.semaphore(f"d_{q}{s}"))

    def _deps(self, rec, reads, writes):
        deps = []
        for b in reads:
            deps.extend(b.w)
        for b in writes:
            for d in b.w:
                if d.is_dma or rec.is_dma or d.eng != rec.eng:
                    deps.append(d)
            for d in b.r:
                if d.is_dma or rec.is_dma or d.eng != rec.eng:
                    deps.append(d)
        seen = set()
        for d in deps:
            if d is rec or id(d) in seen or d.epoch < self.epoch:
                continue
            if (not d.is_dma) and (not rec.is_dma) and d.eng == "pe" and rec.eng == "pe":
                continue
            seen.add(id(d))
            rec.waits.append(d)
            d.signal = True
        for b in reads:
            if not rec.is_dma:
                b.r = [x for x in b.r if x.is_dma or x.eng != rec.eng]
            b.r.append(rec)
        for b in writes:
            b.w = [rec]
            b.r = []

    def op(self, eng, fn, reads=(), writes=()):
        rec = Op(eng, fn, epoch=self.epoch)
        self._deps(rec, reads, writes)
        self.ops[eng].append(rec)
        return rec

    def dma(self, q, out, in_, reads=(), writes=(), **kw):
        rec = Op(q, (out, in_, kw), is_dma=True, epoch=self.epoch)
        i = self.dma_count[q]
        self.dma_count[q] += 1
        K = self.dma_slots[q]
        slot = i % K
        rec.dsem = (q, slot)
        rec.dval = 16 * (i // K + 1)
        prev = self.dma_last[q][slot]
        if prev is not None and prev.epoch == self.epoch:
            rec.waits.append(prev)
        self.dma_last[q][slot] = rec
        self._deps(rec, reads, writes)
        self.ops[q].append(rec)
        return rec

    def flush(self):
        nc = self.nc
        for e in self.ENGS:
            last = None
            for rec in self.ops[e]:
                if not rec.is_dma:
                    last = rec
            if last is not None:
                last.signal = True
            c = self.sigbase[e]
            for rec in self.ops[e]:
                if (not rec.is_dma) and rec.signal:
                    c += 1
                    rec.sigval = c
            self.sigbase[e] = c
        final = {}
        for e in self.ENGS:
            final[e] = self.sigbase[e]
        dfinal = {}
        for q, lst in self.dma_last.items():
            for s, rec in enumerate(lst):
                if rec is not None:
                    dfinal[(q, s)] = rec.dval
        with nc.Block() as block:
            def make(e):
                def body(eng):
                    known = self.known[e]
                    for rec in self.ops[e]:
                        for d in rec.waits:
                            if d.is_dma:
                                key, val, sem = d.dsem, d.dval, self.dsem[d.dsem]
                            else:
                                key, val, sem = d.eng, d.sigval, self.esem[d.eng]
                            if known.get(key, 0) >= val:
                                continue
                            known[key] = val
                            eng.wait_ge(sem, val)
                        if rec.is_dma:
                            out, in_, kw = rec.fn
                            eng.dma_start(out=out, in_=in_, **kw).then_inc(self.dsem[rec.dsem], 16)
                        else:
                            ins = rec.fn(eng)
                            if rec.signal:
                                ins.then_inc(self.esem[e], 1)
                    for k, val in final.items():
                        if val > 0 and known.get(k, 0) < val and k != e:
                            known[k] = val
                            eng.wait_ge(self.esem[k], val)
                    for k, val in dfinal.items():
                        if known.get(k, 0) < val:
                            known[k] = val
                            eng.wait_ge(self.dsem[k], val)
                return body
            for e in self.ENGS:
                getattr(block, self.ENGOBJ[e])(make(e))
        self.ops = {e: [] for e in self.ENGS}
        self.epoch += 1


class V:
    __slots__ = ("ap", "buf")

    def __init__(self, ap, buf):
        self.ap = ap
        self.buf = buf

    def __getitem__(self, idx):
        return V(self.ap[idx], self.buf)

    def re(self, pat, **kw):
        return V(self.ap.rearrange(pat, **kw), self.buf)

    def bc(self, shape):
        return V(self.ap.to_broadcast(list(shape)), self.buf)


class K:
    def __init__(self, nc, stack):
        self.nc = nc
        self.P = Prog(nc, stack)
        self.n = 0

    def sb(self, st, shape, dt=F32, name=None):
        self.n += 1
        h = st.enter_context(self.nc.sbuf_tensor(f"{name or 't'}{self.n}", list(shape), dt))
        return V(h[tuple(slice(None) for _ in shape)], Buf(name or "t"))

    def ps(self, st, shape, dt=F32, name=None):
        self.n += 1
        h = st.enter_context(self.nc.psum_tensor(f"{name or 'p'}{self.n}", list(shape), dt))
        return V(h[tuple(slice(None) for _ in shape)], Buf(name or "p"))

    def dma(self, q, out, in_, **kw):
        outv = out if isinstance(out, V) else V(out, None)
        inv = in_ if isinstance(in_, V) else V(in_, None)
        return self.P.dma(q, outv.ap, inv.ap,
                          reads=[inv.buf] if inv.buf is not None else [],
                          writes=[outv.buf] if outv.buf is not None else [], **kw)

    def mm(self, out, lhsT, rhs, start=True, stop=True):
        self.P.op("pe", lambda e: e.matmul(out.ap, lhsT.ap, rhs.ap, start=start, stop=stop),
                  reads=[lhsT.buf, rhs.buf], writes=[out.buf])

    def tr(self, out, in_, ident):
        self.P.op("pe", lambda e: e.transpose(out.ap, in_.ap, ident.ap),
                  reads=[in_.buf, ident.buf], writes=[out.buf])

    def tt(self, eng, out, a, b, op):
        self.P.op(eng, lambda e: e.tensor_tensor(out.ap, a.ap, b.ap, op),
                  reads=[a.buf, b.buf], writes=[out.buf])

    def ts(self, eng, out, a, s1, s2=None, op0=ALU.mult, op1=None):
        rd = [a.buf]
        s1a = s1.ap if isinstance(s1, V) else s1
        s2a = s2.ap if isinstance(s2, V) else s2
        if isinstance(s1, V):
            rd.append(s1.buf)
        if isinstance(s2, V):
            rd.append(s2.buf)
        if op1 is None:
            self.P.op(eng, lambda e: e.tensor_scalar(out.ap, a.ap, s1a, None, op0), reads=rd, writes=[out.buf])
        else:
            self.P.op(eng, lambda e: e.tensor_scalar(out.ap, a.ap, s1a, s2a, op0, op1), reads=rd, writes=[out.buf])

    def stt(self, eng, out, a, s, b, op0, op1):
        rd = [a.buf, b.buf]
        sa = s.ap if isinstance(s, V) else s
        if isinstance(s, V):
            rd.append(s.buf)
        self.P.op(eng, lambda e: e.scalar_tensor_tensor(out.ap, a.ap, sa, b.ap, op0, op1), reads=rd, writes=[out.buf])

    def act(self, out, a, func, bias=None, scale=None):
        rd = [a.buf]
        kw = {}
        if bias is not None:
            kw["bias"] = bias.ap if isinstance(bias, V) else bias
            if isinstance(bias, V):
                rd.append(bias.buf)
        if scale is not None:
            kw["scale"] = scale.ap if isinstance(scale, V) else scale
            if isinstance(scale, V):
                rd.append(scale.buf)
        self.P.op("act", lambda e: e.activation(out.ap, a.ap, func, **kw), reads=rd, writes=[out.buf])

    def cp(self, eng, out, a):
        if eng == "act":
            self.P.op("act", lambda e: e.copy(out.ap, a.ap), reads=[a.buf], writes=[out.buf])
        else:
            self.P.op(eng, lambda e: e.tensor_copy(out.ap, a.ap), reads=[a.buf], writes=[out.buf])

    def red(self, eng, out, a, op=ALU.add, axis=AX.X):
        self.P.op(eng, lambda e: e.tensor_reduce(out.ap, a.ap, axis, op), reads=[a.buf], writes=[out.buf])

    def recip(self, out, a):
        self.P.op("dve", lambda e: e.reciprocal(out.ap, a.ap), reads=[a.buf], writes=[out.buf])

    def rsqrt(self, out, a, scale, bias):
        self.act(out, a, AF.Sqrt, bias=bias, scale=scale)
        self.recip(out, out)

    def memset(self, eng, out, val):
        self.P.op(eng, lambda e: e.memset(out.ap, val), reads=[], writes=[out.buf])


def row_bcast(ap1d, nparts):
    n = ap1d.shape[-1]
    return ap1d.rearrange("(o n) -> o n", o=1).to_broadcast([nparts, n])


def make_consts():
    c = {}
    c["ident"] = np.eye(128, dtype=np.float32)
    i = np.arange(128)
    c["tri_incl"] = (i[:, None] <= i[None, :]).astype(np.float32)
    c["tri_strict"] = (i[:, None] < i[None, :]).astype(np.float32)
    c["tri_gt"] = (i[:, None] > i[None, :]).astype(np.float32)
    c["ones"] = np.ones((128, 128), dtype=np.float32)
    sel = np.zeros((128, 16, 128), dtype=np.float32)
    for e in range(16):
        sel[e, e, :] = 1.0
    c["sel"] = sel.reshape(128, 16 * 128)
    pm = np.zeros((128, 8), dtype=np.float32)
    for q in range(4):
        pm[32 * q:32 * q + 32, 4 + q] = 1.0
    pm[:64, 0] = 1.0
    pm[64:, 1] = 1.0
    pm[:, 2] = ((i // 16) % 2 == 0)
    pm[:, 3] = ((i // 16) % 2 == 1)
    return np.concatenate([c["ident"], c["tri_incl"], c["tri_strict"], c["tri_gt"], c["ones"], c["sel"], pm], axis=1)


C_IDENT, C_TRII, C_TRIS, C_TRIG, C_ONES, C_SEL = 0, 128, 256, 384, 512, 640
C_PM = 640 + 16 * 128
C_TOTAL = C_PM + 8


class Cfg:
    def __init__(self, n_prompt=2048, n_samp=16, debug=False, phases=None):
        self.n_meta = 16
        self.n_prompt = n_prompt
        self.NP = 16 + n_prompt
        self.n_samp = n_samp
        self.NT = self.NP + 8 * n_samp
        self.debug = debug
        self.phases = phases
        ch = [(0, 16, True, -1)]
        r = 16
        while r < self.NP:
            ch.append((r, 64, False, -1))
            r += 64
        for s in range(n_samp):
            ch.append((self.NP + 8 * s, 8, True, s))
        self.chunks = ch
        self.subtiles = []
        r = 0
        while r < self.NT:
            n = min(128, self.NT - r)
            self.subtiles.append((r, n))
            r += n


def load_consts(kb, st, cst):
    t = kb.sb(st, [128, C_TOTAL], F32, "consts")
    kb.dma("sp", t, cst)
    tb = kb.sb(st, [128, 128], BF16, "identb")
    kb.cp("dve", tb, t[:, C_IDENT:C_IDENT + 128])
    return t, tb


def dense_phase(kb, cfg, cst, src, W, ncols, epi, post=None, pre=None, srcT=None):
    with contextlib.ExitStack() as st:
        consts, identb = load_consts(kb, st, cst)
        KC = 16
        Wb = kb.sb(st, [128, KC, ncols], BF16, "Wb")
        Wv = W.rearrange("(kc p) n -> p kc n", p=128)
        for g in range(4):
            kb.dma("pool", Wb[:, 4 * g:4 * g + 4, :], Wv[:, 4 * g:4 * g + 4, :])
        xs = [kb.sb(st, [128, D], F32, "xs") for _ in range(2)]
        xb = [kb.sb(st, [128, D], BF16, "xb") for _ in range(2)]
        xT = [kb.sb(st, [128, KC, 128], BF16, "xT") for _ in range(2)]
        ptb = [kb.ps(st, [128, KC, 128], BF16, "ptb") for _ in range(1)]
        pacc = [kb.ps(st, [128, 512], F32, "pacc") for _ in range(4)]
        ctx = {"st": st, "consts": consts, "identb": identb}
        if pre is not None:
            pre(ctx)
        ia = 0
        for ts, (r0, n) in enumerate(cfg.subtiles):
            a = ts % 2
            if srcT is not None:
                kb.dma("sp", xT[a][:, :, :n], srcT[:, :, r0:r0 + n].rearrange("kc p t -> p kc t"))
            else:
                kb.dma("sp", xs[a][:n, :], src[r0:r0 + n, :])
                kb.cp("act", xb[a][:n, :], xs[a][:n, :])
                pt = ptb[0]
                for kc in range(KC):
                    kb.tr(pt[:, kc, :n], xb[a][:n, kc * 128:(kc + 1) * 128], identb[:n, :n])
                kb.cp("dve", xT[a][:, 0:8, :n], pt[:, 0:8, :n])
                kb.cp("dve", xT[a][:, 8:16, :n], pt[:, 8:16, :n])
            ncb = (ncols + 511) // 512
            for cb in range(ncb):
                cw = min(512, ncols - cb * 512)
                pa = pacc[ia % 4]
                ia += 1
                for kc in range(KC):
                    kb.mm(pa[:n, :cw], xT[a][:, kc, :n], Wb[:, kc, cb * 512:cb * 512 + cw],
                          start=(kc == 0), stop=(kc == KC - 1))
                epi(ctx, ts, r0, n, cb, cw, pa)
            if post is not None:
                post(ctx, ts, r0, n)
        kb.P.flush()


def phase_inproj(kb, cfg, cst, xtok, w_in, P0):
    for c0 in range(0, EV_COLS, 2048):
        ncols = min(2048, EV_COLS - c0)
        state = {"i": 0}

        def pre(ctx):
            ctx["ot"] = [kb.sb(ctx["st"], [128, 512], F32, "ot") for _ in range(4)]

        def epi(ctx, ts, r0, n, cb, cw, pa, c0=c0, state=state):
            i = state["i"]
            state["i"] += 1
            ot = ctx["ot"][i % 4]
            kb.cp("act" if i % 2 == 0 else "dve", ot[:n, :cw], pa[:n, :cw])
            kb.dma("sp", P0[r0:r0 + n, c0 + cb * 512:c0 + cb * 512 + cw], ot[:n, :cw])

        dense_phase(kb, cfg, cst, xtok, w_in[:, c0:c0 + ncols], ncols, epi, pre=pre)


WEIGHT_SHAPES = {
    "ev_w_in": [D, EV_COLS], "ev_w_out": [D, D], "a_gate_up": [16, 512], "a_gate_b": [512],
    "a_norm_g": [1024], "b_mu": [B_COLS], "b_w0": [1024], "b_w_up": [64, 1024], "b_a0": [1024],
    "b_a_up": [64, 1024], "b_g_up": [128, 1024], "b_k_k": [1024], "b_k_a": [1024], "b_r_k": [1024],
    "b_ln_g": [1024], "b_ln_b": [1024], "od_w_in": [D, D], "c_a_re": [128, 64], "c_a_im": [128, 64],
    "c_log_dt": [128], "c_b_re": [128, 64, 16], "c_b_im": [128, 64, 16], "c_c_re": [128, 16, 64],
    "c_c_im": [128, 16, 64], "c_d": [D], "c_w_glu": [D, D], "c_b_glu": [D], "od_w_out": [D, D],
    "w_router": [D, 16], "moe_w_up": [2, 16, D, D], "moe_w_down": [2, 16, 1024, D],
    "ln_mix_g": [2, D], "ln_mix_b": [2, D], "ln_ffn_g": [2, D], "ln_ffn_b": [2, D],
}


def build(cfg):
    nc = bass.Bass("TRN2", target_bir_lowering=False)
    NT, NS = cfg.NT, cfg.n_samp
    ins = {}

    def inp(name, shape):
        ins[name] = nc.dram_tensor(name, list(shape), F32, kind="ExternalInput").ap()
        return ins[name]

    inp("xtok", [NT, D])
    inp("consts", [128, C_TOTAL])
    inp("state_gla", [NS, 4, 128, 256])
    inp("state_rwkv", [NS, 16, 64, 64])
    inp("state_shift", [NS, B_COLS])
    inp("state_s5_re", [NS, 128, 64])
    inp("state_s5_im", [NS, 128, 64])
    for k, shp in WEIGHT_SHAPES.items():
        if cfg.phases is not None and k.startswith("moe_") and not any(p.startswith("moe") for p in cfg.phases):
            continue
        inp(k, shp)
    outs = {}

    def outp(name, shape):
        outs[name] = nc.dram_tensor(name, list(shape), F32, kind="ExternalOutput").ap()
        return outs[name]

    def scratch(name, shape, dt=F32):
        kind = "ExternalOutput" if cfg.debug else "Internal"
        t = nc.dram_tensor(name, list(shape), dt, kind=kind).ap()
        if cfg.debug:
            outs[name] = t
        return t

    outp("y", [NT, D])
    outp("gla_p", [4, 128, 256]); outp("gla_s", [NS, 4, 128, 256])
    outp("rwkv_p", [16, 64, 64]); outp("rwkv_s", [NS, 16, 64, 64])
    outp("shift_p", [B_COLS]); outp("shift_s", [NS, B_COLS])
    outp("s5re_p", [128, 64]); outp("s5re_s", [NS, 128, 64])
    outp("s5im_p", [128, 64]); outp("s5im_s", [NS, 128, 64])
    S = {}
    S["P0"] = scratch("P0", [NT, EV_COLS])
    S["OM"] = scratch("OM", [NT, D])
    S["X1"] = scratch("X1", [NT, D])
    S["X1T"] = scratch("X1T", [16, 128, NT], BF16)
    S["GT"] = scratch("GT", [16, NT])
    S["X2"] = scratch("X2", [NT, D])
    S["UT"] = scratch("UT", [16, 128, NT])
    S["ZT"] = scratch("ZT", [16, 128, NT], BF16)
    S["X2T"] = scratch("X2T", [16, 128, NT], BF16)
    S["ZZT"] = scratch("ZZT", [16, 128, NT], BF16)
    S["X3"] = scratch("X3", [NT, D])
    S["X3T"] = scratch("X3T", [16, 128, NT], BF16)
    S["GT3"] = scratch("GT3", [16, NT])

    ph = cfg.phases
    with contextlib.ExitStack() as stack:
        kb = K(nc, stack)
        if ph is None or "inproj" in ph:
            phase_inproj(kb, cfg, ins["consts"], ins["xtok"], ins["ev_w_in"], S["P0"])
        if ph is None or "mix0" in ph:
            phase_mix0(kb, cfg, ins, outs, S)
        if ph is None or "out0" in ph:
            phase_outln(kb, cfg, ins, S["OM"], ins["ev_w_out"], ins["xtok"], 0, S["X1"], S["X1T"], S["GT"])
        if ph is None or "moe0" in ph:
            phase_moe(kb, cfg, ins, 0, S["X1"], S["X1T"], S["GT"], S["X2"], S["X2T"])
        if ph is None or "l1" in ph:
            phase_l1(kb, cfg, ins, outs, S)
        if ph is None or "moe1" in ph:
            phase_moe(kb, cfg, ins, 1, S["X3"], S["X3T"], S["GT3"], outs["y"])
    return nc, ins, outs


def phase_l1(*a, **k):
    raise NotImplementedError


class Rot:
    def __init__(self, tiles):
        self.t = tiles
        self.i = 0

    def __call__(self):
        t = self.t[self.i % len(self.t)]
        self.i += 1
        return t


def phase_mix0(kb, cfg, ins, outs, S):
    RSTOP = int(os.environ.get('RSTOP', '99'))
    P0, OM = S["P0"], S["OM"]
    C0 = math.exp(-0.5)
    with contextlib.ExitStack() as st:
        cs = kb.sb(st, [128, 640], F32, "consts")
        kb.dma("sp", cs, ins["consts"][:, 0:640])
        ident = cs[:, C_IDENT:C_IDENT + 128]
        tri_i = cs[:, C_TRII:C_TRII + 128]
        tri_s = cs[:, C_TRIS:C_TRIS + 128]
        tri_g = cs[:, C_TRIG:C_TRIG + 128]
        ones = cs[:, C_ONES:C_ONES + 128]

        def bcast(name, n, parts=64):
            t = kb.sb(st, [parts, n], F32, name)
            kb.dma("sp", t, row_bcast(ins[name], parts))
            return t

        mu_bc = bcast("b_mu", B_COLS)
        kk_bc = bcast("b_k_k", 1024)
        ka_bc = bcast("b_k_a", 1024)
        rk_bc = bcast("b_r_k", 1024)
        lng_bc = bcast("b_ln_g", 1024)
        lnb_bc = bcast("b_ln_b", 1024)
        ng_bc = bcast("a_norm_g", 1024)
        w0_row = bcast("b_w0", 1024, 1)
        a0_row = bcast("b_a0", 1024, 1)
        gb_row = bcast("a_gate_b", 512, 1)
        w_up = kb.sb(st, [64, 1024], F32, "w_up"); kb.dma("sp", w_up, ins["b_w_up"])
        a_up = kb.sb(st, [64, 1024], F32, "a_up"); kb.dma("sp", a_up, ins["b_a_up"])
        g_up = kb.sb(st, [128, 1024], F32, "g_up"); kb.dma("sp", g_up, ins["b_g_up"])
        gate_up = kb.sb(st, [16, 512], F32, "gate_up"); kb.dma("sp", gate_up, ins["a_gate_up"])

        pa = kb.sb(st, [64, A_COLS], F32, "pa")
        pb = kb.sb(st, [64, B_COLS], F32, "pb")
        xm = kb.sb(st, [64, B_COLS], F32, "xm")
        omix = kb.sb(st, [64, D], F32, "omix")
        Sg = kb.sb(st, [128, 4, 256], F32, "Sg")
        M = kb.sb(st, [64, 16, 64], F32, "M")
        Mio = kb.sb(st, [64, 16, 64], F32, "Mio")
        W = [kb.sb(st, [64, 512], F32, f"w{i}") for i in range(16)]
        XT = [kb.sb(st, [64, 8, 64], F32, f"xt{i}") for i in range(4)]
        AM = [kb.sb(st, [64, 8, 64], F32, f"am{i}") for i in range(7)]
        small = Rot([kb.sb(st, [128, 256], F32, f"sm{i}") for i in range(10)])
        col = Rot([kb.sb(st, [128, 16], F32, f"col{i}") for i in range(12)])
        psum = Rot([kb.ps(st, [128, 512], F32, f"ps{i}") for i in range(8)])

        def h3(v, C):
            return v.re("p (h k) -> p h k", h=8)

        def mask3(m, C):
            return m[:C, :C].re("p (o c) -> p o c", o=1).bc([C, 8, C])

        for (r0, C, seq_start, sidx) in cfg.chunks:
            last = (r0 + C == cfg.NP) if sidx < 0 else True
            kb.dma("sp", pa[:C, :], P0[r0:r0 + C, 0:A_COLS])
            kb.dma("sp", pb[:C, :], P0[r0:r0 + C, A_COLS:EV_COLS])
            if seq_start:
                if sidx < 0:
                    kb.memset("dve", xm[0:1, :], 0.0)
                    kb.memset("dve", Sg, 0.0)
                    kb.memset("dve", M, 0.0)
                else:
                    kb.dma("sp", xm[0:1, :], ins["state_shift"][sidx:sidx + 1, :])
                    kb.dma("sp", Sg, ins["state_gla"][sidx].rearrange("h k v -> k h v"))
                    kb.dma("sp", Mio, ins["state_rwkv"][sidx].rearrange("h v k -> v h k"))
                    for hh in range(2):
                        pt = psum()
                        for h in range(8):
                            kb.tr(pt[:64, h * 64:(h + 1) * 64], Mio[:, hh * 8 + h, :], ident[:64, :64])
                        kb.cp("dve", M[:, hh * 8:hh * 8 + 8, :], pt[:64, :].re("p (h v) -> p h v", h=8))
                if C > 1:
                    kb.dma("sp", xm[1:C, :], P0[r0:r0 + C - 1, A_COLS:EV_COLS])
            else:
                kb.dma("sp", xm[:C, :], P0[r0 - 1:r0 + C - 1, A_COLS:EV_COLS])

            pt = psum()
            kb.tr(pt[:16, :C], pa[:C, 3072:3088], ident[:C, :C])
            gdT = small()
            kb.cp("dve", gdT[:16, :C], pt[:16, :C])
            pg = psum()
            kb.mm(pg[:C, :512], gdT[:16, :C], gate_up[:16, :], start=True, stop=False)
            kb.mm(pg[:C, :512], ones[0:1, :C], gb_row[0:1, :], start=False, stop=True)
            lgp = W[0]
            kb.act(lgp[:C, :], pg[:C, :512], AF.Exp, scale=-1.0)
            kb.act(lgp[:C, :], lgp[:C, :], AF.Ln, bias=1.0)
            for h in range(4 if os.environ.get('NOGLA') is None else 0):
                lgh = lgp[:C, h * 128:(h + 1) * 128]
                q_tok = pa[:C, h * 128:(h + 1) * 128]
                k_tok = pa[:C, 512 + h * 128:512 + (h + 1) * 128]
                v_tok = pa[:C, 1024 + h * 256:1024 + (h + 1) * 256]
                r_tok = pa[:C, 2048 + h * 256:2048 + (h + 1) * 256]
                pbT = psum()
                kb.mm(pbT[:, :C], lgh, tri_i[:C, :C])
                eqT = small(); ekT = small()
                kb.act(eqT[:, :C], pbT[:, :C], AF.Exp, scale=-1.0 / 16)
                kb.act(ekT[:, :C], pbT[:, :C], AF.Exp, scale=1.0 / 16)
                pq = psum()
                kb.tr(pq[:, :C], q_tok, ident[:C, :C])
                kb.tr(pq[:, 128:128 + C], k_tok, ident[:C, :C])
                qtT = small(); ktT = small()
                kb.stt("dve", qtT[:, :C], pq[:, :C], 128.0 ** -0.5, eqT[:, :C], ALU.mult, ALU.mult)
                kb.tt("dve", ktT[:, :C], pq[:, 128:128 + C], ekT[:, :C], ALU.mult)
                patt = psum()
                kb.mm(patt[:C, :C], ktT[:, :C], qtT[:, :C])
                attm = small()
                kb.tt("dve", attm[:C, :C], patt[:C, :C], tri_i[:C, :C], ALU.mult)
                pdl = psum()
                kb.mm(pdl[:C, :128], tri_g[:C, :C], lgh)
                khat = small()
                kb.act(khat[:C, :128], pdl[:C, :128], AF.Exp, scale=-1.0 / 16)
                kb.tt("dve", khat[:C, :128], khat[:C, :128], k_tok, ALU.mult)
                po = psum()
                kb.mm(po[:C, :256], attm[:C, :C], v_tok, start=True, stop=False)
                kb.mm(po[:C, :256], qtT[:, :C], Sg[:, h, :], start=False, stop=True)
                pS = psum()
                kb.mm(pS[:, :256], khat[:C, :128], v_tok)
                kb.stt("dve", Sg[:, h, :], Sg[:, h, :], eqT[:, C - 1:C], pS[:, :256], ALU.mult, ALU.add)
                sq = small(); ssq = col()
                kb.act(sq[:C, :], po[:C, :256], AF.Square)
                kb.red("dve", ssq[:C, 0:1], sq[:C, :])
                kb.rsqrt(ssq[:C, 0:1], ssq[:C, 0:1], 1.0 / 256, 1e-5)
                og = small(); sr = small()
                kb.stt("dve", og[:C, :], po[:C, :256], ssq[:C, 0:1], ng_bc[:C, h * 256:(h + 1) * 256], ALU.mult, ALU.mult)
                kb.act(sr[:C, :], r_tok, AF.Silu)
                kb.tt("dve", omix[:C, h * 256:(h + 1) * 256], og[:C, :], sr[:C, :], ALU.mult)
            if last:
                dst = outs["gla_p"] if sidx < 0 else outs["gla_s"][sidx]
                kb.dma("sp", dst.rearrange("h k v -> k h v"), Sg)

            kb.tt("dve", xm[:C, :], xm[:C, :], pb[:C, :], ALU.subtract)
            kb.tt("dve", xm[:C, :], xm[:C, :], mu_bc[:C, :], ALU.mult)
            kb.tt("dve", xm[:C, :], xm[:C, :], pb[:C, :], ALU.add)
            twd = small(); sgd = small()
            kb.act(twd[:C, :64], xm[:C, 3072:3136], AF.Tanh)
            kb.act(sgd[:C, :128], xm[:C, 3200:3328], AF.Sigmoid)
            pt = psum()
            kb.tr(pt[:64, 0:C], twd[:C, :64], ident[:C, :C])
            kb.tr(pt[:64, 64:64 + C], xm[:C, 3136:3200], ident[:C, :C])
            kb.tr(pt[:128, 128:128 + C], sgd[:C, :128], ident[:C, :C])
            tT = small()
            kb.cp("dve", tT[:64, 0:128], pt[:64, 0:128])
            kb.cp("dve", tT[:128, 128:128 + C], pt[:128, 128:128 + C])
            twdT = tT[:64, 0:C]; adT = tT[:64, 64:64 + C]; sgdT = tT[:128, 128:128 + C]
            for hh in range(2 if os.environ.get('NORWKV') is None else 0):
                hs = hh * 512
                r = xm[:C, hs:hs + 512]
                k = xm[:C, 1024 + hs:1024 + hs + 512]
                v = xm[:C, 2048 + hs:2048 + hs + 512]
                wl, a, g, kk, kmod, b, Gs, rt, eGn, nbt, kt, kap, oc, tmp1, tmp2, tmp3 = [w[:C, :] for w in W]
                pz = psum()
                kb.mm(pz[:C, :], twdT, w_up[:64, hs:hs + 512], start=True, stop=False)
                kb.mm(pz[:C, :], ones[0:1, :C], w0_row[0:1, hs:hs + 512], start=False, stop=True)
                kb.act(wl, pz[:C, :], AF.Sigmoid)
                pz = psum()
                kb.mm(pz[:C, :], adT, a_up[:64, hs:hs + 512], start=True, stop=False)
                kb.mm(pz[:C, :], ones[0:1, :C], a0_row[0:1, hs:hs + 512], start=False, stop=True)
                kb.act(a, pz[:C, :], AF.Sigmoid)
                pz = psum()
                kb.mm(pz[:C, :], sgdT, g_up[:128, hs:hs + 512])
                kb.cp("act", g, pz[:C, :])
                if RSTOP <= 1:
                    continue
                kb.tt("dve", kk, k, kk_bc[:C, hs:hs + 512], ALU.mult)
                kb.tt("dve", tmp1, kk, kk, ALU.mult)
                ss = col()
                kb.red("dve", ss[:C, 0:8], h3(tmp1, C))
                kb.act(ss[:C, 0:8], ss[:C, 0:8], AF.Sqrt)
                kb.ts("dve", ss[:C, 0:8], ss[:C, 0:8], 1e-12, None, ALU.max)
                kb.recip(ss[:C, 0:8], ss[:C, 0:8])
                kb.tt("dve", h3(kk, C), h3(kk, C), ss[:C, 0:8].re("p (h o) -> p h o", o=1).bc([C, 8, 64]), ALU.mult)
                kb.stt("dve", tmp1, a, -1.0, ka_bc[:C, hs:hs + 512], ALU.add, ALU.mult)
                kb.stt("dve", kmod, tmp1, 1.0, k, ALU.add, ALU.mult)
                kb.tt("dve", b, kk, a, ALU.mult)
                if RSTOP <= 2:
                    continue
                pG = psum()
                kb.mm(pG[:C, :], tri_i[:C, :C], wl)
                kb.cp("act", Gs, pG[:C, :])
                kb.act(rt, Gs, AF.Exp, scale=-C0)
                kb.act(eGn, Gs, AF.Exp, scale=C0)
                kb.tt("dve", tmp1, Gs, wl, ALU.subtract)
                kb.act(kap, tmp1, AF.Exp, scale=-C0)
                kb.tt("dve", kap, kap, kk, ALU.mult)
                kb.tt("dve", rt, rt, r, ALU.mult)
                kb.tt("dve", b, b, eGn, ALU.mult)
                kb.ts("dve", nbt, b, -1.0, None, ALU.mult)
                kb.tt("dve", kt, kmod, eGn, ALU.mult)
                if RSTOP <= 3:
                    continue
                for i, src in enumerate((kap, b, kt, rt)):
                    pt = psum()
                    for h in range(8):
                        kb.tr(pt[:64, h * 64:h * 64 + C], src[:, h * 64:(h + 1) * 64], ident[:C, :C])
                    kb.cp("act" if i % 2 == 0 else "dve", XT[i][:, :, :C],
                          pt[:64, :].re("p (h c) -> p h c", h=8)[:, :, :C])
                if RSTOP <= 4:
                    continue
                kapT, btT, ktT, rtT = XT
                AT, A, A2T, A3T, A4T, Q0, Q1 = AM

                def amat(dst, lT, rT, mask, neg=False):
                    pp = psum()
                    for h in range(8):
                        kb.mm(pp[:C, h * 64:h * 64 + C], lT[:, h, :C], rT[:, h, :C])
                    src = pp[:C, :].re("p (h c) -> p h c", h=8)[:, :, :C]
                    if neg:
                        kb.stt("dve", dst[:C, :, :C], src, -1.0, mask3(mask, C), ALU.mult, ALU.mult)
                    else:
                        kb.tt("dve", dst[:C, :, :C], src, mask3(mask, C), ALU.mult)

                amat(AT, btT, kapT, tri_s)
                amat(A, kapT, btT, tri_g)
                amat(A2T, ktT, kapT, tri_s)
                amat(A3T, ktT, rtT, tri_i)
                amat(A4T, btT, rtT, tri_i, neg=True)
                if RSTOP <= 5:
                    continue
                pW = psum()
                for h in range(8):
                    kb.mm(pW[:C, h * 64:(h + 1) * 64], kapT[:, h, :C], M[:, hh * 8 + h, :], start=True, stop=False)
                    kb.mm(pW[:C, h * 64:(h + 1) * 64], A2T[:C, h, :C], v[:, h * 64:(h + 1) * 64], start=False, stop=True)
                U = tmp2
                kb.cp("act", U, pW[:C, :])
                if RSTOP <= 6:
                    continue
                curP, curPT = A, AT
                nxt = [(Q0, Q1), (A2T, A), (Q0, Q1), (A2T, A), (Q0, Q1)]
                n = 0
                while True:
                    pU = psum()
                    for h in range(8):
                        kb.mm(pU[:C, h * 64:(h + 1) * 64], curPT[:C, h, :C], U[:, h * 64:(h + 1) * 64])
                    kb.tt("dve", U, U, pU[:C, :], ALU.subtract if n == 0 else ALU.add)
                    if (2 << n) >= C:
                        break
                    nP, nPT = nxt[n]
                    if n == 1:
                        nP, nPT = A2T, AT
                    p1 = psum(); p2 = psum()
                    for h in range(8):
                        kb.mm(p1[:C, h * 64:h * 64 + C], curPT[:C, h, :C], curP[:C, h, :C])
                        kb.mm(p2[:C, h * 64:h * 64 + C], curP[:C, h, :C], curPT[:C, h, :C])
                    tgt = [t for t in (Q0, Q1, A2T, A, AT) if t is not curP and t is not curPT][:2]
                    nP, nPT = tgt
                    kb.cp("act", nP[:C, :, :C], p1[:C, :].re("p (h c) -> p h c", h=8)[:, :, :C])
                    kb.cp("dve", nPT[:C, :, :C], p2[:C, :].re("p (h c) -> p h c", h=8)[:, :, :C])
                    curP, curPT = nP, nPT
                    n += 1
                if RSTOP <= 7:
                    continue
                pO = psum()
                for h in range(8):
                    sl = slice(h * 64, (h + 1) * 64)
                    kb.mm(pO[:C, sl], rtT[:, h, :C], M[:, hh * 8 + h, :], start=True, stop=False)
                    kb.mm(pO[:C, sl], A3T[:C, h, :C], v[:, sl], start=False, stop=False)
                    kb.mm(pO[:C, sl], A4T[:C, h, :C], U[:, sl], start=False, stop=True)
                if RSTOP <= 8:
                    continue
                pM = psum()
                for h in range(8):
                    sl = slice(h * 64, (h + 1) * 64)
                    kb.mm(pM[:64, sl], kt[:, sl], v[:, sl], start=True, stop=False)
                    kb.mm(pM[:64, sl], nbt[:, sl], U[:, sl], start=False, stop=True)
                pGm = psum()
                for h in range(8):
                    kb.mm(pGm[:64, 64 * h:64 * h + 64], wl[:, h * 64:(h + 1) * 64], ones[:C, 0:64])
                gam = col()
                kb.act(gam[:64, 0:8], pGm[:64, 0:512].re('p (h t) -> p h t', t=64)[:, :, 0], AF.Exp, scale=-C0)
                Mh = M[:, hh * 8:hh * 8 + 8, :]
                kb.tt("dve", Mh, Mh, pM[:64, :].re("p (h v) -> p h v", h=8), ALU.add)
                kb.tt("dve", Mh, Mh, gam[:64, 0:8].re("p (h o) -> p h o", o=1).bc([64, 8, 64]), ALU.mult)
                if RSTOP <= 9:
                    continue
                kb.cp("act", oc, pO[:C, :])
                mean = col()
                kb.red("dve", mean[:C, 0:8], h3(oc, C))
                kb.ts("dve", mean[:C, 0:8], mean[:C, 0:8], -1.0 / 64, None, ALU.mult)
                kb.tt("dve", h3(oc, C), h3(oc, C), mean[:C, 0:8].re("p (h o) -> p h o", o=1).bc([C, 8, 64]), ALU.add)
                if RSTOP <= 10:
                    continue
                kb.tt("dve", tmp1, oc, oc, ALU.mult)
                var = col()
                kb.red("dve", var[:C, 0:8], h3(tmp1, C))
                kb.rsqrt(var[:C, 0:8], var[:C, 0:8], 1.0 / 64, 64e-5)
                kb.tt("dve", h3(oc, C), h3(oc, C), var[:C, 0:8].re("p (h o) -> p h o", o=1).bc([C, 8, 64]), ALU.mult)
                if RSTOP <= 11:
                    continue
                kb.tt("dve", oc, oc, lng_bc[:C, hs:hs + 512], ALU.mult)
                kb.tt("dve", oc, oc, lnb_bc[:C, hs:hs + 512], ALU.add)
                if RSTOP <= 12:
                    continue
                kb.tt("dve", tmp1, r, kmod, ALU.mult)
                kb.tt("dve", tmp1, tmp1, rk_bc[:C, hs:hs + 512], ALU.mult)
                if RSTOP <= 13:
                    continue
                bon = col()
                kb.red("dve", bon[:C, 0:8], h3(tmp1, C))
                kb.tt("dve", h3(tmp1, C), h3(v, C), bon[:C, 0:8].re("p (h o) -> p h o", o=1).bc([C, 8, 64]), ALU.mult)
                if RSTOP <= 14:
                    continue
                kb.tt("dve", oc, oc, tmp1, ALU.add)
                if RSTOP <= 15:
                    continue
                kb.tt("dve", omix[:C, 1024 + hs:1024 + hs + 512], oc, g, ALU.mult)
            kb.dma("sp", OM[r0:r0 + C, :], omix[:C, :])
            if last:
                for hh in range(2):
                    pt = psum()
                    for h in range(8):
                        kb.tr(pt[:64, h * 64:(h + 1) * 64], M[:, hh * 8 + h, :], ident[:64, :64])
                    kb.cp("dve", Mio[:, hh * 8:hh * 8 + 8, :], pt[:64, :].re("p (h v) -> p h v", h=8))
                dst = outs["rwkv_p"] if sidx < 0 else outs["rwkv_s"][sidx]
                kb.dma("sp", dst.rearrange("h v k -> v h k"), Mio)
                lastrow = r0 + C - 1
                dsts = outs["shift_p"] if sidx < 0 else outs["shift_s"][sidx]
                kb.dma("sp", dsts.rearrange("(o n) -> o n", o=1), P0[lastrow:lastrow + 1, A_COLS:EV_COLS])
        kb.P.flush()


def ln_and_route(kb, ctx, cfg, ins, n, r0, v, layer, which, X1, X1T, GT, psum):
    col, c, ident, g_bc, b_bc = ctx["col"], ctx["c"], ctx["ident"], ctx["g_bc"], ctx["b_bc"]
    s1 = col()
    kb.red("dve", s1[:n, 0:1], v[:n, :])
    kb.ts("dve", s1[:n, 0:1], s1[:n, 0:1], -1.0 / D, None, ALU.mult)
    kb.ts("dve", v[:n, :], v[:n, :], s1[:n, 0:1], None, ALU.add)
    kb.act(c[:n, :], v[:n, :], AF.Square)
    s2 = col()
    kb.red("dve", s2[:n, 0:1], c[:n, :])
    kb.rsqrt(s2[:n, 0:1], s2[:n, 0:1], 1.0 / D, LN_EPS)
    kb.stt("dve", v[:n, :], v[:n, :], s2[:n, 0:1], g_bc[:n, :], ALU.mult, ALU.mult)
    kb.tt("dve", v[:n, :], v[:n, :], b_bc[:n, :], ALU.add)
    kb.dma("sp", X1[r0:r0 + n, :], v[:n, :])
    if X1T is None or os.environ.get('NOROUTE'):
        return
    x1T, x1Tb, wr = ctx["x1T"], ctx["x1Tb"], ctx.get("wr")
    for grp in range(4):
        pt = psum()
        for j in range(4):
            kc = grp * 4 + j
            kb.tr(pt[:, j * 128:j * 128 + n], v[:n, kc * 128:(kc + 1) * 128], ident[:n, :n])
        src = pt.re("p (j t) -> p j t", j=4)[:, :, :n]
        kb.cp("act", x1T[:, grp * 4:grp * 4 + 4, :n], src)
        kb.cp("dve", x1Tb[:, grp * 4:grp * 4 + 4, :n], x1T[:, grp * 4:grp * 4 + 4, :n])
    if not os.environ.get('NOX1T'):
        kb.dma("sp", X1T[:, :, r0:r0 + n].rearrange("kc p t -> p kc t"), x1Tb[:, :, :n])
    if GT is None:
        return
    pl = psum()
    for kc in range(16):
        kb.mm(pl[:n, 0:128], x1T[:, kc, :n], wr[:, kc, :], start=(kc == 0), stop=(kc == 15))
    if os.environ.get('NOGATE'):
        return
    sm = ctx["sm"]
    l = sm(); e = sm(); t3 = sm(); t4 = sm(); gate = sm()
    BIG = 1e30
    kb.cp("dve", l[:n, 0:16], pl[:n, 0:16])
    mx = col()
    kb.red("dve", mx[:n, 0:1], l[:n, 0:16], op=ALU.max)
    kb.ts("dve", mx[:n, 0:1], mx[:n, 0:1], -1.0, None, ALU.mult)
    kb.act(e[:n, 0:16], l[:n, 0:16], AF.Exp, bias=mx[:n, 0:1], scale=1.0)
    e3 = e[:n, 0:16].re("p (g j) -> p g j", g=4)
    m1 = col(); m2 = col(); sc = col(); oh = col()
    kb.red("dve", m1[:n, 0:4], e3, op=ALU.max)
    t33 = t3[:n, 0:16].re("p (g j) -> p g j", g=4)
    kb.tt("dve", t33, e3, m1[:n, 0:4].re("p (g o) -> p g o", o=1).bc([n, 4, 4]), ALU.is_equal)
    kb.stt("dve", t33, t33, -BIG, e3, ALU.mult, ALU.add)
    kb.red("dve", m2[:n, 0:4], t33, op=ALU.max)
    kb.tt("dve", sc[:n, 0:4], m1[:n, 0:4], m2[:n, 0:4], ALU.add)
    gm = col()
    kb.red("dve", gm[:n, 0:1], sc[:n, 0:4], op=ALU.max)
    kb.ts("dve", oh[:n, 0:4], sc[:n, 0:4], gm[:n, 0:1], None, ALU.is_equal)
    kb.tt("dve", t33, e3, oh[:n, 0:4].re("p (g o) -> p g o", o=1).bc([n, 4, 4]), ALU.mult)
    ing = col()
    kb.red("dve", ing[:n, 0:4], t3[:n, 0:16].re("p (g j) -> p j g", g=4))
    v1 = col(); v2 = col(); q1 = col(); q2 = col(); i2 = col()
    kb.red("dve", v1[:n, 0:1], ing[:n, 0:4], op=ALU.max)
    kb.ts("dve", q1[:n, 0:4], ing[:n, 0:4], v1[:n, 0:1], None, ALU.is_equal)
    kb.stt("dve", i2[:n, 0:4], q1[:n, 0:4], -BIG, ing[:n, 0:4], ALU.mult, ALU.add)
    kb.red("dve", v2[:n, 0:1], i2[:n, 0:4], op=ALU.max)
    kb.ts("dve", q2[:n, 0:4], i2[:n, 0:4], v2[:n, 0:1], None, ALU.is_equal)
    kb.tt("dve", q1[:n, 0:4], q1[:n, 0:4], q2[:n, 0:4], ALU.add)
    kb.tt("dve", v1[:n, 0:1], v1[:n, 0:1], v2[:n, 0:1], ALU.add)
    kb.recip(v1[:n, 0:1], v1[:n, 0:1])
    kb.tt("dve", q1[:n, 0:4], q1[:n, 0:4], ing[:n, 0:4], ALU.mult)
    kb.ts("dve", q1[:n, 0:4], q1[:n, 0:4], v1[:n, 0:1], None, ALU.mult)
    kb.tt("dve", gate[:n, 0:16].re("p (g j) -> p g j", g=4),
          oh[:n, 0:4].re("p (g o) -> p g o", o=1).bc([n, 4, 4]),
          q1[:n, 0:4].re("p (o j) -> p o j", o=1).bc([n, 4, 4]), ALU.mult)
    pg = psum()
    kb.tr(pg[:16, :n], gate[:n, 0:16], ident[:n, :n])
    gT = sm()
    kb.cp("dve", gT[:16, :n], pg[:16, :n])
    kb.dma("sp", GT[:, r0:r0 + n], gT[:16, :n])


def ln_ctx(kb, ctx, ins, gname, bname, layer, route=True, trans=False):
    st = ctx["st"]
    ctx["ident"] = ctx["consts"][:, C_IDENT:C_IDENT + 128]
    ctx["g_bc"] = kb.sb(st, [128, D], F32, "g_bc")
    kb.dma("sp", ctx["g_bc"], row_bcast(ins[gname][layer], 128))
    ctx["b_bc"] = kb.sb(st, [128, D], F32, "b_bc")
    kb.dma("sp", ctx["b_bc"], row_bcast(ins[bname][layer], 128))
    ctx["c"] = kb.sb(st, [128, D], F32, "c")
    ctx["col"] = Rot([kb.sb(st, [128, 4], F32, f"col{i}") for i in range(24)])
    if route or trans:
        ctx["x1T"] = kb.sb(st, [128, 16, 128], F32, "x1T")
        ctx["x1Tb"] = kb.sb(st, [128, 16, 128], BF16, "x1Tb")
    if route:
        ctx["wr"] = kb.sb(st, [128, 16, 128], F32, "wr")
        kb.memset("dve", ctx["wr"], 0.0)
        kb.dma("sp", ctx["wr"][:, :, 0:16], ins["w_router"].rearrange("(kc p) e -> p kc e", p=128))
        ctx["sm"] = Rot([kb.sb(st, [128, 128], F32, f"sm{i}") for i in range(8)])


def phase_outln(kb, cfg, ins, src, W, xres, layer, X1, X1T, GT, srcT=None):
    def pre(ctx):
        ln_ctx(kb, ctx, ins, "ln_mix_g", "ln_mix_b", layer)
        ctx["xr"] = [kb.sb(ctx["st"], [128, D], F32, "xr") for _ in range(2)]
        ctx["v"] = kb.sb(ctx["st"], [128, D], F32, "v")
        ctx["ps2"] = Rot([kb.ps(ctx["st"], [128, 512], F32, f"ps2{i}") for i in range(2)])

    def epi(ctx, ts, r0, n, cb, cw, pa):
        xr = ctx["xr"][ts % 2]
        if cb == 0:
            kb.dma("sp", xr[:n, :], xres[r0:r0 + n, :])
        kb.stt("dve", ctx["v"][:n, cb * 512:cb * 512 + cw], xr[:n, cb * 512:cb * 512 + cw], ALPHA, pa[:n, :cw], ALU.mult, ALU.add)

    def post(ctx, ts, r0, n):
        ln_and_route(kb, ctx, cfg, ins, n, r0, ctx["v"], layer, "mix", X1, X1T, GT, ctx["ps2"])

    dense_phase(kb, cfg, ins["consts"], src, W, D, epi, post=post, pre=pre, srcT=srcT)


def inherit(dsts, srcs):
    w = [op for b in srcs for op in b.w]
    r = [op for b in srcs for op in b.r]
    for d in dsts:
        d.w = list(w)
        d.r = list(r)


def phase_moe(kb, cfg, ins, layer, X1, X1T, GT, OUT, OUTT=None):
    NT = cfg.NT
    w_up, w_dn = ins["moe_w_up"][layer], ins["moe_w_down"][layer]
    ntile = (NT + 1095) // 1096
    base = ((NT + ntile - 1) // ntile + 7) // 8 * 8
    tiles = []
    t0 = 0
    while t0 < NT:
        tm = min(base, NT - t0)
        tiles.append((t0, tm))
        t0 += tm
    TMAX = max(tm for _, tm in tiles)
    NSUB = (TMAX + 127) // 128
    with contextlib.ExitStack() as st:
        xT = kb.sb(st, [128, 16, TMAX], BF16, "xT")
        yacc = kb.sb(st, [128, NSUB, D], F32, "yacc")
        gb = kb.sb(st, [128, TMAX], F32, "gbc")
        hhT = kb.sb(st, [128, 8, TMAX], BF16, "hhT")
        arWd = kb.sb(st, [128, 8192], F32, "arWd")
        arW = kb.sb(st, [128, 8192], F32, "arW")
        sA = Rot([kb.sb(st, [128, 512], F32, f"sA{i}") for i in range(2)])
        sB = Rot([kb.sb(st, [128, 512], F32, f"sB{i}") for i in range(2)])
        ident = kb.sb(st, [128, 128], F32, "ident")
        kb.dma("sp", ident, ins["consts"][:, 0:128])
        col = Rot([kb.sb(st, [128, 4], F32, f"col{i}") for i in range(24)])
        psum = Rot([kb.ps(st, [128, 512], F32, f"ps{i}") for i in range(8)])
        Wd = V(arWd.ap.bitcast(BF16).rearrange("p (f n) -> p f n", f=8), Buf("Wd"))
        Wv = [V(arW.ap[:, i * 2048:(i + 1) * 2048].bitcast(BF16).rearrange("p (k n) -> p k n", k=16), Buf(f"W{i}")) for i in range(4)]
        W1 = [Wv[0], Wv[2]]
        W2 = [Wv[1], Wv[3]]
        g_bc = V(arWd.ap[:, 0:2048], Buf("g_bc"))
        b_bc = V(arWd.ap[:, 2048:4096], Buf("b_bc"))
        cc = V(arWd.ap[:, 4096:6144], Buf("c"))
        xr = V(arWd.ap[:, 6144:8192], Buf("xr"))
        x1T = V(arW.ap[:, 0:2048].rearrange("p (k n) -> p k n", k=16), Buf("x1T"))
        x1Tb = V(arW.ap[:, 2048:3072].bitcast(BF16).rearrange("p (k n) -> p k n", k=16), Buf("x1Tb"))
        ctx = {"ident": ident, "g_bc": g_bc, "b_bc": b_bc, "c": cc, "col": col, "x1T": x1T, "x1Tb": x1Tb}
        for (t0, tm) in tiles:
            nsub = (tm + 127) // 128
            cblocks = [(c0, min(512, tm - c0)) for c0 in range(0, tm, 512)]
            inherit([Wd.buf], [g_bc.buf, b_bc.buf, cc.buf, xr.buf])
            inherit([w.buf for w in Wv], [x1T.buf, x1Tb.buf])
            kb.dma("sp", xT[:, :, :tm], X1T[:, :, t0:t0 + tm].rearrange("kc p t -> p kc t"))
            iw = 0
            for e in range(16):
                kb.dma("sp", gb[:, :tm], GT[e:e + 1, t0:t0 + tm].to_broadcast([128, tm]))
                wup = w_up[e].rearrange("(kc p) n -> p kc n", p=128)
                kb.dma("pool", Wd, w_dn[e].rearrange("(fc p) n -> p fc n", p=128))
                for fb in range(4):
                    w1, w2 = W1[iw % 2], W2[iw % 2]
                    iw += 1
                    kb.dma("pool", w1, wup[:, :, fb * 256:(fb + 1) * 256])
                    kb.dma("pool", w2, wup[:, :, 1024 + fb * 256:1024 + (fb + 1) * 256])
                    for f2 in range(2):
                        fc = fb * 2 + f2
                        for (c0, cw) in cblocks:
                            pA = psum(); pB = psum()
                            for kc in range(16):
                                kb.mm(pA[:, :cw], w1[:, kc, f2 * 128:(f2 + 1) * 128], xT[:, kc, c0:c0 + cw], start=(kc == 0), stop=(kc == 15))
                            for kc in range(16):
                                kb.mm(pB[:, :cw], w2[:, kc, f2 * 128:(f2 + 1) * 128], xT[:, kc, c0:c0 + cw], start=(kc == 0), stop=(kc == 15))
                            a = sA(); b = sB()
                            kb.act(a[:, :cw], pA[:, :cw], AF.Silu)
                            kb.tt("dve", b[:, :cw], pB[:, :cw], gb[:, c0:c0 + cw], ALU.mult)
                            kb.tt("dve", hhT[:, fc, c0:c0 + cw], a[:, :cw], b[:, :cw], ALU.mult)
                for su in range(nsub):
                    n = min(128, tm - su * 128)
                    for db in range(4):
                        py = psum()
                        for fc in range(8):
                            kb.mm(py[:n, :], hhT[:, fc, su * 128:su * 128 + n], Wd[:, fc, db * 512:(db + 1) * 512],
                                  start=(fc == 0), stop=(fc == 7))
                        ya = yacc[:n, su, db * 512:(db + 1) * 512]
                        if e == 0:
                            kb.cp("act", ya, py[:n, :])
                        else:
                            kb.tt("dve", ya, ya, py[:n, :], ALU.add)
            inherit([g_bc.buf, b_bc.buf, cc.buf, xr.buf], [Wd.buf])
            inherit([x1T.buf, x1Tb.buf], [w.buf for w in Wv])
            kb.dma("sp", g_bc, row_bcast(ins["ln_ffn_g"][layer], 128))
            kb.dma("sp", b_bc, row_bcast(ins["ln_ffn_b"][layer], 128))
            for su in range(nsub):
                n = min(128, tm - su * 128)
                r0 = t0 + su * 128
                kb.dma("sp", xr[:n, :], X1[r0:r0 + n, :])
                kb.stt("dve", yacc[:n, su, :], xr[:n, :], ALPHA, yacc[:n, su, :], ALU.mult, ALU.add)
                ln_and_route(kb, ctx, cfg, ins, n, r0, yacc[:, su, :], layer, "ffn", OUT, OUTT, None, psum)
        kb.P.flush()


_CACHE = {}


def kernel(**inputs):
    cfg = Cfg()
    if "nc" not in _CACHE:
        _CACHE["nc"] = build(cfg)
    nc, ins, outs = _CACHE["nc"]
    f = lambda k: np.ascontiguousarray(np.asarray(inputs[k], dtype=np.float32))
    consts = make_consts()
    shared = {"consts": consts}
    for k in WEIGHT_SHAPES:
        a = f(k)
        if k.startswith("moe_") or k.startswith("ln_") or k == "w_router":
            shared[k] = a
        else:
            shared[k] = np.ascontiguousarray(a[0])
    xp, xs, meta = f("x_prompt"), f("x_sample"), f("meta")
    in_maps = []
    for c in range(8):
        b = c % 4
        s0 = 16 * c
        m = dict(shared)
        m["xtok"] = np.ascontiguousarray(np.concatenate([meta, xp[b], xs[s0:s0 + 16].reshape(128, D)], 0))
        m["state_gla"] = np.ascontiguousarray(f("state_gla")[0, s0:s0 + 16])
        m["state_rwkv"] = np.ascontiguousarray(f("state_rwkv")[0, s0:s0 + 16])
        m["state_shift"] = np.ascontiguousarray(f("state_shift")[0, s0:s0 + 16])
        m["state_s5_re"] = np.ascontiguousarray(f("state_s5_re")[0, s0:s0 + 16])
        m["state_s5_im"] = np.ascontiguousarray(f("state_s5_im")[0, s0:s0 + 16])
        in_maps.append({k: m[k] for k in ins})
    res = run_bass_kernel_spmd(nc, in_maps, core_ids=list(range(8)))
    R = res.results
    NP = cfg.NP
    y_p = np.stack([R[b]["y"][16:NP] for b in range(4)], 0)
    y_s = np.concatenate([R[c]["y"][NP:].reshape(16, 8, D) for c in range(8)], 0)

    def pr(name):
        return np.stack([R[b][name] for b in range(4)], 0)[None]

    def sm(name):
        return np.concatenate([R[c][name] for c in range(8)], 0)[None]

    return (y_p.astype(np.float32), y_s.astype(np.float32),
            pr("gla_p"), sm("gla_s"), pr("rwkv_p"), sm("rwkv_s"), pr("shift_p"), sm("shift_s"),
            pr("s5re_p"), sm("s5re_s"), pr("s5im_p"), sm("s5im_s"))


def dense_T(kb, cfg, ins, srcT, W, epi, pre=None, TB=512):
    NT = cfg.NT
    with contextlib.ExitStack() as st:
        Wb = kb.sb(st, [128, 16, D], BF16, "Wb")
        Wv = W.rearrange("(kc p) n -> p kc n", p=128)
        for g in range(4):
            kb.dma("pool", Wb[:, 4 * g:4 * g + 4, :], Wv[:, 4 * g:4 * g + 4, :])
        xT = [kb.sb(st, [128, 16, TB], BF16, "xT") for _ in range(2)]
        psum = Rot([kb.ps(st, [128, 512], F32, f"ps{i}") for i in range(8)])
        ctx = {"st": st}
        if pre is not None:
            pre(ctx)
        t0 = 0
        i = 0
        while t0 < NT:
            tb = min(TB, NT - t0)
            x = xT[i % 2]
            kb.dma("sp", x[:, :, :tb], srcT[:, :, t0:t0 + tb].rearrange("kc p t -> p kc t"))
            for cc in range(16):
                pa = psum()
                for kc in range(16):
                    kb.mm(pa[:, :tb], Wb[:, kc, cc * 128:(cc + 1) * 128], x[:, kc, :tb], start=(kc == 0), stop=(kc == 15))
                epi(ctx, cc, t0, tb, pa, x)
            t0 += tb
            i += 1
        kb.P.flush()


def phase_s5(kb, cfg, ins, outs, UT, ZT):
    NT, NP = cfg.NT, cfg.NP
    LMAX = 80 if cfg.NT % 64 == 16 else 64
    with contextlib.ExitStack() as st:
        cs = kb.sb(st, [128, C_TOTAL], F32, "consts")
        kb.dma("sp", cs, ins["consts"])
        ident = cs[:, C_IDENT:C_IDENT + 128]
        pm = cs[:, C_PM:C_PM + 8]
        psum = Rot([kb.ps(st, [128, 512], F32, f"ps{i}") for i in range(8)])

        def pj(name):
            t = kb.sb(st, [128, 64], F32, name)
            kb.dma("sp", t, ins[name].rearrange("(j gl) p -> (gl p) j", gl=2), allow_slow_non_contiguous=True)
            return t

        are, aim = pj("c_a_re"), pj("c_a_im")
        dt = kb.sb(st, [128, 64], F32, "dt")
        ld = ins["c_log_dt"].rearrange("(j gl) -> gl j", gl=2)
        for gl in range(2):
            kb.dma("sp", dt[gl * 64:(gl + 1) * 64, :], ld[gl:gl + 1, :].to_broadcast([64, 64]), allow_slow_non_contiguous=True)
        kb.act(dt, dt, AF.Exp)
        T = [kb.sb(st, [128, 64], F32, f"pt{i}") for i in range(10)]
        er, sn, cn, Are, Aim, x_, den, cre, cim, tmp = T
        kb.tt("dve", tmp, are, dt, ALU.mult)
        kb.act(er, tmp, AF.Exp)
        kb.tt("dve", tmp, aim, dt, ALU.mult)
        kb.act(sn, tmp, AF.Sin, scale=1.0 / 16)
        kb.ts("dve", tmp, tmp, 1.0 / 16, math.pi / 2, ALU.mult, ALU.add)
        kb.act(cn, tmp, AF.Sin)
        for _ in range(4):
            kb.tt("dve", tmp, sn, cn, ALU.mult)
            kb.tt("dve", cn, cn, cn, ALU.mult)
            kb.tt("dve", sn, sn, sn, ALU.mult)
            kb.tt("dve", cn, cn, sn, ALU.subtract)
            kb.ts("dve", sn, tmp, 2.0, None, ALU.mult)
        kb.tt("dve", Are, er, cn, ALU.mult)
        kb.tt("dve", Aim, er, sn, ALU.mult)
        kb.ts("dve", x_, Are, -1.0, None, ALU.add)
        kb.tt("dve", den, are, are, ALU.mult)
        kb.tt("dve", tmp, aim, aim, ALU.mult)
        kb.tt("dve", den, den, tmp, ALU.add)
        kb.recip(den, den)
        kb.tt("dve", cre, x_, are, ALU.mult)
        kb.tt("dve", tmp, Aim, aim, ALU.mult)
        kb.tt("dve", cre, cre, tmp, ALU.add)
        kb.tt("dve", cre, cre, den, ALU.mult)
        kb.tt("dve", cim, Aim, are, ALU.mult)
        kb.tt("dve", tmp, x_, aim, ALU.mult)
        kb.tt("dve", cim, cim, tmp, ALU.subtract)
        kb.tt("dve", cim, cim, den, ALU.mult)
        LBb = [kb.sb(st, [128, 16, 128], BF16, f"LBb{i}") for i in range(2)]
        LCb = [kb.sb(st, [128, 16, 128], BF16, f"LCb{i}") for i in range(2)]
        LB3b = [kb.sb(st, [128, 16, 128], BF16, f"LB3b{i}") for i in range(2)]
        with contextlib.ExitStack() as st2:
            LB = [kb.sb(st2, [128, 16, 128], F32, f"LB{i}") for i in range(2)]
            LC = [kb.sb(st2, [128, 16, 128], F32, f"LC{i}") for i in range(2)]
            LB3 = [kb.sb(st2, [128, 16, 128], F32, f"LB3{i}") for i in range(2)]
            Bre = kb.sb(st2, [128, 64, 16], F32, "Bre")
            Bim = kb.sb(st2, [128, 64, 16], F32, "Bim")
            kb.dma("sp", Bre, ins["c_b_re"].rearrange("(j gl) p c -> (gl p) j c", gl=2))
            kb.dma("sp", Bim, ins["c_b_im"].rearrange("(j gl) p c -> (gl p) j c", gl=2))
            Xr = kb.sb(st2, [128, 64, 16], F32, "Xr")
            Xi = kb.sb(st2, [128, 64, 16], F32, "Xi")
            t1 = kb.sb(st2, [128, 64, 16], F32, "t1")
            X4 = kb.sb(st2, [128, 64, 2, 16], F32, "X4")
            bc = lambda v: v.re("p (j o) -> p j o", o=1).bc([128, 64, 16])
            kb.tt("dve", Xr, Bre, bc(cre), ALU.mult)
            kb.tt("dve", t1, Bim, bc(cim), ALU.mult)
            kb.tt("dve", Xr, Xr, t1, ALU.subtract)
            kb.tt("dve", Xi, Bim, bc(cre), ALU.mult)
            kb.tt("dve", t1, Bre, bc(cim), ALU.mult)
            kb.tt("dve", Xi, Xi, t1, ALU.add)
            for i, X in enumerate((Xr, Xi)):
                for gl in range(2):
                    kb.ts("dve", X4[:, :, gl, :], X, pm[:, gl:gl + 1], None, ALU.mult)
                for m in range(16):
                    pt = psum()
                    kb.tr(pt[:, 0:128], X4[:, 4 * m:4 * m + 4, :, :].re("p a b c -> p (a b c)"), ident)
                    kb.cp("act" if m % 2 == 0 else "dve", LB[i][:, m, :], pt[:, 0:128])
            for i in range(2):
                kb.ts("dve", LB3[i][64:128, :, :], LB[i][64:128, :, :], pm[64:128, 7:8], None, ALU.mult)
            Rr = kb.sb(st2, [128, 16, 64], F32, "Rr")
            R4 = kb.sb(st2, [128, 16, 2, 64], F32, "R4")
            for i, name in enumerate(("c_c_re", "c_c_im")):
                kb.dma("sp", Rr, ins[name].rearrange("(m qg) c p -> (qg c) m p", m=16))
                for gl in range(2):
                    kb.ts("dve", R4[:, :, gl, :], Rr, pm[:, 2 + gl:3 + gl], (1.0 if i == 0 else -1.0), ALU.mult, ALU.mult)
                for m in range(16):
                    pt = psum()
                    kb.tr(pt[:, 0:128], R4[:, m, :, :].re("p a b -> p (a b)"), ident)
                    kb.cp("act" if m % 2 == 0 else "dve", LC[i][:, m, :], pt[:, 0:128])
            for i in range(2):
                kb.cp("dve", LBb[i], LB[i])
                kb.cp("act", LCb[i], LC[i])
                kb.cp("dve", LB3b[i][64:128, :, :], LB3[i][64:128, :, :])
            kb.P.flush()
        S5STOP = int(os.environ.get("L1STOP", "99"))
        if S5STOP <= 2:
            return
        Dcol = kb.sb(st, [128, 16], F32, "Dcol")
        kb.dma("sp", Dcol, ins["c_d"].rearrange("(m p) -> p m", p=128), allow_slow_non_contiguous=True)
        HS = [kb.sb(st, [128, 64, LMAX], F32, f"HS{i}") for i in range(2)]
        BUs = [[kb.sb(st, [128, 64, LMAX], F32, f"BU{b}{i}") for i in range(2)] for b in range(2)]
        carry = [kb.sb(st, [128, 64], F32, f"carry{i}") for i in range(2)]
        init = [kb.sb(st, [128, 64], F32, f"init{i}") for i in range(2)]
        sio = [kb.sb(st, [64, 128], F32, f"sio{i}") for i in range(2)]
        sT = [kb.sb(st, [128, 64], F32, f"st{i}") for i in range(4)]
        uT = [kb.sb(st, [128, 16, LMAX], F32, f"uT{i}") for i in range(2)]
        ub = [kb.sb(st, [128, 16, LMAX], BF16, f"ub{i}") for i in range(2)]
        HSb = [kb.sb(st, [128, 64, LMAX], BF16, f"HSb{i}") for i in range(2)]
        yT = kb.sb(st, [128, LMAX], F32, "yT")
        yq = kb.sb(st, [128, LMAX], F32, "yq")
        zb = Rot([kb.sb(st, [128, LMAX], BF16, f"zb{i}") for i in range(2)])
        starts = {0: -1}
        ends = {NP - 1: -1}
        for sidx in range(cfg.n_samp):
            starts[NP + 8 * sidx] = sidx
            ends[NP + 8 * sidx + 7] = sidx
        blocks = []
        t0 = 0
        while t0 < NT:
            rem = NT - t0
            L = 64 if (rem == 64 or rem - 64 >= 32) else rem
            assert L <= LMAX
            blocks.append((t0, L))
            t0 += L

        def bproj(bi):
            t0, L = blocks[bi]
            u = uT[bi % 2]
            kb.dma("sp", u[:, :, :L], UT[:, :, t0:t0 + L].rearrange("kc p t -> p kc t"))
            uq = ub[bi % 2]
            kb.cp("act", uq[:, :, :L], u[:, :, :L])
            ppb = min(8, 512 // L)
            for i in range(2):
                BUv = BUs[bi % 2][i].re("p (m q) t -> p q m t", q=4)
                for q in range(4):
                    m0 = 0
                    while m0 < 16:
                        nm = min(ppb, 16 - m0)
                        pp = psum()
                        for m in range(m0, m0 + nm):
                            osl = pp[:, (m - m0) * L:(m - m0 + 1) * L]
                            if q < 3:
                                kb.mm(osl, LBb[i][32 * q:32 * q + 32, m, :], uq[32 * q:32 * q + 32, m, :L])
                            else:
                                kb.mm(osl, LB3b[i][64:128, m, :], uq[64:128, m, :L])
                        kb.cp("act", BUv[:, q, m0:m0 + nm, :L], pp[:, 0:nm * L].re("p (a t) -> p a t", t=L))
                        m0 += nm

        bproj(0)
        for bi, (t0, L) in enumerate(blocks):
            u = uT[bi % 2]
            BU = BUs[bi % 2]
            if bi + 1 < len(blocks):
                bproj(bi + 1)
            for tl in range(L):
                t = t0 + tl
                if t in starts:
                    sidx = starts[t]
                    if sidx < 0:
                        kb.memset("dve", init[0], 0.0)
                        kb.memset("dve", init[1], 0.0)
                    else:
                        for i, nm in enumerate(("state_s5_re", "state_s5_im")):
                            kb.dma("sp", sio[i], ins[nm][sidx].rearrange("(j gl) p -> j (gl p)", gl=2))
                            pt = psum()
                            kb.tr(pt[:, 0:64], sio[i], ident[:64, :64])
                            kb.cp("dve", init[i], pt[:, 0:64])
                    pr, pi = init[0], init[1]
                elif tl == 0:
                    pr, pi = carry[0], carry[1]
                else:
                    pr, pi = HS[0][:, :, tl - 1], HS[1][:, :, tl - 1]
                kb.tt("dve", sT[0], Are, pr, ALU.mult)
                kb.tt("pool", sT[2], Are, pi, ALU.mult)
                kb.tt("dve", sT[1], Aim, pi, ALU.mult)
                kb.tt("pool", sT[3], Aim, pr, ALU.mult)
                kb.tt("dve", sT[0], sT[0], sT[1], ALU.subtract)
                kb.tt("pool", sT[2], sT[2], sT[3], ALU.add)
                kb.tt("dve", HS[0][:, :, tl], sT[0], BU[0][:, :, tl], ALU.add)
                kb.tt("pool", HS[1][:, :, tl], sT[2], BU[1][:, :, tl], ALU.add)
                if t in ends:
                    sidx = ends[t]
                    for i, (pn, sn_) in enumerate((("s5re_p", "s5re_s"), ("s5im_p", "s5im_s"))):
                        pt = psum()
                        kb.tr(pt[:64, 0:128], HS[i][:, :, tl], ident)
                        so = sio[i]
                        kb.cp("dve", so, pt[:64, 0:128])
                        dst = outs[pn] if sidx < 0 else outs[sn_][sidx]
                        kb.dma("sp", dst.rearrange("(j gl) p -> j (gl p)", gl=2), so)
            kb.cp("dve", carry[0], HS[0][:, :, L - 1])
            kb.cp("dve", carry[1], HS[1][:, :, L - 1])
            kb.cp("act", HSb[0][:, :, :L], HS[0][:, :, :L])
            kb.cp("act", HSb[1][:, :, :L], HS[1][:, :, :L])
            for m in range(16):
                pps = [psum(), psum()] if 4 * L > 512 else [psum()]
                for q in range(4):
                    pp = pps[q // 2] if len(pps) == 2 else pps[0]
                    off = (q % 2 if len(pps) == 2 else q) * L
                    kb.mm(pp[:, off:off + L], LCb[0][:, m, :], HSb[0][:, 4 * m + q, :L], start=True, stop=False)
                    kb.mm(pp[:, off:off + L], LCb[1][:, m, :], HSb[1][:, 4 * m + q, :L], start=False, stop=True)
                    if q == 0:
                        kb.ts("dve", yq[:, :L], pp[:, off:off + L], pm[:, 4:5], None, ALU.mult)
                    else:
                        kb.stt("dve", yq[:, :L], pp[:, off:off + L], pm[:, 4 + q:5 + q], yq[:, :L], ALU.mult, ALU.add)
                kb.stt("dve", yT[:, :L], u[:, m, :L], Dcol[:, m:m + 1], yq[:, :L], ALU.mult, ALU.add)
                z = zb()
                kb.act(z[:, :L], yT[:, :L], AF.Gelu_apprx_tanh)
                kb.dma("sp", ZT[m, :, t0:t0 + L], z[:, :L])
        kb.P.flush()


def phase_l1(kb, cfg, ins, outs, S):
    def pre_a(ctx):
        ctx["ot"] = Rot([kb.sb(ctx["st"], [128, 512], F32, "ot") for _ in range(4)])
        ctx["i"] = 0

    def epi_a(ctx, cc, t0, tb, pa, x):
        ot = ctx["ot"]()
        ctx["i"] += 1
        kb.cp("act" if ctx["i"] % 2 == 0 else "dve", ot[:, :tb], pa[:, :tb])
        kb.dma("sp", S["UT"][cc, :, t0:t0 + tb], ot[:, :tb])

    L1STOP = int(os.environ.get("L1STOP", "99"))
    dense_T(kb, cfg, ins, S["X2T"], ins["od_w_in"], epi_a, pre=pre_a)
    if L1STOP <= 1:
        return
    phase_s5(kb, cfg, ins, outs, S["UT"], S["ZT"])
    if L1STOP <= 5:
        return

    def pre_c(ctx):
        ctx["sg"] = Rot([kb.sb(ctx["st"], [128, 512], F32, "sg") for _ in range(2)])
        ctx["zz"] = Rot([kb.sb(ctx["st"], [128, 512], BF16, "zz") for _ in range(4)])
        ctx["bcol"] = kb.sb(ctx["st"], [128, 16], F32, "bcol")
        kb.dma("sp", ctx["bcol"], ins["c_b_glu"].rearrange("(m p) -> p m", p=128), allow_slow_non_contiguous=True)

    def epi_c(ctx, cc, t0, tb, pa, x):
        sg = ctx["sg"]()
        zz = ctx["zz"]()
        kb.act(sg[:, :tb], pa[:, :tb], AF.Sigmoid, bias=ctx["bcol"][:, cc:cc + 1], scale=1.0)
        kb.tt("dve", zz[:, :tb], x[:, cc, :tb], sg[:, :tb], ALU.mult)
        kb.dma("sp", S["ZZT"][cc, :, t0:t0 + tb], zz[:, :tb])

    dense_T(kb, cfg, ins, S["ZT"], ins["c_w_glu"], epi_c, pre=pre_c)
    phase_outln(kb, cfg, ins, None, ins["od_w_out"], S["X2"], 1, S["X3"], S["X3T"], S["GT3"], srcT=S["ZZT"])
```

```python
import contextlib
import math
import os
import numpy as np
import concourse.bass as bass
import concourse.mybir as mybir
from concourse.bass_utils import run_bass_kernel_spmd

F32 = mybir.dt.float32
BF16 = mybir.dt.bfloat16
AF = mybir.ActivationFunctionType
ALU = mybir.AluOpType
AX = mybir.AxisListType

D = 2048
A_COLS = 3088
B_COLS = 3328
EV_COLS = A_COLS + B_COLS
ALPHA = 4.0 ** 0.25
LN_EPS = 1e-5


class Buf:
    __slots__ = ("name", "w", "r")

    def __init__(self, name=""):
        self.name = name
        self.w = []
        self.r = []


class Op:
    __slots__ = ("eng", "fn", "waits", "signal", "sigval", "is_dma", "dsem", "dval", "epoch")

    def __init__(self, eng, fn, is_dma=False, epoch=0):
        self.eng = eng
        self.fn = fn
        self.waits = []
        self.signal = False
        self.sigval = 0
        self.is_dma = is_dma
        self.dsem = None
        self.dval = 0
        self.epoch = epoch


class Prog:
    ENGS = ("pe", "act", "dve", "pool", "sp")
    ENGOBJ = {"pe": "tensor", "act": "scalar", "dve": "vector", "pool": "gpsimd", "sp": "sync"}

    def __init__(self, nc, stack, dma_slots=None):
        self.nc = nc
        self.ops = {e: [] for e in self.ENGS}
        self.dma_slots = dma_slots or {"sp": 16, "act": 8, "pool": 12}
        self.dma_count = {q: 0 for q in self.dma_slots}
        self.dma_last = {q: [None] * n for q, n in self.dma_slots.items()}
        self.epoch = 0
        self.sigbase = {e: 0 for e in self.ENGS}
        self.known = {e: {} for e in self.ENGS}
        self.esem = {e: stack.enter_context(nc.semaphore(f"s_{e}")) for e in self.ENGS}
        self.dsem = {}
        for q, n in self.dma_slots.items():
            for s in range(n):
                self.dsem[(q, s)] = stack.enter_context(nc.semaphore(f"d_{q}{s}"))

    def _deps(self, rec, reads, writes):
        deps = []
        for b in reads:
            deps.extend(b.w)
        for b in writes:
            for d in b.w:
                if d.is_dma or rec.is_dma or d.eng != rec.eng:
                    deps.append(d)
            for d in b.r:
                if d.is_dma or rec.is_dma or d.eng != rec.eng:
                    deps.append(d)
        seen = set()
        for d in deps:
            if d is rec or id(d) in seen or d.epoch < self.epoch:
                continue
            if (not d.is_dma) and (not rec.is_dma) and d.eng == "pe" and rec.eng == "pe":
                continue
            seen.add(id(d))
            rec.waits.append(d)
            d.signal = True
        for b in reads:
            if not rec.is_dma:
                b.r = [x for x in b.r if x.is_dma or x.eng != rec.eng]
            b.r.append(rec)
        for b in writes:
            b.w = [rec]
            b.r = []

    def op(self, eng, fn, reads=(), writes=()):
        rec = Op(eng, fn, epoch=self.epoch)
        self._deps(rec, reads, writes)
        self.ops[eng].append(rec)
        return rec

    def dma(self, q, out, in_, reads=(), writes=(), **kw):
        rec = Op(q, (out, in_, kw), is_dma=True, epoch=self.epoch)
        i = self.dma_count[q]
        self.dma_count[q] += 1
        K = self.dma_slots[q]
        slot = i % K
        rec.dsem = (q, slot)
        rec.dval = 16 * (i // K + 1)
        prev = self.dma_last[q][slot]
        if prev is not None and prev.epoch == self.epoch:
            rec.waits.append(prev)
        self.dma_last[q][slot] = rec
        self._deps(rec, reads, writes)
        self.ops[q].append(rec)
        return rec

    def flush(self):
        nc = self.nc
        for e in self.ENGS:
            last = None
            for rec in self.ops[e]:
                if not rec.is_dma:
                    last = rec
            if last is not None:
                last.signal = True
            c = self.sigbase[e]
            for rec in self.ops[e]:
                if (not rec.is_dma) and rec.signal:
                    c += 1
                    rec.sigval = c
            self.sigbase[e] = c
        final = {}
        for e in self.ENGS:
            final[e] = self.sigbase[e]
        dfinal = {}
        for q, lst in self.dma_last.items():
            for s, rec in enumerate(lst):
                if rec is not None:
                    dfinal[(q, s)] = rec.dval
        with nc.Block() as block:
            def make(e):
                def body(eng):
                    known = self.known[e]
                    for rec in self.ops[e]:
                        for d in rec.waits:
                            if d.is_dma:
                                key, val, sem = d.dsem, d.dval, self.dsem[d.dsem]
                            else:
                                key, val, sem = d.eng, d.sigval, self.esem[d.eng]
                            if known.get(key, 0) >= val:
                                continue
                            known[key] = val
                            eng.wait_ge(sem, val)
                        if rec.is_dma:
                            out, in_, kw = rec.fn
                            eng.dma_start(out=out, in_=in_, **kw).then_inc(self.dsem[rec.dsem], 16)
                        else:
                            ins = rec.fn(eng)
                            if rec.signal:
                                ins.then_inc(self.esem[e], 1)
                    for k, val in final.items():
                        if val > 0 and known.get(k, 0) < val and k != e:
                            known[k] = val
                            eng.wait_ge(self.esem[k], val)
                    for k, val in dfinal.items():
                        if known.get(k, 0) < val:
                            known[k] = val
                            eng.wait_ge(self.dsem[k], val)
                return body
            for e in self.ENGS:
                getattr(block, self.ENGOBJ[e])(make(e))
        self.ops = {e: [] for e in self.ENGS}
        self.epoch += 1


class V:
    __slots__ = ("ap", "buf")

    def __init__(self, ap, buf):
        self.ap = ap
        self.buf = buf

    def __getitem__(self, idx):
        return V(self.ap[idx], self.buf)

    def re(self, pat, **kw):
        return V(self.ap.rearrange(pat, **kw), self.buf)

    def bc(self, shape):
        return V(self.ap.to_broadcast(list(shape)), self.buf)


class K:
    def __init__(self, nc, stack):
        self.nc = nc
        self.P = Prog(nc, stack)
        self.n = 0

    def sb(self, st, shape, dt=F32, name=None):
        self.n += 1
        h = st.enter_context(self.nc.sbuf_tensor(f"{name or 't'}{self.n}", list(shape), dt))
        return V(h[tuple(slice(None) for _ in shape)], Buf(name or "t"))

    def ps(self, st, shape, dt=F32, name=None):
        self.n += 1
        h = st.enter_context(self.nc.psum_tensor(f"{name or 'p'}{self.n}", list(shape), dt))
        return V(h[tuple(slice(None) for _ in shape)], Buf(name or "p"))

    def dma(self, q, out, in_, **kw):
        outv = out if isinstance(out, V) else V(out, None)
        inv = in_ if isinstance(in_, V) else V(in_, None)
        return self.P.dma(q, outv.ap, inv.ap,
                          reads=[inv.buf] if inv.buf is not None else [],
                          writes=[outv.buf] if outv.buf is not None else [], **kw)

    def mm(self, out, lhsT, rhs, start=True, stop=True):
        self.P.op("pe", lambda e: e.matmul(out.ap, lhsT.ap, rhs.ap, start=start, stop=stop),
                  reads=[lhsT.buf, rhs.buf], writes=[out.buf])

    def tr(self, out, in_, ident):
        self.P.op("pe", lambda e: e.transpose(out.ap, in_.ap, ident.ap),
                  reads=[in_.buf, ident.buf], writes=[out.buf])

    def tt(self, eng, out, a, b, op):
        self.P.op(eng, lambda e: e.tensor_tensor(out.ap, a.ap, b.ap, op),
                  reads=[a.buf, b.buf], writes=[out.buf])

    def ts(self, eng, out, a, s1, s2=None, op0=ALU.mult, op1=None):
        rd = [a.buf]
        s1a = s1.ap if isinstance(s1, V) else s1
        s2a = s2.ap if isinstance(s2, V) else s2
        if isinstance(s1, V):
            rd.append(s1.buf)
        if isinstance(s2, V):
            rd.append(s2.buf)
        if op1 is None:
            self.P.op(eng, lambda e: e.tensor_scalar(out.ap, a.ap, s1a, None, op0), reads=rd, writes=[out.buf])
        else:
            self.P.op(eng, lambda e: e.tensor_scalar(out.ap, a.ap, s1a, s2a, op0, op1), reads=rd, writes=[out.buf])

    def stt(self, eng, out, a, s, b, op0, op1):
        rd = [a.buf, b.buf]
        sa = s.ap if isinstance(s, V) else s
        if isinstance(s, V):
            rd.append(s.buf)
        self.P.op(eng, lambda e: e.scalar_tensor_tensor(out.ap, a.ap, sa, b.ap, op0, op1), reads=rd, writes=[out.buf])

    def act(self, out, a, func, bias=None, scale=None):
        rd = [a.buf]
        kw = {}
        if bias is not None:
            kw["bias"] = bias.ap if isinstance(bias, V) else bias
            if isinstance(bias, V):
                rd.append(bias.buf)
        if scale is not None:
            kw["scale"] = scale.ap if isinstance(scale, V) else scale
            if isinstance(scale, V):
                rd.append(scale.buf)
        self.P.op("act", lambda e: e.activation(out.ap, a.ap, func, **kw), reads=rd, writes=[out.buf])

    def cp(self, eng, out, a):
        if eng == "act":
            self.P.op("act", lambda e: e.copy(out.ap, a.ap), reads=[a.buf], writes=[out.buf])
        else:
            self.P.op(eng, lambda e: e.tensor_copy(out.ap, a.ap), reads=[a.buf], writes=[out.buf])

    def red(self, eng, out, a, op=ALU.add, axis=AX.X):
        self.P.op(eng, lambda e: e.tensor_reduce(out.ap, a.ap, axis, op), reads=[a.buf], writes=[out.buf])

    def recip(self, out, a):
        self.P.op("dve", lambda e: e.reciprocal(out.ap, a.ap), reads=[a.buf], writes=[out.buf])

    def rsqrt(self, out, a, scale, bias):
        self.act(out, a, AF.Sqrt, bias=bias, scale=scale)
        self.recip(out, out)

    def memset(self, eng, out, val):
        self.P.op(eng, lambda e: e.memset(out.ap, val), reads=[], writes=[out.buf])


def row_bcast(ap1d, nparts):
    n = ap1d.shape[-1]
    return ap1d.rearrange("(o n) -> o n", o=1).to_broadcast([nparts, n])


def make_consts():
    c = {}
    c["ident"] = np.eye(128, dtype=np.float32)
    i = np.arange(128)
    c["tri_incl"] = (i[:, None] <= i[None, :]).astype(np.float32)
    c["tri_strict"] = (i[:, None] < i[None, :]).astype(np.float32)
    c["tri_gt"] = (i[:, None] > i[None, :]).astype(np.float32)
    c["ones"] = np.ones((128, 128), dtype=np.float32)
    sel = np.zeros((128, 16, 128), dtype=np.float32)
    for e in range(16):
        sel[e, e, :] = 1.0
    c["sel"] = sel.reshape(128, 16 * 128)
    pm = np.zeros((128, 8), dtype=np.float32)
    for q in range(4):
        pm[32 * q:32 * q + 32, 4 + q] = 1.0
    pm[:64, 0] = 1.0
    pm[64:, 1] = 1.0
    pm[:, 2] = ((i // 16) % 2 == 0)
    pm[:, 3] = ((i // 16) % 2 == 1)
    return np.concatenate([c["ident"], c["tri_incl"], c["tri_strict"], c["tri_gt"], c["ones"], c["sel"], pm], axis=1)


C_IDENT, C_TRII, C_TRIS, C_TRIG, C_ONES, C_SEL = 0, 128, 256, 384, 512, 640
C_PM = 640 + 16 * 128
C_TOTAL = C_PM + 8


class Cfg:
    def __init__(self, n_prompt=2048, n_samp=16, debug=False, phases=None):
        self.n_meta = 16
        self.n_prompt = n_prompt
        self.NP = 16 + n_prompt
        self.n_samp = n_samp
        self.NT = self.NP + 8 * n_samp
        self.debug = debug
        self.phases = phases
        ch = [(0, 16, True, -1)]
        r = 16
        while r < self.NP:
            ch.append((r, 64, False, -1))
            r += 64
        for s in range(n_samp):
            ch.append((self.NP + 8 * s, 8, True, s))
        self.chunks = ch
        self.subtiles = []
        r = 0
        while r < self.NT:
            n = min(128, self.NT - r)
            self.subtiles.append((r, n))
            r += n


def load_consts(kb, st, cst):
    t = kb.sb(st, [128, C_TOTAL], F32, "consts")
    kb.dma("sp", t, cst)
    tb = kb.sb(st, [128, 128], BF16, "identb")
    kb.cp("dve", tb, t[:, C_IDENT:C_IDENT + 128])
    return t, tb


def dense_phase(kb, cfg, cst, src, W, ncols, epi, post=None, pre=None, srcT=None):
    with contextlib.ExitStack() as st:
        consts, identb = load_consts(kb, st, cst)
        KC = 16
        Wb = kb.sb(st, [128, KC, ncols], BF16, "Wb")
        Wv = W.rearrange("(kc p) n -> p kc n", p=128)
        for g in range(4):
            kb.dma("pool", Wb[:, 4 * g:4 * g + 4, :], Wv[:, 4 * g:4 * g + 4, :])
        xs = [kb.sb(st, [128, D], F32, "xs") for _ in range(2)]
        xb = [kb.sb(st, [128, D], BF16, "xb") for _ in range(2)]
        xT = [kb.sb(st, [128, KC, 128], BF16, "xT") for _ in range(2)]
        ptb = [kb.ps(st, [128, KC, 128], BF16, "ptb") for _ in range(1)]
        pacc = [kb.ps(st, [128, 512], F32, "pacc") for _ in range(4)]
        ctx = {"st": st, "consts": consts, "identb": identb}
        if pre is not None:
            pre(ctx)
        ia = 0
        for ts, (r0, n) in enumerate(cfg.subtiles):
            a = ts % 2
            if srcT is not None:
                kb.dma("sp", xT[a][:, :, :n], srcT[:, :, r0:r0 + n].rearrange("kc p t -> p kc t"))
            else:
                kb.dma("sp", xs[a][:n, :], src[r0:r0 + n, :])
                kb.cp("act", xb[a][:n, :], xs[a][:n, :])
                pt = ptb[0]
                for kc in range(KC):
                    kb.tr(pt[:, kc, :n], xb[a][:n, kc * 128:(kc + 1) * 128], identb[:n, :n])
                kb.cp("dve", xT[a][:, 0:8, :n], pt[:, 0:8, :n])
                kb.cp("dve", xT[a][:, 8:16, :n], pt[:, 8:16, :n])
            ncb = (ncols + 511) // 512
            for cb in range(ncb):
                cw = min(512, ncols - cb * 512)
                pa = pacc[ia % 4]
                ia += 1
                for kc in range(KC):
                    kb.mm(pa[:n, :cw], xT[a][:, kc, :n], Wb[:, kc, cb * 512:cb * 512 + cw],
                          start=(kc == 0), stop=(kc == KC - 1))
                epi(ctx, ts, r0, n, cb, cw, pa)
            if post is not None:
                post(ctx, ts, r0, n)
        kb.P.flush()


def phase_inproj(kb, cfg, cst, xtok, w_in, P0):
    for c0 in range(0, EV_COLS, 2048):
        ncols = min(2048, EV_COLS - c0)
        state = {"i": 0}

        def pre(ctx):
            ctx["ot"] = [kb.sb(ctx["st"], [128, 512], F32, "ot") for _ in range(4)]

        def epi(ctx, ts, r0, n, cb, cw, pa, c0=c0, state=state):
            i = state["i"]
            state["i"] += 1
            ot = ctx["ot"][i % 4]
            kb.cp("act" if i % 2 == 0 else "dve", ot[:n, :cw], pa[:n, :cw])
            kb.dma("sp", P0[r0:r0 + n, c0 + cb * 512:c0 + cb * 512 + cw], ot[:n, :cw])

        dense_phase(kb, cfg, cst, xtok, w_in[:, c0:c0 + ncols], ncols, epi, pre=pre)


WEIGHT_SHAPES = {
    "ev_w_in": [D, EV_COLS], "ev_w_out": [D, D], "a_gate_up": [16, 512], "a_gate_b": [512],
    "a_norm_g": [1024], "b_mu": [B_COLS], "b_w0": [1024], "b_w_up": [64, 1024], "b_a0": [1024],
    "b_a_up": [64, 1024], "b_g_up": [128, 1024], "b_k_k": [1024], "b_k_a": [1024], "b_r_k": [1024],
    "b_ln_g": [1024], "b_ln_b": [1024], "od_w_in": [D, D], "c_a_re": [128, 64], "c_a_im": [128, 64],
    "c_log_dt": [128], "c_b_re": [128, 64, 16], "c_b_im": [128, 64, 16], "c_c_re": [128, 16, 64],
    "c_c_im": [128, 16, 64], "c_d": [D], "c_w_glu": [D, D], "c_b_glu": [D], "od_w_out": [D, D],
    "w_router": [D, 16], "moe_w_up": [2, 16, D, D], "moe_w_down": [2, 16, 1024, D],
    "ln_mix_g": [2, D], "ln_mix_b": [2, D], "ln_ffn_g": [2, D], "ln_ffn_b": [2, D],
}


def build(cfg):
    nc = bass.Bass("TRN2", target_bir_lowering=False)
    NT, NS = cfg.NT, cfg.n_samp
    ins = {}

    def inp(name, shape):
        ins[name] = nc.dram_tensor(name, list(shape), F32, kind="ExternalInput").ap()
        return ins[name]

    inp("xtok", [NT, D])
    inp("consts", [128, C_TOTAL])
    inp("state_gla", [NS, 4, 128, 256])
    inp("state_rwkv", [NS, 16, 64, 64])
    inp("state_shift", [NS, B_COLS])
    inp("state_s5_re", [NS, 128, 64])
    inp("state_s5_im", [NS, 128, 64])
    for k, shp in WEIGHT_SHAPES.items():
        if cfg.phases is not None and k.startswith("moe_") and not any(p.startswith("moe") for p in cfg.phases):
            continue
        inp(k, shp)
    outs = {}

    def outp(name, shape):
        outs[name] = nc.dram_tensor(name, list(shape), F32, kind="ExternalOutput").ap()
        return outs[name]

    def scratch(name, shape, dt=F32):
        kind = "ExternalOutput" if cfg.debug else "Internal"
        t = nc.dram_tensor(name, list(shape), dt, kind=kind).ap()
        if cfg.debug:
            outs[name] = t
        return t

    outp("y", [NT, D])
    outp("gla_p", [4, 128, 256]); outp("gla_s", [NS, 4, 128, 256])
    outp("rwkv_p", [16, 64, 64]); outp("rwkv_s", [NS, 16, 64, 64])
    outp("shift_p", [B_COLS]); outp("shift_s", [NS, B_COLS])
    outp("s5re_p", [128, 64]); outp("s5re_s", [NS, 128, 64])
    outp("s5im_p", [128, 64]); outp("s5im_s", [NS, 128, 64])
    S = {}
    S["P0"] = scratch("P0", [NT, EV_COLS])
    S["OM"] = scratch("OM", [NT, D])
    S["X1"] = scratch("X1", [NT, D])
    S["X1T"] = scratch("X1T", [16, 128, NT], BF16)
    S["GT"] = scratch("GT", [16, NT])
    S["X2"] = scratch("X2", [NT, D])
    S["UT"] = scratch("UT", [16, 128, NT])
    S["ZT"] = scratch("ZT", [16, 128, NT], BF16)
    S["X2T"] = scratch("X2T", [16, 128, NT], BF16)
    S["ZZT"] = scratch("ZZT", [16, 128, NT], BF16)
    S["X3"] = scratch("X3", [NT, D])
    S["X3T"] = scratch("X3T", [16, 128, NT], BF16)
    S["GT3"] = scratch("GT3", [16, NT])

    ph = cfg.phases
    with contextlib.ExitStack() as stack:
        kb = K(nc, stack)
        if ph is None or "inproj" in ph:
            phase_inproj(kb, cfg, ins["consts"], ins["xtok"], ins["ev_w_in"], S["P0"])
        if ph is None or "mix0" in ph:
            phase_mix0(kb, cfg, ins, outs, S)
        if ph is None or "out0" in ph:
            phase_outln(kb, cfg, ins, S["OM"], ins["ev_w_out"], ins["xtok"], 0, S["X1"], S["X1T"], S["GT"])
        if ph is None or "moe0" in ph:
            phase_moe(kb, cfg, ins, 0, S["X1"], S["X1T"], S["GT"], S["X2"], S["X2T"])
        if ph is None or "l1" in ph:
            phase_l1(kb, cfg, ins, outs, S)
        if ph is None or "moe1" in ph:
            phase_moe(kb, cfg, ins, 1, S["X3"], S["X3T"], S["GT3"], outs["y"])
    return nc, ins, outs


def phase_l1(*a, **k):
    raise NotImplementedError


class Rot:
    def __init__(self, tiles):
        self.t = tiles
        self.i = 0

    def __call__(self):
        t = self.t[self.i % len(self.t)]
        self.i += 1
        return t


def phase_mix0(kb, cfg, ins, outs, S):
    RSTOP = int(os.environ.get('RSTOP', '99'))
    P0, OM = S["P0"], S["OM"]
    C0 = math.exp(-0.5)
    with contextlib.ExitStack() as st:
        cs = kb.sb(st, [128, 640], F32, "consts")
        kb.dma("sp", cs, ins["consts"][:, 0:640])
        ident = cs[:, C_IDENT:C_IDENT + 128]
        tri_i = cs[:, C_TRII:C_TRII + 128]
        tri_s = cs[:, C_TRIS:C_TRIS + 128]
        tri_g = cs[:, C_TRIG:C_TRIG + 128]
        ones = cs[:, C_ONES:C_ONES + 128]

        def bcast(name, n, parts=64):
            t = kb.sb(st, [parts, n], F32, name)
            kb.dma("sp", t, row_bcast(ins[name], parts))
            return t

        mu_bc = bcast("b_mu", B_COLS)
        kk_bc = bcast("b_k_k", 1024)
        ka_bc = bcast("b_k_a", 1024)
        rk_bc = bcast("b_r_k", 1024)
        lng_bc = bcast("b_ln_g", 1024)
        lnb_bc = bcast("b_ln_b", 1024)
        ng_bc = bcast("a_norm_g", 1024)
        w0_row = bcast("b_w0", 1024, 1)
        a0_row = bcast("b_a0", 1024, 1)
        gb_row = bcast("a_gate_b", 512, 1)
        w_up = kb.sb(st, [64, 1024], F32, "w_up"); kb.dma("sp", w_up, ins["b_w_up"])
        a_up = kb.sb(st, [64, 1024], F32, "a_up"); kb.dma("sp", a_up, ins["b_a_up"])
        g_up = kb.sb(st, [128, 1024], F32, "g_up"); kb.dma("sp", g_up, ins["b_g_up"])
        gate_up = kb.sb(st, [16, 512], F32, "gate_up"); kb.dma("sp", gate_up, ins["a_gate_up"])

        pa = kb.sb(st, [64, A_COLS], F32, "pa")
        pb = kb.sb(st, [64, B_COLS], F32, "pb")
        xm = kb.sb(st, [64, B_COLS], F32, "xm")
        omix = kb.sb(st, [64, D], F32, "omix")
        Sg = kb.sb(st, [128, 4, 256], F32, "Sg")
        M = kb.sb(st, [64, 16, 64], F32, "M")
        Mio = kb.sb(st, [64, 16, 64], F32, "Mio")
        W = [kb.sb(st, [64, 512], F32, f"w{i}") for i in range(16)]
        XT = [kb.sb(st, [64, 8, 64], F32, f"xt{i}") for i in range(4)]
        AM = [kb.sb(st, [64, 8, 64], F32, f"am{i}") for i in range(7)]
        small = Rot([kb.sb(st, [128, 256], F32, f"sm{i}") for i in range(10)])
        col = Rot([kb.sb(st, [128, 16], F32, f"col{i}") for i in range(12)])
        psum = Rot([kb.ps(st, [128, 512], F32, f"ps{i}") for i in range(8)])

        def h3(v, C):
            return v.re("p (h k) -> p h k", h=8)

        def mask3(m, C):
            return m[:C, :C].re("p (o c) -> p o c", o=1).bc([C, 8, C])

        for (r0, C, seq_start, sidx) in cfg.chunks:
            last = (r0 + C == cfg.NP) if sidx < 0 else True
            kb.dma("sp", pa[:C, :], P0[r0:r0 + C, 0:A_COLS])
            kb.dma("sp", pb[:C, :], P0[r0:r0 + C, A_COLS:EV_COLS])
            if seq_start:
                if sidx < 0:
                    kb.memset("dve", xm[0:1, :], 0.0)
                    kb.memset("dve", Sg, 0.0)
                    kb.memset("dve", M, 0.0)
                else:
                    kb.dma("sp", xm[0:1, :], ins["state_shift"][sidx:sidx + 1, :])
                    kb.dma("sp", Sg, ins["state_gla"][sidx].rearrange("h k v -> k h v"))
                    kb.dma("sp", Mio, ins["state_rwkv"][sidx].rearrange("h v k -> v h k"))
                    for hh in range(2):
                        pt = psum()
                        for h in range(8):
                            kb.tr(pt[:64, h * 64:(h + 1) * 64], Mio[:, hh * 8 + h, :], ident[:64, :64])
                        kb.cp("dve", M[:, hh * 8:hh * 8 + 8, :], pt[:64, :].re("p (h v) -> p h v", h=8))
                if C > 1:
                    kb.dma("sp", xm[1:C, :], P0[r0:r0 + C - 1, A_COLS:EV_COLS])
            else:
                kb.dma("sp", xm[:C, :], P0[r0 - 1:r0 + C - 1, A_COLS:EV_COLS])

            pt = psum()
            kb.tr(pt[:16, :C], pa[:C, 3072:3088], ident[:C, :C])
            gdT = small()
            kb.cp("dve", gdT[:16, :C], pt[:16, :C])
            pg = psum()
            kb.mm(pg[:C, :512], gdT[:16, :C], gate_up[:16, :], start=True, stop=False)
            kb.mm(pg[:C, :512], ones[0:1, :C], gb_row[0:1, :], start=False, stop=True)
            lgp = W[0]
            kb.act(lgp[:C, :], pg[:C, :512], AF.Exp, scale=-1.0)
            kb.act(lgp[:C, :], lgp[:C, :], AF.Ln, bias=1.0)
            for h in range(4 if os.environ.get('NOGLA') is None else 0):
                lgh = lgp[:C, h * 128:(h + 1) * 128]
                q_tok = pa[:C, h * 128:(h + 1) * 128]
                k_tok = pa[:C, 512 + h * 128:512 + (h + 1) * 128]
                v_tok = pa[:C, 1024 + h * 256:1024 + (h + 1) * 256]
                r_tok = pa[:C, 2048 + h * 256:2048 + (h + 1) * 256]
                pbT = psum()
                kb.mm(pbT[:, :C], lgh, tri_i[:C, :C])
                eqT = small(); ekT = small()
                kb.act(eqT[:, :C], pbT[:, :C], AF.Exp, scale=-1.0 / 16)
                kb.act(ekT[:, :C], pbT[:, :C], AF.Exp, scale=1.0 / 16)
                pq = psum()
                kb.tr(pq[:, :C], q_tok, ident[:C, :C])
                kb.tr(pq[:, 128:128 + C], k_tok, ident[:C, :C])
                qtT = small(); ktT = small()
                kb.stt("dve", qtT[:, :C], pq[:, :C], 128.0 ** -0.5, eqT[:, :C], ALU.mult, ALU.mult)
                kb.tt("dve", ktT[:, :C], pq[:, 128:128 + C], ekT[:, :C], ALU.mult)
                patt = psum()
                kb.mm(patt[:C, :C], ktT[:, :C], qtT[:, :C])
                attm = small()
                kb.tt("dve", attm[:C, :C], patt[:C, :C], tri_i[:C, :C], ALU.mult)
                pdl = psum()
                kb.mm(pdl[:C, :128], tri_g[:C, :C], lgh)
                khat = small()
                kb.act(khat[:C, :128], pdl[:C, :128], AF.Exp, scale=-1.0 / 16)
                kb.tt("dve", khat[:C, :128], khat[:C, :128], k_tok, ALU.mult)
                po = psum()
                kb.mm(po[:C, :256], attm[:C, :C], v_tok, start=True, stop=False)
                kb.mm(po[:C, :256], qtT[:, :C], Sg[:, h, :], start=False, stop=True)
                pS = psum()
                kb.mm(pS[:, :256], khat[:C, :128], v_tok)
                kb.stt("dve", Sg[:, h, :], Sg[:, h, :], eqT[:, C - 1:C], pS[:, :256], ALU.mult, ALU.add)
                sq = small(); ssq = col()
                kb.act(sq[:C, :], po[:C, :256], AF.Square)
                kb.red("dve", ssq[:C, 0:1], sq[:C, :])
                kb.rsqrt(ssq[:C, 0:1], ssq[:C, 0:1], 1.0 / 256, 1e-5)
                og = small(); sr = small()
                kb.stt("dve", og[:C, :], po[:C, :256], ssq[:C, 0:1], ng_bc[:C, h * 256:(h + 1) * 256], ALU.mult, ALU.mult)
                kb.act(sr[:C, :], r_tok, AF.Silu)
                kb.tt("dve", omix[:C, h * 256:(h + 1) * 256], og[:C, :], sr[:C, :], ALU.mult)
            if last:
                dst = outs["gla_p"] if sidx < 0 else outs["gla_s"][sidx]
                kb.dma("sp", dst.rearrange("h k v -> k h v"), Sg)

            kb.tt("dve", xm[:C, :], xm[:C, :], pb[:C, :], ALU.subtract)
            kb.tt("dve", xm[:C, :], xm[:C, :], mu_bc[:C, :], ALU.mult)
            kb.tt("dve", xm[:C, :], xm[:C, :], pb[:C, :], ALU.add)
            twd = small(); sgd = small()
            kb.act(twd[:C, :64], xm[:C, 3072:3136], AF.Tanh)
            kb.act(sgd[:C, :128], xm[:C, 3200:3328], AF.Sigmoid)
            pt = psum()
            kb.tr(pt[:64, 0:C], twd[:C, :64], ident[:C, :C])
            kb.tr(pt[:64, 64:64 + C], xm[:C, 3136:3200], ident[:C, :C])
            kb.tr(pt[:128, 128:128 + C], sgd[:C, :128], ident[:C, :C])
            tT = small()
            kb.cp("dve", tT[:64, 0:128], pt[:64, 0:128])
            kb.cp("dve", tT[:128, 128:128 + C], pt[:128, 128:128 + C])
            twdT = tT[:64, 0:C]; adT = tT[:64, 64:64 + C]; sgdT = tT[:128, 128:128 + C]
            for hh in range(2 if os.environ.get('NORWKV') is None else 0):
                hs = hh * 512
                r = xm[:C, hs:hs + 512]
                k = xm[:C, 1024 + hs:1024 + hs + 512]
                v = xm[:C, 2048 + hs:2048 + hs + 512]
                wl, a, g, kk, kmod, b, Gs, rt, eGn, nbt, kt, kap, oc, tmp1, tmp2, tmp3 = [w[:C, :] for w in W]
                pz = psum()
                kb.mm(pz[:C, :], twdT, w_up[:64, hs:hs + 512], start=True, stop=False)
                kb.mm(pz[:C, :], ones[0:1, :C], w0_row[0:1, hs:hs + 512], start=False, stop=True)
                kb.act(wl, pz[:C, :], AF.Sigmoid)
                pz = psum()
                kb.mm(pz[:C, :], adT, a_up[:64, hs:hs + 512], start=True, stop=False)
                kb.mm(pz[:C, :], ones[0:1, :C], a0_row[0:1, hs:hs + 512], start=False, stop=True)
                kb.act(a, pz[:C, :], AF.Sigmoid)
                pz = psum()
                kb.mm(pz[:C, :], sgdT, g_up[:128, hs:hs + 512])
                kb.cp("act", g, pz[:C, :])
                if RSTOP <= 1:
                    continue
                kb.tt("dve", kk, k, kk_bc[:C, hs:hs + 512], ALU.mult)
                kb.tt("dve", tmp1, kk, kk, ALU.mult)
                ss = col()
                kb.red("dve", ss[:C, 0:8], h3(tmp1, C))
                kb.act(ss[:C, 0:8], ss[:C, 0:8], AF.Sqrt)
                kb.ts("dve", ss[:C, 0:8], ss[:C, 0:8], 1e-12, None, ALU.max)
                kb.recip(ss[:C, 0:8], ss[:C, 0:8])
                kb.tt("dve", h3(kk, C), h3(kk, C), ss[:C, 0:8].re("p (h o) -> p h o", o=1).bc([C, 8, 64]), ALU.mult)
                kb.stt("dve", tmp1, a, -1.0, ka_bc[:C, hs:hs + 512], ALU.add, ALU.mult)
                kb.stt("dve", kmod, tmp1, 1.0, k, ALU.add, ALU.mult)
                kb.tt("dve", b, kk, a, ALU.mult)
                if RSTOP <= 2:
                    continue
                pG = psum()
                kb.mm(pG[:C, :], tri_i[:C, :C], wl)
                kb.cp("act", Gs, pG[:C, :])
                kb.act(rt, Gs, AF.Exp, scale=-C0)
                kb.act(eGn, Gs, AF.Exp, scale=C0)
                kb.tt("dve", tmp1, Gs, wl, ALU.subtract)
                kb.act(kap, tmp1, AF.Exp, scale=-C0)
                kb.tt("dve", kap, kap, kk, ALU.mult)
                kb.tt("dve", rt, rt, r, ALU.mult)
                kb.tt("dve", b, b, eGn, ALU.mult)
                kb.ts("dve", nbt, b, -1.0, None, ALU.mult)
                kb.tt("dve", kt, kmod, eGn, ALU.mult)
                if RSTOP <= 3:
                    continue
                for i, src in enumerate((kap, b, kt, rt)):
                    pt = psum()
                    for h in range(8):
                        kb.tr(pt[:64, h * 64:h * 64 + C], src[:, h * 64:(h + 1) * 64], ident[:C, :C])
                    kb.cp("act" if i % 2 == 0 else "dve", XT[i][:, :, :C],
                          pt[:64, :].re("p (h c) -> p h c", h=8)[:, :, :C])
                if RSTOP <= 4:
                    continue
                kapT, btT, ktT, rtT = XT
                AT, A, A2T, A3T, A4T, Q0, Q1 = AM

                def amat(dst, lT, rT, mask, neg=False):
                    pp = psum()
                    for h in range(8):
                        kb.mm(pp[:C, h * 64:h * 64 + C], lT[:, h, :C], rT[:, h, :C])
                    src = pp[:C, :].re("p (h c) -> p h c", h=8)[:, :, :C]
                    if neg:
                        kb.stt("dve", dst[:C, :, :C], src, -1.0, mask3(mask, C), ALU.mult, ALU.mult)
                    else:
                        kb.tt("dve", dst[:C, :, :C], src, mask3(mask, C), ALU.mult)

                amat(AT, btT, kapT, tri_s)
                amat(A, kapT, btT, tri_g)
                amat(A2T, ktT, kapT, tri_s)
                amat(A3T, ktT, rtT, tri_i)
                amat(A4T, btT, rtT, tri_i, neg=True)
                if RSTOP <= 5:
                    continue
                pW = psum()
                for h in range(8):
                    kb.mm(pW[:C, h * 64:(h + 1) * 64], kapT[:, h, :C], M[:, hh * 8 + h, :], start=True, stop=False)
                    kb.mm(pW[:C, h * 64:(h + 1) * 64], A2T[:C, h, :C], v[:, h * 64:(h + 1) * 64], start=False, stop=True)
                U = tmp2
                kb.cp("act", U, pW[:C, :])
                if RSTOP <= 6:
                    continue
                curP, curPT = A, AT
                nxt = [(Q0, Q1), (A2T, A), (Q0, Q1), (A2T, A), (Q0, Q1)]
                n = 0
                while True:
                    pU = psum()
                    for h in range(8):
                        kb.mm(pU[:C, h * 64:(h + 1) * 64], curPT[:C, h, :C], U[:, h * 64:(h + 1) * 64])
                    kb.tt("dve", U, U, pU[:C, :], ALU.subtract if n == 0 else ALU.add)
                    if (2 << n) >= C:
                        break
                    nP, nPT = nxt[n]
                    if n == 1:
                        nP, nPT = A2T, AT
                    p1 = psum(); p2 = psum()
                    for h in range(8):
                        kb.mm(p1[:C, h * 64:h * 64 + C], curPT[:C, h, :C], curP[:C, h, :C])
                        kb.mm(p2[:C, h * 64:h * 64 + C], curP[:C, h, :C], curPT[:C, h, :C])
                    tgt = [t for t in (Q0, Q1, A2T, A, AT) if t is not curP and t is not curPT][:2]
                    nP, nPT = tgt
                    kb.cp("act", nP[:C, :, :C], p1[:C, :].re("p (h c) -> p h c", h=8)[:, :, :C])
                    kb.cp("dve", nPT[:C, :, :C], p2[:C, :].re("p (h c) -> p h c", h=8)[:, :, :C])
                    curP, curPT = nP, nPT
                    n += 1
                if RSTOP <= 7:
                    continue
                pO = psum()
                for h in range(8):
                    sl = slice(h * 64, (h + 1) * 64)
                    kb.mm(pO[:C, sl], rtT[:, h, :C], M[:, hh * 8 + h, :], start=True, stop=False)
                    kb.mm(pO[:C, sl], A3T[:C, h, :C], v[:, sl], start=False, stop=False)
                    kb.mm(pO[:C, sl], A4T[:C, h, :C], U[:, sl], start=False, stop=True)
                if RSTOP <= 8:
                    continue
                pM = psum()
                for h in range(8):
                    sl = slice(h * 64, (h + 1) * 64)
                    kb.mm(pM[:64, sl], kt[:, sl], v[:, sl], start=True, stop=False)
                    kb.mm(pM[:64, sl], nbt[:, sl], U[:, sl], start=False, stop=True)
                pGm = psum()
                for h in range(8):
                    kb.mm(pGm[:64, 64 * h:64 * h + 64], wl[:, h * 64:(h + 1) * 64], ones[:C, 0:64])
                gam = col()
                kb.act(gam[:64, 0:8], pGm[:64, 0:512].re('p (h t) -> p h t', t=64)[:, :, 0], AF.Exp, scale=-C0)
                Mh = M[:, hh * 8:hh * 8 + 8, :]
                kb.tt("dve", Mh, Mh, pM[:64, :].re("p (h v) -> p h v", h=8), ALU.add)
                kb.tt("dve", Mh, Mh, gam[:64, 0:8].re("p (h o) -> p h o", o=1).bc([64, 8, 64]), ALU.mult)
                if RSTOP <= 9:
                    continue
                kb.cp("act", oc, pO[:C, :])
                mean = col()
                kb.red("dve", mean[:C, 0:8], h3(oc, C))
                kb.ts("dve", mean[:C, 0:8], mean[:C, 0:8], -1.0 / 64, None, ALU.mult)
                kb.tt("dve", h3(oc, C), h3(oc, C), mean[:C, 0:8].re("p (h o) -> p h o", o=1).bc([C, 8, 64]), ALU.add)
                if RSTOP <= 10:
                    continue
                kb.tt("dve", tmp1, oc, oc, ALU.mult)
                var = col()
                kb.red("dve", var[:C, 0:8], h3(tmp1, C))
                kb.rsqrt(var[:C, 0:8], var[:C, 0:8], 1.0 / 64, 64e-5)
                kb.tt("dve", h3(oc, C), h3(oc, C), var[:C, 0:8].re("p (h o) -> p h o", o=1).bc([C, 8, 64]), ALU.mult)
                if RSTOP <= 11:
                    continue
                kb.tt("dve", oc, oc, lng_bc[:C, hs:hs + 512], ALU.mult)
                kb.tt("dve", oc, oc, lnb_bc[:C, hs:hs + 512], ALU.add)
                if RSTOP <= 12:
                    continue
                kb.tt("dve", tmp1, r, kmod, ALU.mult)
                kb.tt("dve", tmp1, tmp1, rk_bc[:C, hs:hs + 512], ALU.mult)
                if RSTOP <= 13:
                    continue
                bon = col()
                kb.red("dve", bon[:C, 0:8], h3(tmp1, C))
                kb.tt("dve", h3(tmp1, C), h3(v, C), bon[:C, 0:8].re("p (h o) -> p h o", o=1).bc([C, 8, 64]), ALU.mult)
                if RSTOP <= 14:
                    continue
                kb.tt("dve", oc, oc, tmp1, ALU.add)
                if RSTOP <= 15:
                    continue
                kb.tt("dve", omix[:C, 1024 + hs:1024 + hs + 512], oc, g, ALU.mult)
            kb.dma("sp", OM[r0:r0 + C, :], omix[:C, :])
            if last:
                for hh in range(2):
                    pt = psum()
                    for h in range(8):
                        kb.tr(pt[:64, h * 64:(h + 1) * 64], M[:, hh * 8 + h, :], ident[:64, :64])
                    kb.cp("dve", Mio[:, hh * 8:hh * 8 + 8, :], pt[:64, :].re("p (h v) -> p h v", h=8))
                dst = outs["rwkv_p"] if sidx < 0 else outs["rwkv_s"][sidx]
                kb.dma("sp", dst.rearrange("h v k -> v h k"), Mio)
                lastrow = r0 + C - 1
                dsts = outs["shift_p"] if sidx < 0 else outs["shift_s"][sidx]
                kb.dma("sp", dsts.rearrange("(o n) -> o n", o=1), P0[lastrow:lastrow + 1, A_COLS:EV_COLS])
        kb.P.flush()


def ln_and_route(kb, ctx, cfg, ins, n, r0, v, layer, which, X1, X1T, GT, psum):
    col, c, ident, g_bc, b_bc = ctx["col"], ctx["c"], ctx["ident"], ctx["g_bc"], ctx["b_bc"]
    s1 = col()
    kb.red("dve", s1[:n, 0:1], v[:n, :])
    kb.ts("dve", s1[:n, 0:1], s1[:n, 0:1], -1.0 / D, None, ALU.mult)
    kb.ts("dve", v[:n, :], v[:n, :], s1[:n, 0:1], None, ALU.add)
    kb.act(c[:n, :], v[:n, :], AF.Square)
    s2 = col()
    kb.red("dve", s2[:n, 0:1], c[:n, :])
    kb.rsqrt(s2[:n, 0:1], s2[:n, 0:1], 1.0 / D, LN_EPS)
    kb.stt("dve", v[:n, :], v[:n, :], s2[:n, 0:1], g_bc[:n, :], ALU.mult, ALU.mult)
    kb.tt("dve", v[:n, :], v[:n, :], b_bc[:n, :], ALU.add)
    kb.dma("sp", X1[r0:r0 + n, :], v[:n, :])
    if X1T is None or os.environ.get('NOROUTE'):
        return
    x1T, x1Tb, wr = ctx["x1T"], ctx["x1Tb"], ctx.get("wr")
    for grp in range(4):
        pt = psum()
        for j in range(4):
            kc = grp * 4 + j
            kb.tr(pt[:, j * 128:j * 128 + n], v[:n, kc * 128:(kc + 1) * 128], ident[:n, :n])
        src = pt.re("p (j t) -> p j t", j=4)[:, :, :n]
        kb.cp("act", x1T[:, grp * 4:grp * 4 + 4, :n], src)
        kb.cp("dve", x1Tb[:, grp * 4:grp * 4 + 4, :n], x1T[:, grp * 4:grp * 4 + 4, :n])
    if not os.environ.get('NOX1T'):
        kb.dma("sp", X1T[:, :, r0:r0 + n].rearrange("kc p t -> p kc t"), x1Tb[:, :, :n])
    if GT is None:
        return
    pl = psum()
    for kc in range(16):
        kb.mm(pl[:n, 0:128], x1T[:, kc, :n], wr[:, kc, :], start=(kc == 0), stop=(kc == 15))
    if os.environ.get('NOGATE'):
        return
    sm = ctx["sm"]
    l = sm(); e = sm(); t3 = sm(); t4 = sm(); gate = sm()
    BIG = 1e30
    kb.cp("dve", l[:n, 0:16], pl[:n, 0:16])
    mx = col()
    kb.red("dve", mx[:n, 0:1], l[:n, 0:16], op=ALU.max)
    kb.ts("dve", mx[:n, 0:1], mx[:n, 0:1], -1.0, None, ALU.mult)
    kb.act(e[:n, 0:16], l[:n, 0:16], AF.Exp, bias=mx[:n, 0:1], scale=1.0)
    e3 = e[:n, 0:16].re("p (g j) -> p g j", g=4)
    m1 = col(); m2 = col(); sc = col(); oh = col()
    kb.red("dve", m1[:n, 0:4], e3, op=ALU.max)
    t33 = t3[:n, 0:16].re("p (g j) -> p g j", g=4)
    kb.tt("dve", t33, e3, m1[:n, 0:4].re("p (g o) -> p g o", o=1).bc([n, 4, 4]), ALU.is_equal)
    kb.stt("dve", t33, t33, -BIG, e3, ALU.mult, ALU.add)
    kb.red("dve", m2[:n, 0:4], t33, op=ALU.max)
    kb.tt("dve", sc[:n, 0:4], m1[:n, 0:4], m2[:n, 0:4], ALU.add)
    gm = col()
    kb.red("dve", gm[:n, 0:1], sc[:n, 0:4], op=ALU.max)
    kb.ts("dve", oh[:n, 0:4], sc[:n, 0:4], gm[:n, 0:1], None, ALU.is_equal)
    kb.tt("dve", t33, e3, oh[:n, 0:4].re("p (g o) -> p g o", o=1).bc([n, 4, 4]), ALU.mult)
    ing = col()
    kb.red("dve", ing[:n, 0:4], t3[:n, 0:16].re("p (g j) -> p j g", g=4))
    v1 = col(); v2 = col(); q1 = col(); q2 = col(); i2 = col()
    kb.red("dve", v1[:n, 0:1], ing[:n, 0:4], op=ALU.max)
    kb.ts("dve", q1[:n, 0:4], ing[:n, 0:4], v1[:n, 0:1], None, ALU.is_equal)
    kb.stt("dve", i2[:n, 0:4], q1[:n, 0:4], -BIG, ing[:n, 0:4], ALU.mult, ALU.add)
    kb.red("dve", v2[:n, 0:1], i2[:n, 0:4], op=ALU.max)
    kb.ts("dve", q2[:n, 0:4], i2[:n, 0:4], v2[:n, 0:1], None, ALU.is_equal)
    kb.tt("dve", q1[:n, 0:4], q1[:n, 0:4], q2[:n, 0:4], ALU.add)
    kb.tt("dve", v1[:n, 0:1], v1[:n, 0:1], v2[:n, 0:1], ALU.add)
    kb.recip(v1[:n, 0:1], v1[:n, 0:1])
    kb.tt("dve", q1[:n, 0:4], q1[:n, 0:4], ing[:n, 0:4], ALU.mult)
    kb.ts("dve", q1[:n, 0:4], q1[:n, 0:4], v1[:n, 0:1], None, ALU.mult)
    kb.tt("dve", gate[:n, 0:16].re("p (g j) -> p g j", g=4),
          oh[:n, 0:4].re("p (g o) -> p g o", o=1).bc([n, 4, 4]),
          q1[:n, 0:4].re("p (o j) -> p o j", o=1).bc([n, 4, 4]), ALU.mult)
    pg = psum()
    kb.tr(pg[:16, :n], gate[:n, 0:16], ident[:n, :n])
    gT = sm()
    kb.cp("dve", gT[:16, :n], pg[:16, :n])
    kb.dma("sp", GT[:, r0:r0 + n], gT[:16, :n])


def ln_ctx(kb, ctx, ins, gname, bname, layer, route=True, trans=False):
    st = ctx["st"]
    ctx["ident"] = ctx["consts"][:, C_IDENT:C_IDENT + 128]
    ctx["g_bc"] = kb.sb(st, [128, D], F32, "g_bc")
    kb.dma("sp", ctx["g_bc"], row_bcast(ins[gname][layer], 128))
    ctx["b_bc"] = kb.sb(st, [128, D], F32, "b_bc")
    kb.dma("sp", ctx["b_bc"], row_bcast(ins[bname][layer], 128))
    ctx["c"] = kb.sb(st, [128, D], F32, "c")
    ctx["col"] = Rot([kb.sb(st, [128, 4], F32, f"col{i}") for i in range(24)])
    if route or trans:
        ctx["x1T"] = kb.sb(st, [128, 16, 128], F32, "x1T")
        ctx["x1Tb"] = kb.sb(st, [128, 16, 128], BF16, "x1Tb")
    if route:
        ctx["wr"] = kb.sb(st, [128, 16, 128], F32, "wr")
        kb.memset("dve", ctx["wr"], 0.0)
        kb.dma("sp", ctx["wr"][:, :, 0:16], ins["w_router"].rearrange("(kc p) e -> p kc e", p=128))
        ctx["sm"] = Rot([kb.sb(st, [128, 128], F32, f"sm{i}") for i in range(8)])


def phase_outln(kb, cfg, ins, src, W, xres, layer, X1, X1T, GT, srcT=None):
    def pre(ctx):
        ln_ctx(kb, ctx, ins, "ln_mix_g", "ln_mix_b", layer)
        ctx["xr"] = [kb.sb(ctx["st"], [128, D], F32, "xr") for _ in range(2)]
        ctx["v"] = kb.sb(ctx["st"], [128, D], F32, "v")
        ctx["ps2"] = Rot([kb.ps(ctx["st"], [128, 512], F32, f"ps2{i}") for i in range(2)])

    def epi(ctx, ts, r0, n, cb, cw, pa):
        xr = ctx["xr"][ts % 2]
        if cb == 0:
            kb.dma("sp", xr[:n, :], xres[r0:r0 + n, :])
        kb.stt("dve", ctx["v"][:n, cb * 512:cb * 512 + cw], xr[:n, cb * 512:cb * 512 + cw], ALPHA, pa[:n, :cw], ALU.mult, ALU.add)

    def post(ctx, ts, r0, n):
        ln_and_route(kb, ctx, cfg, ins, n, r0, ctx["v"], layer, "mix", X1, X1T, GT, ctx["ps2"])

    dense_phase(kb, cfg, ins["consts"], src, W, D, epi, post=post, pre=pre, srcT=srcT)


def inherit(dsts, srcs):
    w = [op for b in srcs for op in b.w]
    r = [op for b in srcs for op in b.r]
    for d in dsts:
        d.w = list(w)
        d.r = list(r)


def phase_moe(kb, cfg, ins, layer, X1, X1T, GT, OUT, OUTT=None):
    NT = cfg.NT
    w_up, w_dn = ins["moe_w_up"][layer], ins["moe_w_down"][layer]
    ntile = (NT + 1095) // 1096
    base = ((NT + ntile - 1) // ntile + 7) // 8 * 8
    tiles = []
    t0 = 0
    while t0 < NT:
        tm = min(base, NT - t0)
        tiles.append((t0, tm))
        t0 += tm
    TMAX = max(tm for _, tm in tiles)
    NSUB = (TMAX + 127) // 128
    with contextlib.ExitStack() as st:
        xT = kb.sb(st, [128, 16, TMAX], BF16, "xT")
        yacc = kb.sb(st, [128, NSUB, D], F32, "yacc")
        gb = kb.sb(st, [128, TMAX], F32, "gbc")
        hhT = kb.sb(st, [128, 8, TMAX], BF16, "hhT")
        arWd = kb.sb(st, [128, 8192], F32, "arWd")
        arW = kb.sb(st, [128, 8192], F32, "arW")
        sA = Rot([kb.sb(st, [128, 512], F32, f"sA{i}") for i in range(2)])
        sB = Rot([kb.sb(st, [128, 512], F32, f"sB{i}") for i in range(2)])
        ident = kb.sb(st, [128, 128], F32, "ident")
        kb.dma("sp", ident, ins["consts"][:, 0:128])
        col = Rot([kb.sb(st, [128, 4], F32, f"col{i}") for i in range(24)])
        psum = Rot([kb.ps(st, [128, 512], F32, f"ps{i}") for i in range(8)])
        Wd = V(arWd.ap.bitcast(BF16).rearrange("p (f n) -> p f n", f=8), Buf("Wd"))
        Wv = [V(arW.ap[:, i * 2048:(i + 1) * 2048].bitcast(BF16).rearrange("p (k n) -> p k n", k=16), Buf(f"W{i}")) for i in range(4)]
        W1 = [Wv[0], Wv[2]]
        W2 = [Wv[1], Wv[3]]
        g_bc = V(arWd.ap[:, 0:2048], Buf("g_bc"))
        b_bc = V(arWd.ap[:, 2048:4096], Buf("b_bc"))
        cc = V(arWd.ap[:, 4096:6144], Buf("c"))
        xr = V(arWd.ap[:, 6144:8192], Buf("xr"))
        x1T = V(arW.ap[:, 0:2048].rearrange("p (k n) -> p k n", k=16), Buf("x1T"))
        x1Tb = V(arW.ap[:, 2048:3072].bitcast(BF16).rearrange("p (k n) -> p k n", k=16), Buf("x1Tb"))
        ctx = {"ident": ident, "g_bc": g_bc, "b_bc": b_bc, "c": cc, "col": col, "x1T": x1T, "x1Tb": x1Tb}
        for (t0, tm) in tiles:
            nsub = (tm + 127) // 128
            cblocks = [(c0, min(512, tm - c0)) for c0 in range(0, tm, 512)]
            inherit([Wd.buf], [g_bc.buf, b_bc.buf, cc.buf, xr.buf])
            inherit([w.buf for w in Wv], [x1T.buf, x1Tb.buf])
            kb.dma("sp", xT[:, :, :tm], X1T[:, :, t0:t0 + tm].rearrange("kc p t -> p kc t"))
            iw = 0
            for e in range(16):
                kb.dma("sp", gb[:, :tm], GT[e:e + 1, t0:t0 + tm].to_broadcast([128, tm]))
                wup = w_up[e].rearrange("(kc p) n -> p kc n", p=128)
                kb.dma("pool", Wd, w_dn[e].rearrange("(fc p) n -> p fc n", p=128))
                for fb in range(4):
                    w1, w2 = W1[iw % 2], W2[iw % 2]
                    iw += 1
                    kb.dma("pool", w1, wup[:, :, fb * 256:(fb + 1) * 256])
                    kb.dma("pool", w2, wup[:, :, 1024 + fb * 256:1024 + (fb + 1) * 256])
                    for f2 in range(2):
                        fc = fb * 2 + f2
                        for (c0, cw) in cblocks:
                            pA = psum(); pB = psum()
                            for kc in range(16):
                                kb.mm(pA[:, :cw], w1[:, kc, f2 * 128:(f2 + 1) * 128], xT[:, kc, c0:c0 + cw], start=(kc == 0), stop=(kc == 15))
                            for kc in range(16):
                                kb.mm(pB[:, :cw], w2[:, kc, f2 * 128:(f2 + 1) * 128], xT[:, kc, c0:c0 + cw], start=(kc == 0), stop=(kc == 15))
                            a = sA(); b = sB()
                            kb.act(a[:, :cw], pA[:, :cw], AF.Silu)
                            kb.tt("dve", b[:, :cw], pB[:, :cw], gb[:, c0:c0 + cw], ALU.mult)
                            kb.tt("dve", hhT[:, fc, c0:c0 + cw], a[:, :cw], b[:, :cw], ALU.mult)
                for su in range(nsub):
                    n = min(128, tm - su * 128)
                    for db in range(4):
                        py = psum()
                        for fc in range(8):
                            kb.mm(py[:n, :], hhT[:, fc, su * 128:su * 128 + n], Wd[:, fc, db * 512:(db + 1) * 512],
                                  start=(fc == 0), stop=(fc == 7))
                        ya = yacc[:n, su, db * 512:(db + 1) * 512]
                        if e == 0:
                            kb.cp("act", ya, py[:n, :])
                        else:
                            kb.tt("dve", ya, ya, py[:n, :], ALU.add)
            inherit([g_bc.buf, b_bc.buf, cc.buf, xr.buf], [Wd.buf])
            inherit([x1T.buf, x1Tb.buf], [w.buf for w in Wv])
            kb.dma("sp", g_bc, row_bcast(ins["ln_ffn_g"][layer], 128))
            kb.dma("sp", b_bc, row_bcast(ins["ln_ffn_b"][layer], 128))
            for su in range(nsub):
                n = min(128, tm - su * 128)
                r0 = t0 + su * 128
                kb.dma("sp", xr[:n, :], X1[r0:r0 + n, :])
                kb.stt("dve", yacc[:n, su, :], xr[:n, :], ALPHA, yacc[:n, su, :], ALU.mult, ALU.add)
                ln_and_route(kb, ctx, cfg, ins, n, r0, yacc[:, su, :], layer, "ffn", OUT, OUTT, None, psum)
        kb.P.flush()


_CACHE = {}


def kernel(**inputs):
    cfg = Cfg()
    if "nc" not in _CACHE:
        _CACHE["nc"] = build(cfg)
    nc, ins, outs = _CACHE["nc"]
    f = lambda k: np.ascontiguousarray(np.asarray(inputs[k], dtype=np.float32))
    consts = make_consts()
    shared = {"consts": consts}
    for k in WEIGHT_SHAPES:
        a = f(k)
        if k.startswith("moe_") or k.startswith("ln_") or k == "w_router":
            shared[k] = a
        else:
            shared[k] = np.ascontiguousarray(a[0])
    xp, xs, meta = f("x_prompt"), f("x_sample"), f("meta")
    in_maps = []
    for c in range(8):
        b = c % 4
        s0 = 16 * c
        m = dict(shared)
        m["xtok"] = np.ascontiguousarray(np.concatenate([meta, xp[b], xs[s0:s0 + 16].reshape(128, D)], 0))
        m["state_gla"] = np.ascontiguousarray(f("state_gla")[0, s0:s0 + 16])
        m["state_rwkv"] = np.ascontiguousarray(f("state_rwkv")[0, s0:s0 + 16])
        m["state_shift"] = np.ascontiguousarray(f("state_shift")[0, s0:s0 + 16])
        m["state_s5_re"] = np.ascontiguousarray(f("state_s5_re")[0, s0:s0 + 16])
        m["state_s5_im"] = np.ascontiguousarray(f("state_s5_im")[0, s0:s0 + 16])
        in_maps.append({k: m[k] for k in ins})
    res = run_bass_kernel_spmd(nc, in_maps, core_ids=list(range(8)))
    R = res.results
    NP = cfg.NP
    y_p = np.stack([R[b]["y"][16:NP] for b in range(4)], 0)
    y_s = np.concatenate([R[c]["y"][NP:].reshape(16, 8, D) for c in range(8)], 0)

    def pr(name):
        return np.stack([R[b][name] for b in range(4)], 0)[None]

    def sm(name):
        return np.concatenate([R[c][name] for c in range(8)], 0)[None]

    return (y_p.astype(np.float32), y_s.astype(np.float32),
            pr("gla_p"), sm("gla_s"), pr("rwkv_p"), sm("rwkv_s"), pr("shift_p"), sm("shift_s"),
            pr("s5re_p"), sm("s5re_s"), pr("s5im_p"), sm("s5im_s"))


def dense_T(kb, cfg, ins, srcT, W, epi, pre=None, TB=512):
    NT = cfg.NT
    with contextlib.ExitStack() as st:
        Wb = kb.sb(st, [128, 16, D], BF16, "Wb")
        Wv = W.rearrange("(kc p) n -> p kc n", p=128)
        for g in range(4):
            kb.dma("pool", Wb[:, 4 * g:4 * g + 4, :], Wv[:, 4 * g:4 * g + 4, :])
        xT = [kb.sb(st, [128, 16, TB], BF16, "xT") for _ in range(2)]
        psum = Rot([kb.ps(st, [128, 512], F32, f"ps{i}") for i in range(8)])
        ctx = {"st": st}
        if pre is not None:
            pre(ctx)
        t0 = 0
        i = 0
        while t0 < NT:
            tb = min(TB, NT - t0)
            x = xT[i % 2]
            kb.dma("sp", x[:, :, :tb], srcT[:, :, t0:t0 + tb].rearrange("kc p t -> p kc t"))
            for cc in range(16):
                pa = psum()
                for kc in range(16):
                    kb.mm(pa[:, :tb], Wb[:, kc, cc * 128:(cc + 1) * 128], x[:, kc, :tb], start=(kc == 0), stop=(kc == 15))
                epi(ctx, cc, t0, tb, pa, x)
            t0 += tb
            i += 1
        kb.P.flush()


def phase_s5(kb, cfg, ins, outs, UT, ZT):
    NT, NP = cfg.NT, cfg.NP
    LMAX = 80 if cfg.NT % 64 == 16 else 64
    with contextlib.ExitStack() as st:
        cs = kb.sb(st, [128, C_TOTAL], F32, "consts")
        kb.dma("sp", cs, ins["consts"])
        ident = cs[:, C_IDENT:C_IDENT + 128]
        pm = cs[:, C_PM:C_PM + 8]
        psum = Rot([kb.ps(st, [128, 512], F32, f"ps{i}") for i in range(8)])

        def pj(name):
            t = kb.sb(st, [128, 64], F32, name)
            kb.dma("sp", t, ins[name].rearrange("(j gl) p -> (gl p) j", gl=2), allow_slow_non_contiguous=True)
            return t

        are, aim = pj("c_a_re"), pj("c_a_im")
        dt = kb.sb(st, [128, 64], F32, "dt")
        ld = ins["c_log_dt"].rearrange("(j gl) -> gl j", gl=2)
        for gl in range(2):
            kb.dma("sp", dt[gl * 64:(gl + 1) * 64, :], ld[gl:gl + 1, :].to_broadcast([64, 64]), allow_slow_non_contiguous=True)
        kb.act(dt, dt, AF.Exp)
        T = [kb.sb(st, [128, 64], F32, f"pt{i}") for i in range(10)]
        er, sn, cn, Are, Aim, x_, den, cre, cim, tmp = T
        kb.tt("dve", tmp, are, dt, ALU.mult)
        kb.act(er, tmp, AF.Exp)
        kb.tt("dve", tmp, aim, dt, ALU.mult)
        kb.act(sn, tmp, AF.Sin, scale=1.0 / 16)
        kb.ts("dve", tmp, tmp, 1.0 / 16, math.pi / 2, ALU.mult, ALU.add)
        kb.act(cn, tmp, AF.Sin)
        for _ in range(4):
            kb.tt("dve", tmp, sn, cn, ALU.mult)
            kb.tt("dve", cn, cn, cn, ALU.mult)
            kb.tt("dve", sn, sn, sn, ALU.mult)
            kb.tt("dve", cn, cn, sn, ALU.subtract)
            kb.ts("dve", sn, tmp, 2.0, None, ALU.mult)
        kb.tt("dve", Are, er, cn, ALU.mult)
        kb.tt("dve", Aim, er, sn, ALU.mult)
        kb.ts("dve", x_, Are, -1.0, None, ALU.add)
        kb.tt("dve", den, are, are, ALU.mult)
        kb.tt("dve", tmp, aim, aim, ALU.mult)
        kb.tt("dve", den, den, tmp, ALU.add)
        kb.recip(den, den)
        kb.tt("dve", cre, x_, are, ALU.mult)
        kb.tt("dve", tmp, Aim, aim, ALU.mult)
        kb.tt("dve", cre, cre, tmp, ALU.add)
        kb.tt("dve", cre, cre, den, ALU.mult)
        kb.tt("dve", cim, Aim, are, ALU.mult)
        kb.tt("dve", tmp, x_, aim, ALU.mult)
        kb.tt("dve", cim, cim, tmp, ALU.subtract)
        kb.tt("dve", cim, cim, den, ALU.mult)
        LBb = [kb.sb(st, [128, 16, 128], BF16, f"LBb{i}") for i in range(2)]
        LCb = [kb.sb(st, [128, 16, 128], BF16, f"LCb{i}") for i in range(2)]
        LB3b = [kb.sb(st, [128, 16, 128], BF16, f"LB3b{i}") for i in range(2)]
        with contextlib.ExitStack() as st2:
            LB = [kb.sb(st2, [128, 16, 128], F32, f"LB{i}") for i in range(2)]
            LC = [kb.sb(st2, [128, 16, 128], F32, f"LC{i}") for i in range(2)]
            LB3 = [kb.sb(st2, [128, 16, 128], F32, f"LB3{i}") for i in range(2)]
            Bre = kb.sb(st2, [128, 64, 16], F32, "Bre")
            Bim = kb.sb(st2, [128, 64, 16], F32, "Bim")
            kb.dma("sp", Bre, ins["c_b_re"].rearrange("(j gl) p c -> (gl p) j c", gl=2))
            kb.dma("sp", Bim, ins["c_b_im"].rearrange("(j gl) p c -> (gl p) j c", gl=2))
            Xr = kb.sb(st2, [128, 64, 16], F32, "Xr")
            Xi = kb.sb(st2, [128, 64, 16], F32, "Xi")
            t1 = kb.sb(st2, [128, 64, 16], F32, "t1")
            X4 = kb.sb(st2, [128, 64, 2, 16], F32, "X4")
            bc = lambda v: v.re("p (j o) -> p j o", o=1).bc([128, 64, 16])
            kb.tt("dve", Xr, Bre, bc(cre), ALU.mult)
            kb.tt("dve", t1, Bim, bc(cim), ALU.mult)
            kb.tt("dve", Xr, Xr, t1, ALU.subtract)
            kb.tt("dve", Xi, Bim, bc(cre), ALU.mult)
            kb.tt("dve", t1, Bre, bc(cim), ALU.mult)
            kb.tt("dve", Xi, Xi, t1, ALU.add)
            for i, X in enumerate((Xr, Xi)):
                for gl in range(2):
                    kb.ts("dve", X4[:, :, gl, :], X, pm[:, gl:gl + 1], None, ALU.mult)
                for m in range(16):
                    pt = psum()
                    kb.tr(pt[:, 0:128], X4[:, 4 * m:4 * m + 4, :, :].re("p a b c -> p (a b c)"), ident)
                    kb.cp("act" if m % 2 == 0 else "dve", LB[i][:, m, :], pt[:, 0:128])
            for i in range(2):
                kb.ts("dve", LB3[i][64:128, :, :], LB[i][64:128, :, :], pm[64:128, 7:8], None, ALU.mult)
            Rr = kb.sb(st2, [128, 16, 64], F32, "Rr")
            R4 = kb.sb(st2, [128, 16, 2, 64], F32, "R4")
            for i, name in enumerate(("c_c_re", "c_c_im")):
                kb.dma("sp", Rr, ins[name].rearrange("(m qg) c p -> (qg c) m p", m=16))
                for gl in range(2):
                    kb.ts("dve", R4[:, :, gl, :], Rr, pm[:, 2 + gl:3 + gl], (1.0 if i == 0 else -1.0), ALU.mult, ALU.mult)
                for m in range(16):
                    pt = psum()
                    kb.tr(pt[:, 0:128], R4[:, m, :, :].re("p a b -> p (a b)"), ident)
                    kb.cp("act" if m % 2 == 0 else "dve", LC[i][:, m, :], pt[:, 0:128])
            for i in range(2):
                kb.cp("dve", LBb[i], LB[i])
                kb.cp("act", LCb[i], LC[i])
                kb.cp("dve", LB3b[i][64:128, :, :], LB3[i][64:128, :, :])
            kb.P.flush()
        S5STOP = int(os.environ.get("L1STOP", "99"))
        if S5STOP <= 2:
            return
        Dcol = kb.sb(st, [128, 16], F32, "Dcol")
        kb.dma("sp", Dcol, ins["c_d"].rearrange("(m p) -> p m", p=128), allow_slow_non_contiguous=True)
        HS = [kb.sb(st, [128, 64, LMAX], F32, f"HS{i}") for i in range(2)]
        BUs = [[kb.sb(st, [128, 64, LMAX], F32, f"BU{b}{i}") for i in range(2)] for b in range(2)]
        carry = [kb.sb(st, [128, 64], F32, f"carry{i}") for i in range(2)]
        init = [kb.sb(st, [128, 64], F32, f"init{i}") for i in range(2)]
        sio = [kb.sb(st, [64, 128], F32, f"sio{i}") for i in range(2)]
        sT = [kb.sb(st, [128, 64], F32, f"st{i}") for i in range(4)]
        uT = [kb.sb(st, [128, 16, LMAX], F32, f"uT{i}") for i in range(2)]
        ub = [kb.sb(st, [128, 16, LMAX], BF16, f"ub{i}") for i in range(2)]
        HSb = [kb.sb(st, [128, 64, LMAX], BF16, f"HSb{i}") for i in range(2)]
        yT = kb.sb(st, [128, LMAX], F32, "yT")
        yq = kb.sb(st, [128, LMAX], F32, "yq")
        zb = Rot([kb.sb(st, [128, LMAX], BF16, f"zb{i}") for i in range(2)])
        starts = {0: -1}
        ends = {NP - 1: -1}
        for sidx in range(cfg.n_samp):
            starts[NP + 8 * sidx] = sidx
            ends[NP + 8 * sidx + 7] = sidx
        blocks = []
        t0 = 0
        while t0 < NT:
            rem = NT - t0
            L = 64 if (rem == 64 or rem - 64 >= 32) else rem
            assert L <= LMAX
            blocks.append((t0, L))
            t0 += L

        def bproj(bi):
            t0, L = blocks[bi]
            u = uT[bi % 2]
            kb.dma("sp", u[:, :, :L], UT[:, :, t0:t0 + L].rearrange("kc p t -> p kc t"))
            uq = ub[bi % 2]
            kb.cp("act", uq[:, :, :L], u[:, :, :L])
            ppb = min(8, 512 // L)
            for i in range(2):
                BUv = BUs[bi % 2][i].re("p (m q) t -> p q m t", q=4)
                for q in range(4):
                    m0 = 0
                    while m0 < 16:
                        nm = min(ppb, 16 - m0)
                        pp = psum()
                        for m in range(m0, m0 + nm):
                            osl = pp[:, (m - m0) * L:(m - m0 + 1) * L]
                            if q < 3:
                                kb.mm(osl, LBb[i][32 * q:32 * q + 32, m, :], uq[32 * q:32 * q + 32, m, :L])
                            else:
                                kb.mm(osl, LB3b[i][64:128, m, :], uq[64:128, m, :L])
                        kb.cp("act", BUv[:, q, m0:m0 + nm, :L], pp[:, 0:nm * L].re("p (a t) -> p a t", t=L))
                        m0 += nm

        bproj(0)
        for bi, (t0, L) in enumerate(blocks):
            u = uT[bi % 2]
            BU = BUs[bi % 2]
            if bi + 1 < len(blocks):
                bproj(bi + 1)
            for tl in range(L):
                t = t0 + tl
                if t in starts:
                    sidx = starts[t]
                    if sidx < 0:
                        kb.memset("dve", init[0], 0.0)
                        kb.memset("dve", init[1], 0.0)
                    else:
                        for i, nm in enumerate(("state_s5_re", "state_s5_im")):
                            kb.dma("sp", sio[i], ins[nm][sidx].rearrange("(j gl) p -> j (gl p)", gl=2))
                            pt = psum()
                            kb.tr(pt[:, 0:64], sio[i], ident[:64, :64])
                            kb.cp("dve", init[i], pt[:, 0:64])
                    pr, pi = init[0], init[1]
                elif tl == 0:
                    pr, pi = carry[0], carry[1]
                else:
                    pr, pi = HS[0][:, :, tl - 1], HS[1][:, :, tl - 1]
                kb.tt("dve", sT[0], Are, pr, ALU.mult)
                kb.tt("pool", sT[2], Are, pi, ALU.mult)
                kb.tt("dve", sT[1], Aim, pi, ALU.mult)
                kb.tt("pool", sT[3], Aim, pr, ALU.mult)
                kb.tt("dve", sT[0], sT[0], sT[1], ALU.subtract)
                kb.tt("pool", sT[2], sT[2], sT[3], ALU.add)
                kb.tt("dve", HS[0][:, :, tl], sT[0], BU[0][:, :, tl], ALU.add)
                kb.tt("pool", HS[1][:, :, tl], sT[2], BU[1][:, :, tl], ALU.add)
                if t in ends:
                    sidx = ends[t]
                    for i, (pn, sn_) in enumerate((("s5re_p", "s5re_s"), ("s5im_p", "s5im_s"))):
                        pt = psum()
                        kb.tr(pt[:64, 0:128], HS[i][:, :, tl], ident)
                        so = sio[i]
                        kb.cp("dve", so, pt[:64, 0:128])
                        dst = outs[pn] if sidx < 0 else outs[sn_][sidx]
                        kb.dma("sp", dst.rearrange("(j gl) p -> j (gl p)", gl=2), so)
            kb.cp("dve", carry[0], HS[0][:, :, L - 1])
            kb.cp("dve", carry[1], HS[1][:, :, L - 1])
            kb.cp("act", HSb[0][:, :, :L], HS[0][:, :, :L])
            kb.cp("act", HSb[1][:, :, :L], HS[1][:, :, :L])
            for m in range(16):
                pps = [psum(), psum()] if 4 * L > 512 else [psum()]
                offs = []
                for q in range(4):
                    pp = pps[q // 2] if len(pps) == 2 else pps[0]
                    off = (q % 2 if len(pps) == 2 else q) * L
                    offs.append((pp, off))
                    kb.mm(pp[:, off:off + L], LCb[0][:, m, :], HSb[0][:, 4 * m + q, :L], start=True, stop=False)
                    kb.mm(pp[:, off:off + L], LCb[1][:, m, :], HSb[1][:, 4 * m + q, :L], start=False, stop=True)
                for q in range(4):
                    pp, off = offs[q]
                    if q == 0:
                        kb.ts("dve", yq[:, :L], pp[:, off:off + L], pm[:, 4:5], None, ALU.mult)
                    else:
                        kb.stt("dve", yq[:, :L], pp[:, off:off + L], pm[:, 4 + q:5 + q], yq[:, :L], ALU.mult, ALU.add)
                kb.stt("dve", yT[:, :L], u[:, m, :L], Dcol[:, m:m + 1], yq[:, :L], ALU.mult, ALU.add)
                z = zb()
                kb.act(z[:, :L], yT[:, :L], AF.Gelu_apprx_tanh)
                kb.dma("sp", ZT[m, :, t0:t0 + L], z[:, :L])
        kb.P.flush()


def phase_l1(kb, cfg, ins, outs, S):
    def pre_a(ctx):
        ctx["ot"] = Rot([kb.sb(ctx["st"], [128, 512], F32, "ot") for _ in range(4)])
        ctx["i"] = 0

    def epi_a(ctx, cc, t0, tb, pa, x):
        ot = ctx["ot"]()
        ctx["i"] += 1
        kb.cp("act" if ctx["i"] % 2 == 0 else "dve", ot[:, :tb], pa[:, :tb])
        kb.dma("sp", S["UT"][cc, :, t0:t0 + tb], ot[:, :tb])

    L1STOP = int(os.environ.get("L1STOP", "99"))
    dense_T(kb, cfg, ins, S["X2T"], ins["od_w_in"], epi_a, pre=pre_a)
    if L1STOP <= 1:
        return
    phase_s5(kb, cfg, ins, outs, S["UT"], S["ZT"])
    if L1STOP <= 5:
        return

    def pre_c(ctx):
        ctx["sg"] = Rot([kb.sb(ctx["st"], [128, 512], F32, "sg") for _ in range(2)])
        ctx["zz"] = Rot([kb.sb(ctx["st"], [128, 512], BF16, "zz") for _ in range(4)])
        ctx["bcol"] = kb.sb(ctx["st"], [128, 16], F32, "bcol")
        kb.dma("sp", ctx["bcol"], ins["c_b_glu"].rearrange("(m p) -> p m", p=128), allow_slow_non_contiguous=True)

    def epi_c(ctx, cc, t0, tb, pa, x):
        sg = ctx["sg"]()
        zz = ctx["zz"]()
        kb.act(sg[:, :tb], pa[:, :tb], AF.Sigmoid, bias=ctx["bcol"][:, cc:cc + 1], scale=1.0)
        kb.tt("dve", zz[:, :tb], x[:, cc, :tb], sg[:, :tb], ALU.mult)
        kb.dma("sp", S["ZZT"][cc, :, t0:t0 + tb], zz[:, :tb])

    dense_T(kb, cfg, ins, S["ZT"], ins["c_w_glu"], epi_c, pre=pre_c)
    phase_outln(kb, cfg, ins, None, ins["od_w_out"], S["X2"], 1, S["X3"], S["X3T"], S["GT3"], srcT=S["ZZT"])
```
